# Optimizing a Trainium2 kernel written in Bass

```python
import functools
import jax, jax.numpy as jnp
from jax import lax
import numpy as np

D_MODEL = 1024
BATCH = 32
SEQ = 2048
DEPTH = 4

CTX_LEN = 256
GRID_W = 64

HA_HEADS = 4
HA_DK = 128
HA_DV = 128
RB_HEADS = 4
RB_DK = 128
RB_DV = 128
GC_HEADS = 4
GC_DK = 128
GC_DV = 256
GC_RANK = 16
GC_TAU = 16.0
N_EXPERTS = 16
EXPERT_FF = 2816
EC_CAPACITY_FACTOR = 2

CHUNK = 64
SUB = 16
N_SUB = CHUNK // SUB
ROPE_BASE = 10000.0
LN_EPS = 1e-5
RMS_EPS = 1e-6
LB_FLOOR = 1e-30
DN_ALPHA = (2 * DEPTH) ** 0.25
DN_BETA = (8 * DEPTH) ** -0.25
N_EVEN = (DEPTH + 1) // 2
N_ODD = DEPTH // 2

HA_KEY = HA_HEADS * HA_DK
HA_VAL = HA_HEADS * HA_DV
RB_KEY = RB_HEADS * RB_DK
RB_VAL = RB_HEADS * RB_DV
GC_KEY = GC_HEADS * GC_DK
GC_VAL = GC_HEADS * GC_DV
EVEN_SIZES = (HA_KEY, HA_KEY, HA_KEY, HA_VAL, HA_VAL, RB_KEY, RB_KEY, RB_VAL, RB_VAL)
EVEN_IN = sum(EVEN_SIZES)
EVEN_OUT = HA_VAL + RB_VAL
ODD_SIZES = (GC_KEY, GC_KEY, GC_VAL, GC_VAL, 2 * GC_RANK)
ODD_IN = sum(ODD_SIZES)
ODD_OUT = GC_VAL

kernel_name = 'hybrid_hgrn2_retnet_gla_ecmoe_diffusion'


def _split(p, sizes):
    return jnp.split(p, np.cumsum(sizes)[:-1].tolist(), axis=-1)


def _heads(t, n_heads):
    b, l, _ = t.shape
    return t.reshape(b, l, n_heads, -1).transpose(0, 2, 1, 3)


def _merge(t):
    b, h, l, d = t.shape
    return t.transpose(0, 2, 1, 3).reshape(b, l, h * d)


def _layer_norm(x, w, b):
    xf = x.astype(jnp.float32)
    mu = xf.mean(-1, keepdims=True)
    var = jnp.square(xf - mu).mean(-1, keepdims=True)
    y = (xf - mu) * lax.rsqrt(var + LN_EPS) * w.astype(jnp.float32) + b.astype(jnp.float32)
    return y.astype(x.dtype)


def _rms_norm(x, w=None):
    y = x * lax.rsqrt(jnp.mean(jnp.square(x), -1, keepdims=True) + RMS_EPS)
    return y if w is None else y * w.astype(jnp.float32)


def _axial_rope(rows):
    r_idx, c_idx = jnp.meshgrid(jnp.arange(rows), jnp.arange(GRID_W), indexing='ij')
    n_freq = RB_DK // 4
    freq = ROPE_BASE ** (-jnp.arange(n_freq, dtype=jnp.float32) / n_freq)
    ang = jnp.concatenate([r_idx.reshape(-1, 1).astype(jnp.float32) * freq,
                           c_idx.reshape(-1, 1).astype(jnp.float32) * freq], axis=-1)
    return jnp.cos(ang), jnp.sin(ang)


def _apply_rope(t, cos, sin):
    t1, t2 = jnp.split(t, 2, axis=-1)
    return jnp.concatenate([t1 * cos - t2 * sin, t1 * sin + t2 * cos], axis=-1)


def _state_pass(chunk_kv, chunk_decay, s0):
    def step(s, inp):
        kv, dec = inp
        return dec[..., None] * s + kv, s
    s_final, s_prev = lax.scan(step, s0, (jnp.moveaxis(chunk_kv, 2, 0), jnp.moveaxis(chunk_decay, 2, 0)))
    return jnp.moveaxis(s_prev, 0, 2), s_final


def _gated_chunk_scan(q, k, v, log_f, s0):
    b_, h_, l_, kd = q.shape
    vd = v.shape[-1]
    n = l_ // CHUNK
    q, k, log_f = [t.reshape(b_, h_, n, CHUNK, kd) for t in (q, k, log_f)]
    v = v.reshape(b_, h_, n, CHUNK, vd)
    cum = jnp.cumsum(log_f, axis=3)
    qs, ks, cs = [t.reshape(b_, h_, n, N_SUB, SUB, kd) for t in (q, k, cum)]
    blk_end = cs[..., -1, :]
    blk_start = jnp.concatenate([jnp.zeros_like(blk_end[:, :, :, :1]), blk_end[:, :, :, :-1]], axis=3)
    mid = cs[..., SUB // 2 - 1:SUB // 2, :]
    diag = jnp.einsum('bhnaik,bhnajk->bhnaij', qs * jnp.exp(cs - mid), ks * jnp.exp(mid - cs))
    diag = jnp.where(jnp.tril(jnp.ones((SUB, SUB), bool)), diag, 0.0)
    q_in = qs * jnp.exp(cs - blk_start[..., None, :])
    k_out = ks * jnp.exp(blk_end[..., None, :] - cs)
    cross_dec = jnp.exp(jnp.minimum(blk_start[:, :, :, :, None, :] - blk_end[:, :, :, None, :, :], 0.0))
    cross = jnp.einsum('bhnaik,bhnack,bhncjk->bhnaicj', q_in, cross_dec, k_out)
    blk_lower = jnp.arange(N_SUB)[:, None] > jnp.arange(N_SUB)[None, :]
    cross = jnp.where(blk_lower[:, None, :, None], cross, 0.0)
    scores = cross + jnp.einsum('bhnaij,ac->bhnaicj', diag, jnp.eye(N_SUB, dtype=diag.dtype))
    scores = scores.reshape(b_, h_, n, CHUNK, CHUNK)
    last = cum[:, :, :, -1:]
    chunk_kv = jnp.einsum('bhnjk,bhnjv->bhnkv', k * jnp.exp(last - cum), v)
    s_prev, s_final = _state_pass(chunk_kv, jnp.exp(last[:, :, :, 0]), s0)
    o = (jnp.einsum('bhnij,bhnjv->bhniv', scores, v)
         + jnp.einsum('bhnik,bhnkv->bhniv', q * jnp.exp(cum), s_prev))
    return o.reshape(b_, h_, l_, vd), s_final


def _retention_chunk_scan(log_gamma, q, k, v, s0):
    b_, h_, l_, kd = q.shape
    vd = v.shape[-1]
    n = l_ // CHUNK
    q, k = [t.reshape(b_, h_, n, CHUNK, kd) for t in (q, k)]
    v = v.reshape(b_, h_, n, CHUNK, vd)
    pos = jnp.arange(CHUNK, dtype=jnp.float32)
    diff = pos[:, None] - pos[None, :]
    decay = jnp.where(diff >= 0, jnp.exp(log_gamma[:, None, None] * jnp.maximum(diff, 0.0)), 0.0)
    scores = jnp.einsum('bhnik,bhnjk->bhnij', q, k) * decay[None, :, None]
    k_dec = jnp.exp(log_gamma[:, None] * (CHUNK - 1 - pos))[None, :, None, :, None]
    q_dec = jnp.exp(log_gamma[:, None] * (pos + 1))[None, :, None, :, None]
    chunk_kv = jnp.einsum('bhnjk,bhnjv->bhnkv', k * k_dec, v)
    chunk_decay = jnp.broadcast_to(jnp.exp(log_gamma * CHUNK)[None, :, None, None], (b_, h_, n, kd))
    s_prev, s_final = _state_pass(chunk_kv, chunk_decay, s0)
    o = (jnp.einsum('bhnij,bhnjv->bhniv', scores, v)
         + jnp.einsum('bhnik,bhnkv->bhniv', q * q_dec, s_prev))
    return o.reshape(b_, h_, l_, vd), s_final


def _bidirectional(scan_f, scan_b, args_f, args_b, s0):
    o_f, s_f = scan_f(*args_f, s0[0])
    o_b, s_b = scan_b(*[jnp.flip(a, axis=2) for a in args_b], s0[1])
    return o_f + jnp.flip(o_b, axis=2), (s_f, s_b)


def _hgrn2_forget(z, lb):
    log_f = jnp.logaddexp(z, jnp.log(jnp.maximum(lb, LB_FLOOR))) - jax.nn.softplus(z)
    key = (1.0 - lb) * jax.nn.sigmoid(-z)
    return log_f, key


def _even_mixer(h, w_in, w_out, lb, ha_norm_w, rb_log_gamma, rope, states):
    p = jnp.einsum('bld,de->ble', h, w_in).astype(jnp.float32)
    qa, fa_f, fa_b, ia, ga, qb, kb, vb, gb = _split(p, EVEN_SIZES)
    qa = _heads(qa, HA_HEADS) * HA_DK ** -0.5
    ia = _heads(ia, HA_HEADS)
    lb = lb.astype(jnp.float32).reshape(2, HA_HEADS, 1, HA_DK)
    lf_f, ka_f = _hgrn2_forget(_heads(fa_f, HA_HEADS), lb[0])
    lf_b, ka_b = _hgrn2_forget(_heads(fa_b, HA_HEADS), lb[1])
    oa, st_a = _bidirectional(_gated_chunk_scan, _gated_chunk_scan,
                              (qa, ka_f, ia, lf_f), (qa, ka_b, ia, lf_b), states[0])
    ya = _merge(_rms_norm(oa, ha_norm_w)) * jax.nn.silu(ga)
    qb = _heads(qb, RB_HEADS) * RB_DK ** -0.5
    kb = _heads(kb, RB_HEADS)
    if rope is not None:
        qb = _apply_rope(qb, *rope)
        kb = _apply_rope(kb, *rope)
    vb = _heads(vb, RB_HEADS)
    ob, st_b = _bidirectional(functools.partial(_retention_chunk_scan, rb_log_gamma[0]),
                              functools.partial(_retention_chunk_scan, rb_log_gamma[1]),
                              (qb, kb, vb), (qb, kb, vb), states[1])
    yb = _merge(_rms_norm(ob)) * jax.nn.silu(gb)
    y = jnp.concatenate([ya, yb], axis=-1).astype(h.dtype)
    return jnp.einsum('ble,ed->bld', y, w_out), (st_a, st_b)


def _odd_mixer(h, w_in, w_out, w2, b2, norm_w, states):
    p = jnp.einsum('bld,de->ble', h, w_in).astype(jnp.float32)
    qc, kc, vc, gc, lr = _split(p, ODD_SIZES)
    q = _heads(qc, GC_HEADS) * GC_DK ** -0.5
    k = _heads(kc, GC_HEADS)
    v = _heads(vc, GC_HEADS)
    lr_f, lr_b = jnp.split(lr, 2, axis=-1)
    w2 = w2.astype(jnp.float32)
    b2 = b2.astype(jnp.float32)
    lf_f = _heads(jax.nn.log_sigmoid(lr_f @ w2[0] + b2[0]) / GC_TAU, GC_HEADS)
    lf_b = _heads(jax.nn.log_sigmoid(lr_b @ w2[1] + b2[1]) / GC_TAU, GC_HEADS)
    o, st = _bidirectional(_gated_chunk_scan, _gated_chunk_scan,
                           (q, k, v, lf_f), (q, k, v, lf_b), states)
    y = (_merge(_rms_norm(o, norm_w)) * jax.nn.silu(gc)).astype(h.dtype)
    return jnp.einsum('ble,ed->bld', y, w_out), st


def _ec_ffn(h, w_router, w_gate, w_up, w_down):
    b, n, d = h.shape
    cap = EC_CAPACITY_FACTOR * n // N_EXPERTS
    aff = jax.nn.softmax(jnp.einsum('bnd,de->bne', h, w_router).astype(jnp.float32), axis=-1)
    gate, idx = lax.top_k(jnp.swapaxes(aff, 1, 2), cap)
    xs = jax.vmap(lambda hb, ib: hb[ib])(h, idx)
    hid = jax.nn.silu(jnp.einsum('becd,edf->becf', xs, w_gate)) * jnp.einsum('becd,edf->becf', xs, w_up)
    ys = jnp.einsum('becf,efd->becd', hid, w_down) * gate[..., None].astype(h.dtype)
    return jax.vmap(lambda yb, ib: jnp.zeros((n, d), h.dtype).at[ib.reshape(-1)].add(yb.reshape(-1, d)))(ys, idx)


def setup_inputs(seed: int = 0) -> dict:
    key = jax.random.key(seed)
    ks = iter(jax.random.split(key, 32))

    def nrm(shape, scale):
        return jax.random.normal(next(ks), shape, jnp.float32) * scale

    d = D_MODEL
    gamma0 = 1.0 - 2.0 ** (-5.0 - jnp.arange(RB_HEADS, dtype=jnp.float32))
    return {
        'x': nrm((BATCH, SEQ, d), 1.0),
        'c': nrm((BATCH, d), 1.0),
        'ctx': nrm((BATCH, CTX_LEN, d), 1.0),
        'c_ctx': nrm((d,), 1.0),
        'ada_w': nrm((DEPTH, d, 6 * d), d ** -0.5),
        'ada_b': nrm((DEPTH, 6 * d), 0.02),
        'ln_w': 1.0 + nrm((DEPTH, 2, d), 0.02),
        'ln_b': nrm((DEPTH, 2, d), 0.02),
        'even_w_in': nrm((N_EVEN, d, EVEN_IN), d ** -0.5),
        'even_w_out': nrm((N_EVEN, EVEN_OUT, d), EVEN_OUT ** -0.5 * DN_BETA),
        'ha_lb': nrm((N_EVEN, 2, HA_KEY), 0.1),
        'ha_norm': 1.0 + nrm((N_EVEN, HA_DV), 0.02),
        'rb_decay': (jnp.log(gamma0) - jnp.log1p(-gamma0)) + nrm((N_EVEN, 2, RB_HEADS), 0.01),
        'odd_w_in': nrm((N_ODD, d, ODD_IN), d ** -0.5),
        'odd_w_out': nrm((N_ODD, ODD_OUT, d), ODD_OUT ** -0.5 * DN_BETA),
        'gc_w2': nrm((N_ODD, 2, GC_RANK, GC_KEY), GC_RANK ** -0.5),
        'gc_b2': nrm((N_ODD, 2, GC_KEY), 0.1),
        'gc_norm': 1.0 + nrm((N_ODD, GC_DV), 0.02),
        'router_w': nrm((DEPTH, d, N_EXPERTS), d ** -0.5),
        'exp_w_gate': nrm((DEPTH, N_EXPERTS, d, EXPERT_FF), d ** -0.5),
        'exp_w_up': nrm((DEPTH, N_EXPERTS, d, EXPERT_FF), d ** -0.5),
        'exp_w_down': nrm((DEPTH, N_EXPERTS, EXPERT_FF, d), EXPERT_FF ** -0.5 * DN_BETA),
    }


def reference(x, c, ctx, c_ctx, ada_w, ada_b, ln_w, ln_b, even_w_in, even_w_out, ha_lb, ha_norm,
              rb_decay, odd_w_in, odd_w_out, gc_w2, gc_b2, gc_norm, router_w, exp_w_gate, exp_w_up,
              exp_w_down):
    n_lat = x.shape[1]
    rows = n_lat // GRID_W
    rope = _axial_rope(rows)
    b_ctx = ctx.shape[0]
    lb_p = jax.nn.softmax(ha_lb.astype(jnp.float32), axis=0)
    lb_all = jnp.cumsum(lb_p, axis=0) - lb_p[0]
    cond_lat = jax.nn.silu(c)
    cond_ctx = jax.nn.silu(c_ctx)

    for l in range(DEPTH):
        last = l == DEPTH - 1
        mod_lat = (cond_lat @ ada_w[l] + ada_b[l])[:, None, :]
        mod_ctx = (cond_ctx @ ada_w[l] + ada_b[l])[None, None, :]
        sh1, sc1, g1, sh2, sc2, g2 = jnp.split(mod_lat, 6, axis=-1)
        csh1, csc1, cg1, csh2, csc2, cg2 = jnp.split(mod_ctx, 6, axis=-1)
        h_lat = x * (1.0 + sc1) + sh1
        h_ctx = ctx * (1.0 + csc1) + csh1

        if l % 2 == 0:
            j = l // 2
            z_a = jnp.zeros((b_ctx, HA_HEADS, HA_DK, HA_DV), jnp.float32)
            z_b = jnp.zeros((b_ctx, RB_HEADS, RB_DK, RB_DV), jnp.float32)
            log_gamma = jax.nn.log_sigmoid(rb_decay[j].astype(jnp.float32))
            y_ctx, st = _even_mixer(h_ctx, even_w_in[j], even_w_out[j], lb_all[j], ha_norm[j],
                                    log_gamma, None, ((z_a, z_a), (z_b, z_b)))
            y_lat, _ = _even_mixer(h_lat, even_w_in[j], even_w_out[j], lb_all[j], ha_norm[j],
                                   log_gamma, rope, st)
        else:
            j = l // 2
            z_c = jnp.zeros((b_ctx, GC_HEADS, GC_DK, GC_DV), jnp.float32)
            y_ctx, st = _odd_mixer(h_ctx, odd_w_in[j], odd_w_out[j], gc_w2[j], gc_b2[j], gc_norm[j],
                                   (z_c, z_c))
            y_lat, _ = _odd_mixer(h_lat, odd_w_in[j], odd_w_out[j], gc_w2[j], gc_b2[j], gc_norm[j], st)

        ffn = (router_w[l], exp_w_gate[l], exp_w_up[l], exp_w_down[l])
        x = _layer_norm(DN_ALPHA * x + g1 * y_lat, ln_w[l, 0], ln_b[l, 0])
        x = _layer_norm(DN_ALPHA * x + g2 * _ec_ffn(x * (1.0 + sc2) + sh2, *ffn), ln_w[l, 1], ln_b[l, 1])
        if not last:
            ctx = _layer_norm(DN_ALPHA * ctx + cg1 * y_ctx, ln_w[l, 0], ln_b[l, 0])
            ctx = _layer_norm(DN_ALPHA * ctx + cg2 * _ec_ffn(ctx * (1.0 + csc2) + csh2, *ffn),
                              ln_w[l, 1], ln_b[l, 1])
    return x
```

```python
import numpy as np
from contextlib import ExitStack
import concourse.bass as bass
import concourse.mybir as mybir
from concourse.bass_utils import run_bass_kernel_spmd

F32 = mybir.dt.float32
BF16 = mybir.dt.bfloat16
AF = mybir.ActivationFunctionType
ALU = mybir.AluOpType

D = 1024
NCH = 8
E = 16
SUB = 32
NSB = 4
LN_EPS = 1e-5
RMS_EPS = 1e-6
DN_ALPHA = 8.0 ** 0.25
GC_TAU = 16.0
CLAMP = 35.0
EVEN_IN = 4608
ODD_IN = 3104
SAME_ENGINE_SYNC = True


class Cfg:
    def __init__(self, B=4, LC=256, LL=2048, F=2816, DEPTH=4, debug=False, stage=99):
        self.B, self.LC, self.LL, self.F, self.DEPTH, self.debug = B, LC, LL, F, DEPTH, debug
        self.stage = stage
        self.sub = 99
        self.NTC, self.NTL = LC // 128, LL // 128
        self.NT = self.NTC + self.NTL
        self.NTOK = LC + LL
        self.capL, self.capC = LL // 8, LC // 8
        self.NS = self.capL + self.capC
        self.NF = F // 128


class Stop(Exception):
    pass


class T:
    __slots__ = ("h", "lw", "rd", "name", "base", "trk")

    def __init__(self, h, name="", base=0, trk=None):
        self.h, self.lw, self.rd, self.name, self.base = h, None, [], name, base
        self.trk = trk if trk is not None else self

    def __getitem__(self, idx):
        return self.h[idx]

    def c(self, lo, hi, p0=0, p1=128):
        return self.h[p0:p1, self.base + lo:self.base + hi]


class Em:
    def __init__(self, nc, stack):
        self.nc, self.stack = nc, stack
        self.eng = {"pe": nc.tensor, "act": nc.scalar, "dve": nc.vector, "pool": nc.gpsimd, "sp": nc.sync}
        self.sem, self.cnt = {}, {}
        for e in self.eng:
            self.sem[e] = stack.enter_context(nc.semaphore("s_" + e))
            self.cnt[e] = 0
        self.KD = 16
        self.dnext = {}
        for q in ("sp", "pool"):
            self.dnext[q] = 0
            for i in range(self.KD):
                key = "d_%s_%d" % (q, i)
                self.sem[key] = stack.enter_context(nc.semaphore("sd_%s_%d" % (q, i)))
                self.cnt[key] = 0
        self.seen = {e: {} for e in self.eng}
        self.n = 0
        self.uid = 0
        self.muted = False

    def sb(self, shape, dt, name="t", stack=None):
        self.uid += 1
        nm = "%s_%d" % (name, self.uid)
        h = (stack or self.stack).enter_context(self.nc.sbuf_tensor(nm, list(shape), dt))
        return T(h, nm)

    def psum_banks(self):
        banks = []
        for i in range(8):
            banks.append(self.stack.enter_context(self.nc.psum_tensor("psb%d" % i, [128, 512], F32)))
        return banks

    def dram(self, shape, dt, name="d"):
        self.uid += 1
        nm = "%s_%d" % (name, self.uid)
        return T(self.nc.dram_tensor(nm, list(shape), dt), nm)

    def _deps(self, e, reads, writes):
        need = {}
        seen = self.seen[e]

        def add(dep):
            s, v = dep
            if s == e and not SAME_ENGINE_SYNC:
                return
            if seen.get(s, 0) >= v:
                return
            if need.get(s, 0) < v:
                need[s] = v
        for t in reads:
            t = t.trk
            if t.lw is not None:
                add(t.lw)
            if t.name.startswith("bk"):
                for r in t.rd:
                    if r[0] != e:
                        add(r)
        for t in writes:
            t = t.trk
            if t.lw is not None:
                add(t.lw)
            for r in t.rd:
                add(r)
        for s, v in need.items():
            self.eng[e].wait_ge(self.sem[s], v)
            seen[s] = v
            self.n += 1

    def _rec(self, key, reads, writes):
        v = self.cnt[key]
        for t in reads:
            t.trk.rd.append((key, v))
        for t in writes:
            t.trk.lw = (key, v)
            t.trk.rd = []

    def op(self, e, fn, reads=(), writes=()):
        if self.muted:
            return None
        self._deps(e, reads, writes)
        ins = fn(self.eng[e])
        self.cnt[e] += 1
        ins.then_inc(self.sem[e], 1)
        self._rec(e, reads, writes)
        self.n += 1
        return ins

    def dma(self, q, out, in_, reads=(), writes=(), **kw):
        if self.muted:
            return None
        self._deps(q, reads, writes)
        key = "d_%s_%d" % (q, self.dnext[q] % self.KD)
        self.dnext[q] += 1
        if self.cnt[key] > 0 and self.seen[q].get(key, 0) < self.cnt[key]:
            self.eng[q].wait_ge(self.sem[key], self.cnt[key])
            self.seen[q][key] = self.cnt[key]
            self.n += 1
        ins = self.eng[q].dma_start(out=out, in_=in_, **kw)
        self.cnt[key] += 16
        ins.then_inc(self.sem[key], 16)
        self._rec(key, reads, writes)
        self.n += 1
        return ins

    def barrier(self):
        for e in self.eng:
            for s, v in self.cnt.items():
                if v > 0 and self.seen[e].get(s, 0) < v and s != e:
                    self.eng[e].wait_ge(self.sem[s], v)
                    self.seen[e][s] = v
                    self.n += 1


def make_consts(cfg):
    j = np.arange(128)
    same = (j[:, None] // SUB) == (j[None, :] // SUB)
    loc = j % SUB
    le = j[:, None] <= j[None, :]
    ge = j[:, None] >= j[None, :]
    mfm = np.zeros((2, 128, 264), np.float32)
    m3 = np.zeros((2, 128, 128), np.float32)
    mask = np.zeros((2, 128, 128), np.float32)
    bm = (j[:, None] // SUB == np.arange(NSB)[None, :]).astype(np.float32)
    mfm[0, :, 0:128] = same * (le.astype(np.float32) - (loc[:, None] <= SUB // 2 - 1))
    mfm[0, :, 128:256] = same * le
    mfm[0, :, 256:260] = bm
    m3[0] = same * (j[:, None] > j[None, :])
    mask[0] = same * le
    mfm[1, :, 0:128] = same * (ge.astype(np.float32) - (loc[:, None] >= SUB // 2))
    mfm[1, :, 128:256] = same * ge
    mfm[1, :, 256:260] = bm
    m3[1] = same * (j[:, None] < j[None, :])
    mask[1] = same * ge
    rot = np.zeros((128, 128), np.float32)
    for k in range(64):
        rot[k + 64, k] = -1.0
        rot[k, k + 64] = 1.0
    rows = cfg.LL // 64
    r_idx, c_idx = np.meshgrid(np.arange(rows), np.arange(64), indexing="ij")
    nfreq = 32
    freq = (10000.0 ** (-np.arange(nfreq, dtype=np.float32) / nfreq)).astype(np.float32)
    ang = np.concatenate([r_idx.reshape(-1, 1).astype(np.float32) * freq,
                          c_idx.reshape(-1, 1).astype(np.float32) * freq], axis=-1)
    cosT = np.concatenate([np.cos(ang).T, np.cos(ang).T], axis=0).astype(np.float32)
    sinT = np.concatenate([np.sin(ang).T, np.sin(ang).T], axis=0).astype(np.float32)
    misc = np.zeros((128, 8), np.float32)
    misc[:, 0] = LN_EPS
    misc[:, 1] = RMS_EPS
    misc[:, 2] = 1.0
    for a in range(4):
        misc[:, 4 + a] = j + 128 * a
    return {
        "c_ident": np.eye(128, dtype=np.float32),
        "c_iota": np.tile(np.arange(512, dtype=np.float32)[None, :], (128, 1)),
        "c_mfm": mfm, "c_m3": m3, "c_mask": mask, "c_bm": bm,
        "c_tri": (j[:, None] < j[None, :]).astype(np.float32),
        "c_ones": np.ones((128, 128), np.float32),
        "c_rot": rot, "c_cos": np.ascontiguousarray(cosT), "c_sin": np.ascontiguousarray(sinT),
        "c_misc": misc,
    }


INPUT_SHAPES = lambda cfg: {
    "x": [cfg.B, cfg.LL, D], "c": [cfg.B, D], "ctx": [cfg.B, cfg.LC, D], "c_ctx": [1, D],
    "ada_w": [4, D, 6 * D], "ada_b": [4, 6 * D], "ln_w": [4, 2, D], "ln_b": [4, 2, D],
    "even_w_in": [2, D, EVEN_IN], "even_w_out": [2, 1024, D], "ha_lb": [2, 2, 512], "ha_norm": [2, 128],
    "rb_decay": [2, 8], "odd_w_in": [2, D, ODD_IN], "odd_w_out": [2, 1024, D],
    "gc_w2": [2, 2, 16, 512], "gc_b2": [2, 2, 512], "gc_norm": [2, 256], "router_w": [4, D, E],
    "exp_w_gate": [4, E, D, cfg.F], "exp_w_up": [4, E, D, cfg.F], "exp_w_down": [4, E, cfg.F, D],
}


def build(cfg):
    nc = bass.Bass("TRN2", target_bir_lowering=False)
    I = {}
    for k, shp in INPUT_SHAPES(cfg).items():
        I[k] = nc.dram_tensor(k, shp, F32, kind="ExternalInput")
    consts = make_consts(cfg)
    for k, v in consts.items():
        I[k] = nc.dram_tensor(k, list(v.shape), F32, kind="ExternalInput")
    out = nc.dram_tensor("out", [cfg.B, cfg.LL, D], F32, kind="ExternalOutput")
    dbg = {}
    if cfg.debug:
        dbg["mod"] = nc.dram_tensor("dbg_mod", [cfg.DEPTH, cfg.B + 1, 6 * D], F32, kind="ExternalOutput")
        dbg["y"] = nc.dram_tensor("dbg_y", [cfg.NTOK, D], F32, kind="ExternalOutput")
        dbg["xmid"] = nc.dram_tensor("dbg_xmid", [cfg.NTOK, D], F32, kind="ExternalOutput")
        dbg["ffn"] = nc.dram_tensor("dbg_ffn", [cfg.NTOK, D], F32, kind="ExternalOutput")
        dbg["o"] = nc.dram_tensor("dbg_o", [8, 128, cfg.NTOK], F32, kind="ExternalOutput")
    B, NT, NTC, NTOK, LC, LL = cfg.B, cfg.NT, cfg.NTC, cfg.NTOK, cfg.LC, cfg.LL

    with ExitStack() as st:
        em = Em(nc, st)
        banks = em.psum_banks()
        BK = [T(banks[i], "bk%d" % i) for i in range(8)]
        PF = [T(banks[i], "pf%d" % i, trk=BK[i]) for i in range(4)]
        PQ = [T(banks[4 + i % 4], "pq%d" % i, base=(i // 4) * 128, trk=BK[4 + i % 4]) for i in range(16)]
        rr = {"f": 0, "q": 0}

        def pfull():
            rr["f"] = (rr["f"] + 1) % 4
            return PF[rr["f"]]

        def pq():
            rr["q"] = (rr["q"] + 1) % 16
            return PQ[rr["q"]]

        XS = em.dram([B, NTOK, D], F32, "XS")
        MOD = em.dram([cfg.DEPTH, B + 1, 6 * D], F32, "MOD")
        YT = em.dram([8, 128, NTOK], BF16, "YT")
        XB = em.dram([NT, 128, D], BF16, "XB")
        RK = em.dram([E, NTOK], F32, "RK")
        OUT = T(out, "out")
        DBG = {k: T(v, "dbg_" + k) for k, v in dbg.items()}

        def cload(name, shape, dt=F32, src=None, q="sp"):
            t = em.sb(shape, dt, name)
            srcap = src if src is not None else I[name].ap()
            em.dma("pool" if dt == BF16 else q, t.h.ap(), srcap, writes=[t])
            return t
        ident = cload("c_ident", [128, 128])
        identb = cload("c_ident", [128, 128], BF16)
        iota = cload("c_iota", [128, 512])
        mfm = [cload("c_mfm", [128, 264], src=I["c_mfm"][d]) for d in range(2)]
        m3 = [cload("c_m3", [128, 128], src=I["c_m3"][d]) for d in range(2)]
        amask = [cload("c_mask", [128, 128], src=I["c_mask"][d]) for d in range(2)]
        bmask = cload("c_bm", [128, 4])
        tri = cload("c_tri", [128, 128])
        ones = cload("c_ones", [128, 128])
        rotb = cload("c_rot", [128, 128], BF16)
        misc = cload("c_misc", [128, 8])
        EPS_LN, EPS_RMS, ONE = misc[:, 0:1], misc[:, 1:2], misc[:, 2:3]

        with ExitStack() as ph:
            cT = em.sb([128, NCH, B + 1], F32, "cT", ph)
            for r in range(B):
                em.dma("sp", cT[:, :, r], I["c"][r].rearrange("(c p) -> p c", p=128), writes=[cT],
                       allow_slow_non_contiguous=True)
            em.dma("sp", cT[:, :, B], I["c_ctx"][0].rearrange("(c p) -> p c", p=128), writes=[cT],
                   allow_slow_non_contiguous=True)
            em.op("act", lambda e: e.activation(out=cT[:, :, :], in_=cT[:, :, :], func=AF.Silu), reads=[cT], writes=[cT])
            wbuf = [em.sb([128, NCH, 512], F32, "adaw", ph) for _ in range(2)]
            bb = [em.sb([B + 1, 512], F32, "adab", ph) for _ in range(2)]
            mo = [em.sb([B + 1, 512], F32, "modo", ph) for _ in range(2)]
            it = 0
            for l in range(cfg.DEPTH):
                for cp in range(12):
                    w = wbuf[it % 2]; bt = bb[it % 2]; m = mo[it % 2]
                    em.dma("sp", w[:, :, :], I["ada_w"][l, :, cp * 512:(cp + 1) * 512].rearrange("(c p) n -> p c n", p=128), writes=[w])
                    em.dma("sp", bt[:, :], I["ada_b"][l:l + 1, cp * 512:(cp + 1) * 512].to_broadcast([B + 1, 512]), writes=[bt])
                    p = pfull()
                    for ch in range(NCH):
                        em.op("pe", lambda e: e.matmul(p.c(0, 512, 0, B + 1), lhsT=cT[:, ch, :], rhs=w[:, ch, :], start=(ch == 0), stop=(ch == NCH - 1)),
                              reads=[cT, w], writes=[p])
                    em.op("dve", lambda e: e.tensor_tensor(out=m[:, :], in0=p.c(0, 512, 0, B + 1), in1=bt[:, :], op=ALU.add), reads=[p, bt], writes=[m])
                    em.dma("sp", MOD[l, :, cp * 512:(cp + 1) * 512], m[:, :], reads=[m], writes=[MOD])
                    it += 1
            if cfg.debug:
                em.dma("sp", DBG["mod"].h.ap(), MOD.h.ap(), reads=[MOD], writes=[DBG["mod"]])
            for s in range(B):
                em.dma("sp", XS[s, 0:LC, :], I["ctx"][s], writes=[XS])
                em.dma("sp", XS[s, LC:NTOK, :], I["x"][s], writes=[XS])
        em.barrier()

        def layernorm_tile(ph_tmp, u, lnw_bc, lnb_bc, dst):
            st2, junk = ph_tmp["st2"], ph_tmp["junk"]
            em.op("dve", lambda e: e.memset(st2[:, :], 0.0), writes=[st2])
            em.op("act", lambda e: e.activation(out=junk[:, :], in_=u[:, :], func=AF.Identity, accum_out=st2[:, 0:1]), reads=[u], writes=[junk, st2])
            em.op("act", lambda e: e.activation(out=junk[:, :], in_=u[:, :], func=AF.Square, accum_out=st2[:, 1:2]), reads=[u], writes=[junk, st2])
            em.op("dve", lambda e: e.tensor_scalar(out=st2[:, 2:3], in0=st2[:, 0:1], scalar1=1.0 / D, scalar2=None, op0=ALU.mult), reads=[st2], writes=[st2])
            em.op("dve", lambda e: e.tensor_tensor(out=st2[:, 3:4], in0=st2[:, 2:3], in1=st2[:, 2:3], op=ALU.mult), reads=[st2], writes=[st2])
            em.op("dve", lambda e: e.scalar_tensor_tensor(out=st2[:, 4:5], in0=st2[:, 1:2], scalar=1.0 / D, in1=st2[:, 3:4], op0=ALU.mult, op1=ALU.subtract), reads=[st2], writes=[st2])
            em.op("act", lambda e: e.activation(out=st2[:, 5:6], in_=st2[:, 4:5], func=AF.Sqrt, bias=EPS_LN, scale=1.0), reads=[st2, misc], writes=[st2])
            em.op("dve", lambda e: e.reciprocal(out=st2[:, 6:7], in_=st2[:, 5:6]), reads=[st2], writes=[st2])
            em.op("dve", lambda e: e.tensor_scalar(out=u[:, :], in0=u[:, :], scalar1=st2[:, 2:3], scalar2=st2[:, 6:7], op0=ALU.subtract, op1=ALU.mult), reads=[u, st2], writes=[u])
            em.op("dve", lambda e: e.tensor_tensor(out=u[:, :], in0=u[:, :], in1=lnw_bc[:, :], op=ALU.mult), reads=[u, lnw_bc], writes=[u])
            em.op("dve", lambda e: e.tensor_tensor(out=dst[:, :], in0=u[:, :], in1=lnb_bc[:, :], op=ALU.add), reads=[u, lnb_bc], writes=[dst])

        def load_modfm(ph, l, s):
            mf = em.sb([128, 2, 6, NCH], F32, "modfm", ph)
            for i, row in enumerate((s, B)):
                for six in range(6):
                    em.dma("sp", mf[:, i, six, :], MOD[l, row, six * D:(six + 1) * D].rearrange("(c p) -> p c", p=128), reads=[MOD], writes=[mf],
                           allow_slow_non_contiguous=True)
            for six in (1, 4):
                em.op("dve", lambda e: e.tensor_scalar(out=mf[:, :, six, :], in0=mf[:, :, six, :], scalar1=1.0, scalar2=None, op0=ALU.add), reads=[mf], writes=[mf])
            return mf

        def bc_load(t, src_row_ap, srcT):
            em.dma("sp", t[:, :], src_row_ap.to_broadcast([128, D]), reads=[srcT], writes=[t])

        def mixer_phase(s, l):
            even = (l % 2 == 0)
            jl = l // 2
            w_in = I["even_w_in"] if even else I["odd_w_in"]
            w_out = I["even_w_out"] if even else I["odd_w_out"]
            def ck(k_):
                if cfg.sub == k_:
                    em.muted = True
            with ExitStack() as ph:
                mixer_body(ph, s, l, even, jl, w_in, w_out, ck)
            if em.muted:
                em.muted = False
                em.barrier()
                return
            em.barrier()
            mixer_out(s, l, jl, w_out)

        def mixer_body(ph, s, l, even, jl, w_in, w_out, ck):
            if True:
                mf = load_modfm(ph, l, s)
                ck(0)
                hT = em.sb([128, NCH, NTOK], BF16, "hT", ph)
                xt = [em.sb([128, D], F32, "xt", ph) for _ in range(2)]
                for n in range(NT):
                    x_t = xt[n % 2]
                    em.dma("sp", x_t[:, :], XS[s, n * 128:(n + 1) * 128, :], reads=[XS], writes=[x_t])
                    mi = 1 if n < NTC else 0
                    import os
                    dbgm = os.environ.get("KSUB2", "c")
                    for ch in range(NCH):
                        if dbgm == "a":
                            continue
                        p = pq()
                        em.op("pe", lambda e: e.matmul(p.c(0, 128), lhsT=x_t[:, ch * 128:(ch + 1) * 128], rhs=ident[:, :], start=True, stop=True), reads=[x_t, ident], writes=[p])
                        if dbgm == "b":
                            continue
                        if dbgm == "d":
                            em.op("act", lambda e: e.activation(out=hT[:, ch, n * 128:(n + 1) * 128], in_=p.c(0, 128), func=AF.Identity), reads=[p, mf], writes=[hT])
                            continue
                        em.op("act", lambda e: e.activation(out=hT[:, ch, n * 128:(n + 1) * 128], in_=p.c(0, 128), func=AF.Identity,
                                                            scale=mf[:, mi, 1, ch:ch + 1], bias=mf[:, mi, 0, ch:ch + 1]), reads=[p, mf], writes=[hT])
                ck(1)
                wts = [em.sb([128, NCH, 256], BF16, "wcol", ph) for _ in range(3)]
                wrr = [0]

                def load_wcols(c0, ncol):
                    wrr[0] = (wrr[0] + 1) % 3
                    w = wts[wrr[0]]
                    em.dma("pool", w[:, :, 0:ncol], w_in[jl, :, c0:c0 + ncol].rearrange("(c p) n -> p c n", p=128), writes=[w])
                    return w

                def proj_fm(c0, M, evac):
                    w = load_wcols(c0, M)
                    for t0 in range(0, NTOK, 512):
                        nt = min(512, NTOK - t0)
                        p = pfull()
                        for ch in range(NCH):
                            em.op("pe", lambda e: e.matmul(p.c(0, nt, 0, M), lhsT=w[:, ch, 0:M], rhs=hT[:, ch, t0:t0 + nt], start=(ch == 0), stop=(ch == NCH - 1)),
                                  reads=[w, hT], writes=[p])
                        evac(p, t0, nt)

                def proj_tm(c0, ncol, dst):
                    w = load_wcols(c0, ncol)
                    for n in range(NT):
                        p = pfull()
                        for ch in range(NCH):
                            em.op("pe", lambda e: e.matmul(p.c(0, ncol), lhsT=hT[:, ch, n * 128:(n + 1) * 128], rhs=w[:, ch, 0:ncol], start=(ch == 0), stop=(ch == NCH - 1)),
                                  reads=[w, hT], writes=[p])
                        em.op("act", lambda e: e.activation(out=dst[:, n, 0:ncol], in_=p.c(0, ncol), func=AF.Copy), reads=[p], writes=[dst])

                prm = em.sb([128, 40], F32, "prm", ph)
                if even:
                    cosT = em.sb([128, LL], F32, "cosT", ph)
                    sinT = em.sb([128, LL], F32, "sinT", ph)
                    em.dma("sp", cosT.h.ap(), I["c_cos"].ap(), writes=[cosT])
                    em.dma("sp", sinT.h.ap(), I["c_sin"].ap(), writes=[sinT])
                    lbc, oml = prm[:, 0:8], prm[:, 8:16]
                    if jl == 0:
                        em.op("dve", lambda e: e.memset(prm[:, 0:8], 0.0), writes=[prm])
                    else:
                        em.dma("sp", prm[:, 0:8], I["ha_lb"][1].rearrange("d (h k) -> k (d h)", k=128), writes=[prm], allow_slow_non_contiguous=True)
                        em.dma("sp", prm[:, 8:16], I["ha_lb"][0].rearrange("d (h k) -> k (d h)", k=128), writes=[prm], allow_slow_non_contiguous=True)
                        em.op("dve", lambda e: e.tensor_tensor(out=prm[:, 0:8], in0=prm[:, 0:8], in1=prm[:, 8:16], op=ALU.subtract), reads=[prm], writes=[prm])
                        em.op("act", lambda e: e.activation(out=prm[:, 0:8], in_=prm[:, 0:8], func=AF.Sigmoid), reads=[prm], writes=[prm])
                    em.op("dve", lambda e: e.tensor_scalar(out=prm[:, 8:16], in0=prm[:, 0:8], scalar1=-1.0, scalar2=1.0, op0=ALU.mult, op1=ALU.add), reads=[prm], writes=[prm])
                    em.op("dve", lambda e: e.tensor_scalar(out=prm[:, 0:8], in0=prm[:, 0:8], scalar1=1e-30, scalar2=None, op0=ALU.max), reads=[prm], writes=[prm])
                    em.dma("sp", prm[:, 16:24], I["rb_decay"][jl:jl + 1, :].to_broadcast([128, 8]), writes=[prm])
                    em.op("act", lambda e: e.activation(out=prm[:, 16:24], in_=prm[:, 16:24], func=AF.Exp, scale=-1.0), reads=[prm], writes=[prm])
                    em.op("act", lambda e: e.activation(out=prm[:, 16:24], in_=prm[:, 16:24], func=AF.Ln, bias=ONE, scale=1.0), reads=[prm, misc], writes=[prm])
                    em.op("dve", lambda e: e.tensor_scalar(out=prm[:, 16:24], in0=prm[:, 16:24], scalar1=-1.0, scalar2=None, op0=ALU.mult), reads=[prm], writes=[prm])
                    em.dma("sp", prm[:, 24:25], I["ha_norm"][jl].rearrange("(k o) -> k o", o=1), writes=[prm], allow_slow_non_contiguous=True)
                    em.op("dve", lambda e: e.memset(prm[:, 25:26], 1.0), writes=[prm])
                else:
                    em.dma("sp", prm[:, 0:8], I["gc_b2"][jl].rearrange("d (h k) -> k (d h)", k=128), writes=[prm], allow_slow_non_contiguous=True)
                    em.op("dve", lambda e: e.tensor_scalar(out=prm[:, 0:8], in0=prm[:, 0:8], scalar1=-1.0, scalar2=None, op0=ALU.mult), reads=[prm], writes=[prm])
                    em.dma("sp", prm[:, 24:26], I["gc_norm"][jl].rearrange("(c k) -> k c", k=128), writes=[prm], allow_slow_non_contiguous=True)
                    w2 = em.sb([16, 2, 512], F32, "w2", ph)
                    em.dma("sp", w2[:, :, :], I["gc_w2"][jl].rearrange("d r k -> r d k"), writes=[w2])

                ck(2)
                nvc = 1 if even else 2
                dv = 128 * nvc
                qT = em.sb([128, NTOK], BF16, "qT", ph)
                kT = em.sb([128, NTOK], BF16, "kT", ph)
                gT = em.sb([128, NTOK], F32, "gT", ph)
                vtm = em.sb([128, NT, dv], BF16, "vtm", ph)
                sgT = em.sb([128, nvc, NTOK], BF16, "sgT", ph)
                obuf = em.sb([128, nvc, NTOK], F32, "obuf", ph)
                S = em.sb([128, dv], F32, "S", ph)
                Sb = em.sb([128, dv], BF16, "Sb", ph)
                tmpE = [em.sb([128, 512], F32, "tmpE", ph) for _ in range(3)]
                lrT = None if even else em.sb([16, NTOK], F32, "lrT", ph)
                def dbl(shape, dt, name):
                    return [em.sb(shape, dt, name, ph) for _ in range(2)]
                gtm = dbl([128, 128], F32, "gtm")
                X1 = dbl([128, 128], F32, "X1"); X1i = dbl([128, 128], F32, "X1i"); X2 = dbl([128, 128], F32, "X2")
                X3 = dbl([128, 128], F32, "X3"); X4 = dbl([128, 4], F32, "X4"); Ecl = dbl([128, 128], F32, "Ecl")
                Qt = dbl([128, 128], BF16, "Qt"); Kt = dbl([128, 128], BF16, "Kt"); Qs = dbl([128, 128], BF16, "Qs")
                Kom = dbl([128, NSB, 128], BF16, "Kom"); At = dbl([128, 128], BF16, "At")
                ofin = dbl([128, nvc, 128], F32, "ofin"); sq = dbl([128, nvc, 128], F32, "sq")
                rstd = dbl([128, 128], F32, "rstd"); ytile = dbl([128, nvc, 128], BF16, "ytile")
                XR = {}

                def exp_tiles(gsrc_tm, d, bi):
                    pf = pfull()
                    em.op("pe", lambda e: e.matmul(pf.c(0, 264), lhsT=gsrc_tm[:, :], rhs=mfm[d][:, :], start=True, stop=True), reads=[gsrc_tm, mfm[d]], writes=[pf])
                    p3 = pq()
                    em.op("pe", lambda e: e.matmul(p3.c(0, 128), lhsT=m3[d][:, :], rhs=gsrc_tm[:, :], start=True, stop=True), reads=[gsrc_tm, m3[d]], writes=[p3])
                    em.op("dve", lambda e: e.tensor_scalar(out=Ecl[bi][:, :], in0=pf.c(0, 128), scalar1=-CLAMP, scalar2=CLAMP, op0=ALU.max, op1=ALU.min), reads=[pf], writes=[Ecl[bi]])
                    em.op("act", lambda e: e.activation(out=X1[bi][:, :], in_=Ecl[bi][:, :], func=AF.Exp), reads=[Ecl[bi]], writes=[X1[bi]])
                    em.op("act", lambda e: e.activation(out=X1i[bi][:, :], in_=Ecl[bi][:, :], func=AF.Exp, scale=-1.0), reads=[Ecl[bi]], writes=[X1i[bi]])
                    em.op("act", lambda e: e.activation(out=X2[bi][:, :], in_=pf.c(128, 256), func=AF.Exp), reads=[pf], writes=[X2[bi]])
                    em.op("act", lambda e: e.activation(out=X4[bi][:, :], in_=pf.c(256, 260), func=AF.Exp), reads=[pf], writes=[X4[bi]])
                    em.op("act", lambda e: e.activation(out=X3[bi][:, :], in_=p3.c(0, 128), func=AF.Exp), reads=[p3], writes=[X3[bi]])

                def scan_dir(d, const_x, post):
                    em.op("dve", lambda e: e.memset(S[:, :], 0.0), writes=[S])
                    em.op("pool", lambda e: e.memset(Sb[:, :], 0.0), writes=[Sb])
                    if d == 0:
                        order = list(range(NT))
                    else:
                        order = list(range(NTC - 1, -1, -1)) + list(range(NT - 1, NTC - 1, -1))
                    for it_, n in enumerate(order):
                        bi = it_ % 2
                        cs = slice(n * 128, (n + 1) * 128)
                        if const_x is None:
                            pg = pq()
                            em.op("pe", lambda e: e.matmul(pg.c(0, 128), lhsT=gT[:, cs], rhs=ident[:, :], start=True, stop=True), reads=[gT, ident], writes=[pg])
                            em.op("act", lambda e: e.activation(out=gtm[bi][:, :], in_=pg.c(0, 128), func=AF.Copy), reads=[pg], writes=[gtm[bi]])
                            exp_tiles(gtm[bi], d, bi)
                            x1, x1i, x2, x3, x4 = X1[bi], X1i[bi], X2[bi], X3[bi], X4[bi]
                        else:
                            x1, x1i, x2, x3, x4 = const_x
                        em.op("dve", lambda e: e.tensor_tensor(out=Qt[bi][:, :], in0=qT[:, cs], in1=x1[:, :], op=ALU.mult), reads=[qT, x1], writes=[Qt[bi]])
                        em.op("dve", lambda e: e.tensor_tensor(out=Kt[bi][:, :], in0=kT[:, cs], in1=x1i[:, :], op=ALU.mult), reads=[kT, x1i], writes=[Kt[bi]])
                        em.op("pool", lambda e: e.tensor_tensor(out=Qs[bi][:, :], in0=qT[:, cs], in1=x2[:, :], op=ALU.mult), reads=[qT, x2], writes=[Qs[bi]])
                        pk = pq()
                        em.op("pe", lambda e: e.matmul(pk.c(0, 128), lhsT=kT[:, cs], rhs=identb[:, :], start=True, stop=True), reads=[kT, identb], writes=[pk])
                        for a in range(NSB):
                            em.op("dve", lambda e: e.scalar_tensor_tensor(out=Kom[bi][:, a, :], in0=pk.c(0, 128), scalar=bmask[:, a:a + 1], in1=x3[:, :], op0=ALU.mult, op1=ALU.mult),
                                  reads=[pk, bmask, x3], writes=[Kom[bi]])
                        pa = pq()
                        em.op("pe", lambda e: e.matmul(pa.c(0, 128), lhsT=Kt[bi][:, :], rhs=Qt[bi][:, :], start=True, stop=True), reads=[Kt[bi], Qt[bi]], writes=[pa])
                        em.op("dve", lambda e: e.tensor_tensor(out=At[bi][:, :], in0=pa.c(0, 128), in1=amask[d][:, :], op=ALU.mult), reads=[pa, amask[d]], writes=[At[bi]])
                        po = [pq() for _ in range(nvc)]
                        blocks = range(NSB) if d == 0 else range(NSB - 1, -1, -1)
                        for a in blocks:
                            c0, c1 = a * SUB, (a + 1) * SUB
                            for c in range(nvc):
                                em.op("pe", lambda e: e.matmul(po[c].c(c0, c1), lhsT=vtm[:, n, c * 128:(c + 1) * 128], rhs=At[bi][:, c0:c1], start=True, stop=False),
                                      reads=[vtm, At[bi]], writes=[po[c]])
                                em.op("pe", lambda e: e.matmul(po[c].c(c0, c1), lhsT=Sb[:, c * 128:(c + 1) * 128], rhs=Qs[bi][:, c0:c1], start=False, stop=True),
                                      reads=[Sb, Qs[bi]], writes=[po[c]])
                            pst = pfull()
                            em.op("pe", lambda e: e.matmul(pst.c(0, dv), lhsT=Kom[bi][:, a, :], rhs=vtm[:, n, :], start=True, stop=True), reads=[Kom[bi], vtm], writes=[pst])
                            em.op("dve", lambda e: e.scalar_tensor_tensor(out=S[:, :], in0=S[:, :], scalar=x4[:, a:a + 1], in1=pst.c(0, dv), op0=ALU.mult, op1=ALU.add),
                                  reads=[S, x4, pst], writes=[S])
                            em.op("act", lambda e: e.activation(out=Sb[:, :], in_=S[:, :], func=AF.Copy), reads=[S], writes=[Sb])
                        if d == 1:
                            for c in range(nvc):
                                em.op("act", lambda e: e.activation(out=obuf[:, c, cs], in_=po[c].c(0, 128), func=AF.Copy), reads=[po[c]], writes=[obuf])
                        else:
                            for c in range(nvc):
                                em.op("dve", lambda e: e.tensor_tensor(out=ofin[bi][:, c, :], in0=po[c].c(0, 128), in1=obuf[:, c, cs], op=ALU.add), reads=[po[c], obuf], writes=[ofin[bi]])
                            post(n, bi)

                def run_group(gi, normcol, ech0, const_x_by_dir=None, prep_dir=None):
                    def post(n, bi):
                        cs = slice(n * 128, (n + 1) * 128)
                        if cfg.debug and l == 0 and s == 0:
                            for c in range(nvc):
                                em.dma("sp", DBG["o"][ech0 + c, :, cs], ofin[bi][:, c, :], reads=[ofin[bi]], writes=[DBG["o"]])
                        em.op("act", lambda e: e.activation(out=sq[bi][:, :, :], in_=ofin[bi][:, :, :], func=AF.Square), reads=[ofin[bi]], writes=[sq[bi]])
                        pss = pq()
                        for c in range(nvc):
                            em.op("pe", lambda e: e.matmul(pss.c(0, 128), lhsT=ones[:, :], rhs=sq[bi][:, c, :], start=(c == 0), stop=(c == nvc - 1)), reads=[ones, sq[bi]], writes=[pss])
                        em.op("act", lambda e: e.activation(out=rstd[bi][:, :], in_=pss.c(0, 128), func=AF.Sqrt, bias=EPS_RMS, scale=1.0 / dv), reads=[pss, misc], writes=[rstd[bi]])
                        em.op("dve", lambda e: e.reciprocal(out=rstd[bi][:, :], in_=rstd[bi][:, :]), reads=[rstd[bi]], writes=[rstd[bi]])
                        for c in range(nvc):
                            em.op("dve", lambda e: e.tensor_tensor(out=ofin[bi][:, c, :], in0=ofin[bi][:, c, :], in1=rstd[bi][:, :], op=ALU.mult), reads=[ofin[bi], rstd[bi]], writes=[ofin[bi]])
                            em.op("dve", lambda e: e.scalar_tensor_tensor(out=ytile[bi][:, c, :], in0=ofin[bi][:, c, :], scalar=normcol[:, c:c + 1], in1=sgT[:, c, cs], op0=ALU.mult, op1=ALU.mult),
                                  reads=[ofin[bi], prm, sgT], writes=[ytile[bi]])
                            em.dma("sp", YT[ech0 + c, :, cs], ytile[bi][:, c, :], reads=[ytile[bi]], writes=[YT])
                    for d in (1, 0):
                        if prep_dir is not None:
                            prep_dir(d)
                        ck(4)
                        scan_dir(d, None if const_x_by_dir is None else const_x_by_dir[d], post)

                def evac_copy(dst, scale=1.0):
                    def f(p, t0, nt):
                        em.op("act", lambda e: e.activation(out=dst[:, t0:t0 + nt], in_=p.c(0, nt), func=AF.Identity, scale=scale), reads=[p], writes=[dst])
                    return f

                def evac_silu(c):
                    def f(p, t0, nt):
                        em.op("act", lambda e: e.activation(out=sgT[:, c, t0:t0 + nt], in_=p.c(0, nt), func=AF.Silu), reads=[p], writes=[sgT])
                    return f

                if even:
                    for h in range(4):
                        proj_fm(0 + h * 128, 128, evac_copy(qT, 128 ** -0.5))
                        proj_tm(1536 + h * 128, 128, vtm)
                        proj_fm(2048 + h * 128, 128, evac_silu(0))
                        ck(3)

                        def prep_dir(d, h=h):
                            idx = d * 4 + h
                            def ev(p, t0, nt):
                                Et, L1, L2 = tmpE
                                em.op("act", lambda e: e.activation(out=Et[:, 0:nt], in_=p.c(0, nt), func=AF.Exp), reads=[p], writes=[Et])
                                em.op("act", lambda e: e.activation(out=L1[:, 0:nt], in_=Et[:, 0:nt], func=AF.Ln, bias=prm[:, idx:idx + 1], scale=1.0), reads=[Et, prm], writes=[L1])
                                em.op("act", lambda e: e.activation(out=L2[:, 0:nt], in_=Et[:, 0:nt], func=AF.Ln, bias=ONE, scale=1.0), reads=[Et, misc], writes=[L2])
                                em.op("dve", lambda e: e.tensor_tensor(out=gT[:, t0:t0 + nt], in0=L1[:, 0:nt], in1=L2[:, 0:nt], op=ALU.subtract), reads=[L1, L2], writes=[gT])
                                em.op("dve", lambda e: e.tensor_scalar(out=Et[:, 0:nt], in0=Et[:, 0:nt], scalar1=1.0, scalar2=None, op0=ALU.add), reads=[Et], writes=[Et])
                                em.op("dve", lambda e: e.reciprocal(out=Et[:, 0:nt], in_=Et[:, 0:nt]), reads=[Et], writes=[Et])
                                em.op("dve", lambda e: e.tensor_scalar(out=kT[:, t0:t0 + nt], in0=Et[:, 0:nt], scalar1=prm[:, 8 + idx:9 + idx], scalar2=None, op0=ALU.mult), reads=[Et, prm], writes=[kT])
                            proj_fm((512 if d == 0 else 1024) + h * 128, 128, ev)
                        run_group(h, prm[:, 24:25], h, None, prep_dir)
                        ck(5)
                    ck(6)
                    q0 = tmpE
                    for h in range(4):
                        def rope_evac(dst, scale):
                            def f(p, t0, nt):
                                em.op("act", lambda e: e.activation(out=dst[:, t0:t0 + nt], in_=p.c(0, nt), func=AF.Identity, scale=scale), reads=[p], writes=[dst])
                                a0 = max(t0, LC); a1 = t0 + nt
                                if a1 <= a0:
                                    return
                                w_ = a1 - a0
                                pr = pfull()
                                em.op("pe", lambda e: e.matmul(pr.c(0, w_), lhsT=rotb[:, :], rhs=dst[:, a0:a1], start=True, stop=True), reads=[rotb, dst], writes=[pr])
                                t1, t2 = tmpE[0], tmpE[1]
                                em.op("dve", lambda e: e.tensor_tensor(out=t1[:, 0:w_], in0=dst[:, a0:a1], in1=cosT[:, a0 - LC:a1 - LC], op=ALU.mult), reads=[dst, cosT], writes=[t1])
                                em.op("dve", lambda e: e.tensor_tensor(out=t2[:, 0:w_], in0=pr.c(0, w_), in1=sinT[:, a0 - LC:a1 - LC], op=ALU.mult), reads=[pr, sinT], writes=[t2])
                                em.op("dve", lambda e: e.tensor_tensor(out=dst[:, a0:a1], in0=t1[:, 0:w_], in1=t2[:, 0:w_], op=ALU.add), reads=[t1, t2], writes=[dst])
                            return f
                        proj_fm(2560 + h * 128, 128, rope_evac(qT, 128 ** -0.5))
                        proj_fm(3072 + h * 128, 128, rope_evac(kT, 1.0))
                        proj_tm(3584 + h * 128, 128, vtm)
                        proj_fm(4096 + h * 128, 128, evac_silu(0))
                        cx = {}
                        for d in (0, 1):
                            idx = 16 + d * 4 + h
                            em.op("dve", lambda e: e.tensor_scalar(out=gtm[d][:, :], in0=ones[:, :], scalar1=prm[:, idx:idx + 1], scalar2=None, op0=ALU.mult), reads=[ones, prm], writes=[gtm[d]])
                            if (h, d) not in XR:
                                XR[(h, d)] = None
                            exp_tiles(gtm[d], d, d)
                            tl = [em.sb([128, 128], F32, "xr", ph) for _ in range(4)] + [em.sb([128, 4], F32, "xr4", ph)] if XR.get("bufs%d" % d) is None else XR["bufs%d" % d]
                            XR["bufs%d" % d] = tl
                            for src, dst_ in zip((X1[d], X1i[d], X2[d], X3[d], X4[d]), tl):
                                em.op("pool", lambda e: e.tensor_copy(out=dst_.h.ap(), in_=src.h.ap()), reads=[src], writes=[dst_])
                            cx[d] = tl
                        run_group(4 + h, prm[:, 25:26], 4 + h, cx, None)
                        ck(7)
                    ck(98)
                else:
                    for h in range(4):
                        proj_fm(0 + h * 128, 128, evac_copy(qT, 128 ** -0.5))
                        proj_fm(512 + h * 128, 128, evac_copy(kT, 1.0))
                        proj_tm(1024 + h * 256, 256, vtm)
                        for c in range(2):
                            proj_fm(2048 + h * 256 + c * 128, 128, evac_silu(c))

                        def prep_dir(d, h=h):
                            idx = d * 4 + h
                            def evl(p, t0, nt):
                                em.op("act", lambda e: e.activation(out=lrT[:, t0:t0 + nt], in_=p.c(0, nt, 0, 16), func=AF.Copy), reads=[p], writes=[lrT])
                            proj_fm(3072 + d * 16, 16, evl)
                            for t0 in range(0, NTOK, 512):
                                nt = min(512, NTOK - t0)
                                p = pfull()
                                em.op("pe", lambda e: e.matmul(p.c(0, nt), lhsT=w2[:, d, h * 128:(h + 1) * 128], rhs=lrT[:, t0:t0 + nt], start=True, stop=True), reads=[w2, lrT], writes=[p])
                                Et, L1 = tmpE[0], tmpE[1]
                                em.op("act", lambda e: e.activation(out=Et[:, 0:nt], in_=p.c(0, nt), func=AF.Exp, scale=-1.0, bias=prm[:, idx:idx + 1]), reads=[p, prm], writes=[Et])
                                em.op("act", lambda e: e.activation(out=L1[:, 0:nt], in_=Et[:, 0:nt], func=AF.Ln, bias=ONE, scale=1.0), reads=[Et, misc], writes=[L1])
                                em.op("dve", lambda e: e.tensor_scalar(out=gT[:, t0:t0 + nt], in0=L1[:, 0:nt], scalar1=-1.0 / GC_TAU, scalar2=None, op0=ALU.mult), reads=[L1], writes=[gT])
                        run_group(h, prm[:, 24:26], 2 * h, None, prep_dir)

        def mixer_out(s, l, jl, w_out):
            def ck(k_):
                if cfg.sub == k_:
                    em.muted = True
            with ExitStack() as ph:
                wo = em.sb([128, NCH, D], BF16, "wo", ph)
                em.dma("pool", wo[:, :, :], w_out[jl].rearrange("(c p) n -> p c n", p=128), writes=[wo])
                g1bc = [em.sb([128, D], F32, "g1bc", ph) for _ in range(2)]
                lnw = em.sb([128, D], F32, "lnw", ph); lnb = em.sb([128, D], F32, "lnb", ph)
                bc_load(g1bc[0], MOD[l, s:s + 1, 2 * D:3 * D], MOD)
                bc_load(g1bc[1], MOD[l, B:B + 1, 2 * D:3 * D], MOD)
                em.dma("sp", lnw[:, :], I["ln_w"][l, 0:1, :].to_broadcast([128, D]), writes=[lnw])
                em.dma("sp", lnb[:, :], I["ln_b"][l, 0:1, :].to_broadcast([128, D]), writes=[lnb])
                tmp = {"st2": em.sb([128, 8], F32, "st2", ph), "junk": em.sb([128, D], F32, "junk", ph)}
                yts = [em.sb([128, NCH, 128], BF16, "yts", ph) for _ in range(2)]
                xts = [em.sb([128, D], F32, "xts", ph) for _ in range(2)]
                us = [em.sb([128, D], F32, "us", ph) for _ in range(2)]
                ck(101)
                for n in range(NT):
                    bi = n % 2
                    cs = slice(n * 128, (n + 1) * 128)
                    em.dma("sp", yts[bi][:, :, :], YT[:, :, cs].rearrange("c p t -> p c t"), reads=[YT], writes=[yts[bi]])
                    em.dma("sp", xts[bi][:, :], XS[s, cs, :], reads=[XS], writes=[xts[bi]])
                    ck(102)
                    gb = g1bc[1 if n < NTC else 0]
                    for dh in range(2):
                        p = pfull()
                        for ch in range(NCH):
                            em.op("pe", lambda e: e.matmul(p.c(0, 512), lhsT=yts[bi][:, ch, :], rhs=wo[:, ch, dh * 512:(dh + 1) * 512], start=(ch == 0), stop=(ch == NCH - 1)),
                                  reads=[yts[bi], wo], writes=[p])
                        if cfg.debug and l == 0 and s == 0:
                            em.op("act", lambda e: e.activation(out=tmp["junk"][:, dh * 512:(dh + 1) * 512], in_=p.c(0, 512), func=AF.Copy), reads=[p], writes=[tmp["junk"]])
                        em.op("dve", lambda e: e.tensor_tensor(out=us[bi][:, dh * 512:(dh + 1) * 512], in0=p.c(0, 512), in1=gb[:, dh * 512:(dh + 1) * 512], op=ALU.mult), reads=[p, gb], writes=[us[bi]])
                    if cfg.debug and l == 0 and s == 0:
                        em.dma("sp", DBG["y"][cs, :], tmp["junk"][:, :], reads=[tmp["junk"]], writes=[DBG["y"]])
                    ck(103)
                    em.op("dve", lambda e: e.scalar_tensor_tensor(out=us[bi][:, :], in0=xts[bi][:, :], scalar=DN_ALPHA, in1=us[bi][:, :], op0=ALU.mult, op1=ALU.add), reads=[xts[bi], us[bi]], writes=[us[bi]])
                    ck(104)
                    layernorm_tile(tmp, us[bi], lnw, lnb, xts[bi])
                    ck(105)
                    em.dma("sp", XS[s, cs, :], xts[bi][:, :], reads=[xts[bi]], writes=[XS])
                    if cfg.debug and l == 0 and s == 0:
                        em.dma("sp", DBG["xmid"][cs, :], xts[bi][:, :], reads=[xts[bi]], writes=[DBG["xmid"]])
            em.muted = False
            em.barrier()

        def ffn_phase(s, l):
            last = (l == cfg.DEPTH - 1)
            NS, capL, capC, NF, Fd = cfg.NS, cfg.capL, cfg.capC, cfg.NF, cfg.F
            ctiles = [(c0, min(128, NS - c0)) for c0 in range(0, NS, 128)]
            with ExitStack() as ph0:
                yacc = em.sb([128, NT, D], F32, "yacc", ph0)
                aff = em.sb([128, NT, E], F32, "aff", ph0)
                rkg = em.sb([128, NT, E], F32, "rkg", ph0)
                ph = ExitStack()
                bcs = [em.sb([128, D], F32, "bc", ph) for _ in range(2)]
                msk = em.sb([128, NT, E], F32, "msk", ph)
                affT = em.sb([E, NTOK], F32, "affT", ph)
                wrk = em.sb([E, NTOK], F32, "wrk", ph)
                mx8 = em.sb([E, 8], F32, "mx8", ph)
                wr = em.sb([128, NCH, E], F32, "wr", ph)
                xts = [em.sb([128, D], F32, "xts", ph) for _ in range(2)]
                x2f = [em.sb([128, D], F32, "x2f", ph) for _ in range(2)]
                x2b = [em.sb([128, D], BF16, "x2b", ph) for _ in range(2)]
                x2T = [em.sb([128, NCH, 128], F32, "x2T", ph) for _ in range(2)]
                sm = [em.sb([128, 4], F32, "sm", ph) for _ in range(2)]
                cum = em.sb([128, E], F32, "cum", ph)
                em.dma("sp", wr[:, :, :], I["router_w"][l].rearrange("(c p) n -> p c n", p=128), writes=[wr])
                em.op("pool", lambda e: e.memset(yacc[:, :, :], 0.0), writes=[yacc])
                for n in range(NT):
                    bi = n % 2
                    cs = slice(n * 128, (n + 1) * 128)
                    if n == 0 or n == NTC:
                        row = B if n < NTC else s
                        bc_load(bcs[0], MOD[l, row:row + 1, 4 * D:5 * D], MOD)
                        em.op("dve", lambda e: e.tensor_scalar(out=bcs[0][:, :], in0=bcs[0][:, :], scalar1=1.0, scalar2=None, op0=ALU.add), reads=[bcs[0]], writes=[bcs[0]])
                        bc_load(bcs[1], MOD[l, row:row + 1, 3 * D:4 * D], MOD)
                    em.dma("sp", xts[bi][:, :], XS[s, cs, :], reads=[XS], writes=[xts[bi]])
                    em.op("dve", lambda e: e.tensor_tensor(out=x2f[bi][:, :], in0=xts[bi][:, :], in1=bcs[0][:, :], op=ALU.mult), reads=[xts[bi], bcs[0]], writes=[x2f[bi]])
                    em.op("dve", lambda e: e.tensor_tensor(out=x2f[bi][:, :], in0=x2f[bi][:, :], in1=bcs[1][:, :], op=ALU.add), reads=[x2f[bi], bcs[1]], writes=[x2f[bi]])
                    em.op("act", lambda e: e.activation(out=x2b[bi][:, :], in_=x2f[bi][:, :], func=AF.Copy), reads=[x2f[bi]], writes=[x2b[bi]])
                    em.dma("sp", XB[n, :, :], x2b[bi][:, :], reads=[x2b[bi]], writes=[XB])
                    for ch in range(NCH):
                        p = pq()
                        em.op("pe", lambda e: e.matmul(p.c(0, 128), lhsT=x2f[bi][:, ch * 128:(ch + 1) * 128], rhs=ident[:, :], start=True, stop=True), reads=[x2f[bi], ident], writes=[p])
                        em.op("act", lambda e: e.activation(out=x2T[bi][:, ch, :], in_=p.c(0, 128), func=AF.Copy), reads=[p], writes=[x2T[bi]])
                    pl = pq()
                    for ch in range(NCH):
                        em.op("pe", lambda e: e.matmul(pl.c(0, E), lhsT=x2T[bi][:, ch, :], rhs=wr[:, ch, :], start=(ch == 0), stop=(ch == NCH - 1)), reads=[x2T[bi], wr], writes=[pl])
                    smt = sm[bi]
                    em.op("dve", lambda e: e.tensor_reduce(out=smt[:, 0:1], in_=pl.c(0, E), axis=mybir.AxisListType.X, op=ALU.max), reads=[pl], writes=[smt])
                    em.op("dve", lambda e: e.tensor_scalar(out=smt[:, 1:2], in0=smt[:, 0:1], scalar1=-1.0, scalar2=None, op0=ALU.mult), reads=[smt], writes=[smt])
                    em.op("dve", lambda e: e.memset(smt[:, 2:3], 0.0), writes=[smt])
                    em.op("act", lambda e: e.activation(out=aff[:, n, :], in_=pl.c(0, E), func=AF.Exp, bias=smt[:, 1:2], scale=1.0, accum_out=smt[:, 2:3]), reads=[pl, smt], writes=[aff, smt])
                    em.op("dve", lambda e: e.reciprocal(out=smt[:, 3:4], in_=smt[:, 2:3]), reads=[smt], writes=[smt])
                    em.op("dve", lambda e: e.tensor_scalar(out=aff[:, n, :], in0=aff[:, n, :], scalar1=smt[:, 3:4], scalar2=None, op0=ALU.mult), reads=[aff, smt], writes=[aff])
                    pt_ = pq()
                    em.op("pe", lambda e: e.matmul(pt_.c(0, 128, 0, E), lhsT=aff[:, n, :], rhs=ident[:, :], start=True, stop=True), reads=[aff, ident], writes=[pt_])
                    em.op("act", lambda e: e.activation(out=affT[:, cs], in_=pt_.c(0, 128, 0, E), func=AF.Copy), reads=[pt_], writes=[affT])
                for (t0, n_, cap, off, nt0, ntn) in ((0, LC, capC, capL, 0, NTC), (LC, LL, capL, 0, NTC, NT)):
                    em.op("dve", lambda e: e.tensor_copy(out=wrk[:, t0:t0 + n_], in_=affT[:, t0:t0 + n_]), reads=[affT], writes=[wrk])
                    for r in range(cap // 8):
                        em.op("dve", lambda e: e.max(out=mx8[:, :], in_=wrk[:, t0:t0 + n_]), reads=[wrk], writes=[mx8])
                        if r < cap // 8 - 1:
                            em.op("dve", lambda e: e.match_replace(out=wrk[:, t0:t0 + n_], in_to_replace=mx8[:, :], in_values=wrk[:, t0:t0 + n_], imm_value=-1e30), reads=[wrk, mx8], writes=[wrk])
                    em.op("dve", lambda e: e.tensor_scalar(out=wrk[:, t0:t0 + n_], in0=affT[:, t0:t0 + n_], scalar1=mx8[:, 7:8], scalar2=None, op0=ALU.is_ge), reads=[affT, mx8], writes=[wrk])
                    em.op("dve", lambda e: e.memset(cum[:, :], 0.0), writes=[cum])
                    for n in range(nt0, ntn):
                        cs = slice(n * 128, (n + 1) * 128)
                        pm = pq()
                        em.op("pe", lambda e: e.matmul(pm.c(0, E), lhsT=wrk[:, cs], rhs=ident[0:E, 0:E], start=True, stop=True), reads=[wrk, ident], writes=[pm])
                        em.op("act", lambda e: e.activation(out=msk[:, n, :], in_=pm.c(0, E), func=AF.Copy), reads=[pm], writes=[msk])
                        pr = pq()
                        em.op("pe", lambda e: e.matmul(pr.c(0, E), lhsT=tri[:, :], rhs=msk[:, n, :], start=True, stop=False), reads=[tri, msk], writes=[pr])
                        em.op("pe", lambda e: e.matmul(pr.c(0, E), lhsT=ones[:, :], rhs=cum[:, :], start=False, stop=True), reads=[ones, cum], writes=[pr])
                        em.op("dve", lambda e: e.scalar_tensor_tensor(out=rkg[:, n, :], in0=pr.c(0, E), scalar=float(off + 1), in1=msk[:, n, :], op0=ALU.add, op1=ALU.mult), reads=[pr, msk], writes=[rkg])
                        em.op("dve", lambda e: e.tensor_scalar(out=rkg[:, n, :], in0=rkg[:, n, :], scalar1=-1.0, scalar2=None, op0=ALU.add), reads=[rkg], writes=[rkg])
                        em.op("dve", lambda e: e.tensor_tensor(out=cum[:, :], in0=cum[:, :], in1=msk[:, n, :], op=ALU.add), reads=[cum, msk], writes=[cum])
                        pt_ = pq()
                        em.op("pe", lambda e: e.matmul(pt_.c(0, 128, 0, E), lhsT=rkg[:, n, :], rhs=ident[:, :], start=True, stop=True), reads=[rkg, ident], writes=[pt_])
                        em.op("act", lambda e: e.activation(out=affT[:, cs], in_=pt_.c(0, 128, 0, E), func=AF.Copy), reads=[pt_], writes=[affT])
                em.dma("sp", RK.h.ap(), affT.h.ap(), reads=[affT], writes=[RK])
                em.barrier()
                ph.close()

                ph = ExitStack()
                rbc = em.sb([128, NTOK], F32, "rbc", ph)
                PTs = [em.sb([128, NTOK], BF16, "PT", ph) for _ in ctiles]
                Pn = em.sb([128, NT, NS], BF16, "Pn", ph)
                xsT = em.sb([128, NCH, NS], BF16, "xsT", ph)
                hidT = em.sb([128, NF, NS], BF16, "hidT", ph)
                ys = [em.sb([128, D], BF16, "ys", ph) for _ in ctiles]
                FG = min(512, Fd)
                wg = [em.sb([128, NCH, FG], BF16, "wg", ph) for _ in range(2)]
                wu = [em.sb([128, NCH, FG], BF16, "wu", ph) for _ in range(2)]
                wd = [em.sb([128, FG // 128, D], BF16, "wd", ph) for _ in range(2)]
                hs = [em.sb([128, NS], F32, "hs", ph) for _ in range(2)]
                xbt = [em.sb([128, D], BF16, "xbt", ph) for _ in range(2)]
                wi = [0]
                for ex in range(E):
                    em.dma("sp", rbc[:, :], RK[ex:ex + 1, :].to_broadcast([128, NTOK]), reads=[RK], writes=[rbc])
                    for ci, (c0, cw) in enumerate(ctiles):
                        em.op("pool", lambda e: e.tensor_scalar(out=PTs[ci][:, :], in0=rbc[:, :], scalar1=misc[:, 4 + ci:5 + ci], scalar2=None, op0=ALU.is_equal), reads=[rbc, misc], writes=[PTs[ci]])
                    for n in range(NT):
                        em.op("pool", lambda e: e.tensor_scalar(out=Pn[:, n, :], in0=iota[:, 0:NS], scalar1=rkg[:, n, ex:ex + 1], scalar2=None, op0=ALU.is_equal), reads=[iota, rkg], writes=[Pn])
                    for half in range(2):
                        pg = [PF[i] for i in range(4)]
                        for n in range(NT):
                            xb_ = xbt[n % 2]
                            em.dma("sp", xb_[:, :], XB[n, :, :], reads=[XB], writes=[xb_])
                            for k in range(4):
                                ch = half * 4 + k
                                em.op("pe", lambda e: e.matmul(pg[k].c(0, NS), lhsT=xb_[:, ch * 128:(ch + 1) * 128], rhs=Pn[:, n, :], start=(n == 0), stop=(n == NT - 1)), reads=[xb_, Pn], writes=[pg[k]])
                        for k in range(4):
                            ch = half * 4 + k
                            em.op("act", lambda e: e.activation(out=xsT[:, ch, :], in_=pg[k].c(0, NS), func=AF.Copy), reads=[pg[k]], writes=[xsT])
                    for f0 in range(0, Fd, FG):
                        fw = min(FG, Fd - f0)
                        wi[0] += 1
                        g_, u_ = wg[wi[0] % 2], wu[wi[0] % 2]
                        em.dma("pool", g_[:, :, 0:fw], I["exp_w_gate"][l, ex, :, f0:f0 + fw].rearrange("(c p) f -> p c f", p=128), writes=[g_])
                        em.dma("pool", u_[:, :, 0:fw], I["exp_w_up"][l, ex, :, f0:f0 + fw].rearrange("(c p) f -> p c f", p=128), writes=[u_])
                        for fc in range(fw // 128):
                            fi = f0 // 128 + fc
                            p1, p2 = pfull(), pfull()
                            for ch in range(NCH):
                                em.op("pe", lambda e: e.matmul(p1.c(0, NS), lhsT=g_[:, ch, fc * 128:(fc + 1) * 128], rhs=xsT[:, ch, :], start=(ch == 0), stop=(ch == NCH - 1)), reads=[g_, xsT], writes=[p1])
                            for ch in range(NCH):
                                em.op("pe", lambda e: e.matmul(p2.c(0, NS), lhsT=u_[:, ch, fc * 128:(fc + 1) * 128], rhs=xsT[:, ch, :], start=(ch == 0), stop=(ch == NCH - 1)), reads=[u_, xsT], writes=[p2])
                            h_ = hs[fi % 2]
                            em.op("act", lambda e: e.activation(out=h_[:, :], in_=p1.c(0, NS), func=AF.Silu), reads=[p1], writes=[h_])
                            em.op("dve", lambda e: e.tensor_tensor(out=hidT[:, fi, :], in0=h_[:, :], in1=p2.c(0, NS), op=ALU.mult), reads=[h_, p2], writes=[hidT])
                    nacc = len(ctiles) * 2
                    assert nacc <= 6
                    pacc = [T(banks[i], "pacc%d" % i, trk=BK[i]) for i in range(nacc)]
                    for f0 in range(0, Fd, FG):
                        fw = min(FG, Fd - f0)
                        wi[0] += 1
                        d_ = wd[wi[0] % 2]
                        em.dma("pool", d_[:, 0:fw // 128, :], I["exp_w_down"][l, ex, f0:f0 + fw, :].rearrange("(c p) n -> p c n", p=128), writes=[d_])
                        for fc in range(fw // 128):
                            fi = f0 // 128 + fc
                            for ci, (c0, cw) in enumerate(ctiles):
                                for dh in range(2):
                                    pa_ = pacc[ci * 2 + dh]
                                    em.op("pe", lambda e: e.matmul(pa_.c(0, 512, 0, cw), lhsT=hidT[:, fi, c0:c0 + cw], rhs=d_[:, fc, dh * 512:(dh + 1) * 512], start=(fi == 0), stop=(fi == NF - 1)),
                                          reads=[hidT, d_], writes=[pa_])
                    for ci, (c0, cw) in enumerate(ctiles):
                        for dh in range(2):
                            pa_ = pacc[ci * 2 + dh]
                            em.op("act", lambda e: e.activation(out=ys[ci][0:cw, dh * 512:(dh + 1) * 512], in_=pa_.c(0, 512, 0, cw), func=AF.Copy), reads=[pa_], writes=[ys[ci]])
                    for n in range(NT):
                        cs = slice(n * 128, (n + 1) * 128)
                        if n < NTC:
                            use = [ci for ci, (c0, cw) in enumerate(ctiles) if c0 + cw > capL]
                        else:
                            use = [ci for ci, (c0, cw) in enumerate(ctiles) if c0 < capL]
                        for dh in range(2):
                            p = pfull()
                            for k, ci in enumerate(use):
                                c0, cw = ctiles[ci]
                                em.op("pe", lambda e: e.matmul(p.c(0, 512), lhsT=PTs[ci][0:cw, cs], rhs=ys[ci][0:cw, dh * 512:(dh + 1) * 512], start=(k == 0), stop=(k == len(use) - 1)),
                                      reads=[PTs[ci], ys[ci]], writes=[p])
                            em.op("dve", lambda e: e.scalar_tensor_tensor(out=yacc[:, n, dh * 512:(dh + 1) * 512], in0=p.c(0, 512), scalar=aff[:, n, ex:ex + 1], in1=yacc[:, n, dh * 512:(dh + 1) * 512], op0=ALU.mult, op1=ALU.add),
                                  reads=[p, aff, yacc], writes=[yacc])
                em.barrier()
                ph.close()
                ph = ExitStack()
                bcs = [em.sb([128, D], F32, "bc", ph) for _ in range(3)]
                tmp = {"st2": em.sb([128, 8], F32, "st2", ph), "junk": em.sb([128, D], F32, "junk", ph)}
                xts = [em.sb([128, D], F32, "xts", ph) for _ in range(2)]
                x2f = [em.sb([128, D], F32, "x2f", ph) for _ in range(2)]
                em.dma("sp", bcs[1][:, :], I["ln_w"][l, 1:2, :].to_broadcast([128, D]), writes=[bcs[1]])
                em.dma("sp", bcs[2][:, :], I["ln_b"][l, 1:2, :].to_broadcast([128, D]), writes=[bcs[2]])
                for n in range(NT):
                    if last and n < NTC:
                        continue
                    bi = n % 2
                    cs = slice(n * 128, (n + 1) * 128)
                    if n == 0 or n == NTC or (last and n == NTC):
                        row = B if n < NTC else s
                        bc_load(bcs[0], MOD[l, row:row + 1, 5 * D:6 * D], MOD)
                    if cfg.debug and l == 0 and s == 0:
                        em.dma("sp", DBG["ffn"][cs, :], yacc[:, n, :], reads=[yacc], writes=[DBG["ffn"]])
                    em.dma("sp", xts[bi][:, :], XS[s, cs, :], reads=[XS], writes=[xts[bi]])
                    em.op("dve", lambda e: e.tensor_tensor(out=x2f[bi][:, :], in0=yacc[:, n, :], in1=bcs[0][:, :], op=ALU.mult), reads=[yacc, bcs[0]], writes=[x2f[bi]])
                    em.op("dve", lambda e: e.scalar_tensor_tensor(out=x2f[bi][:, :], in0=xts[bi][:, :], scalar=DN_ALPHA, in1=x2f[bi][:, :], op0=ALU.mult, op1=ALU.add), reads=[xts[bi], x2f[bi]], writes=[x2f[bi]])
                    layernorm_tile(tmp, x2f[bi], bcs[1], bcs[2], xts[bi])
                    if last:
                        em.dma("sp", OUT[s, (n - NTC) * 128:(n - NTC + 1) * 128, :], xts[bi][:, :], reads=[xts[bi]], writes=[OUT])
                    else:
                        em.dma("sp", XS[s, cs, :], xts[bi][:, :], reads=[xts[bi]], writes=[XS])
                em.barrier()
                ph.close()
            em.barrier()

        try:
            for s in range(B):
                for l in range(cfg.DEPTH):
                    if cfg.stage >= 2 + 2 * l:
                        mixer_phase(s, l)
                    if cfg.stage >= 3 + 2 * l:
                        ffn_phase(s, l)
        except Exception:
            import traceback
            traceback.print_exc()
            raise
        em.barrier()
        build.ninstr = em.n
    return nc, consts


_CACHE = {}


def run(cfg, inputs, ncores):
    key = (cfg.B, cfg.LC, cfg.LL, cfg.F, cfg.DEPTH, cfg.debug, cfg.stage)
    if key not in _CACHE:
        _CACHE[key] = build(cfg)
    nc, consts = _CACHE[key]
    B = cfg.B
    in_maps = []
    shp = INPUT_SHAPES(cfg)
    for c in range(ncores):
        m = {}
        for k in shp:
            a = np.asarray(inputs[k], dtype=np.float32)
            if k in ("x", "c", "ctx"):
                a = a[c * B:(c + 1) * B]
            a = np.ascontiguousarray(a).reshape(shp[k])
            m[k] = a
        m.update(consts)
        in_maps.append(m)
    res = run_bass_kernel_spmd(nc, in_maps, core_ids=list(range(ncores)))
    return res


def kernel(**inputs):
    ncores = 8
    cfg = Cfg(B=32 // ncores)
    res = run(cfg, inputs, ncores)
    out = np.concatenate([r["out"] for r in res.results], axis=0)
    return out.astype(np.float32)
```

```python
import numpy as np
from contextlib import ExitStack
import concourse.bass as bass
import concourse.mybir as mybir
from concourse.bass_utils import run_bass_kernel_spmd

F32 = mybir.dt.float32
BF16 = mybir.dt.bfloat16
AF = mybir.ActivationFunctionType
ALU = mybir.AluOpType

D = 1024
NCH = 8
E = 16
SUB = 32
NSB = 4
LN_EPS = 1e-5
RMS_EPS = 1e-6
DN_ALPHA = 8.0 ** 0.25
GC_TAU = 16.0
CLAMP = 35.0
EVEN_IN = 4608
ODD_IN = 3104
SAME_ENGINE_SYNC = True


class Cfg:
    def __init__(self, B=4, LC=256, LL=2048, F=2816, DEPTH=4, debug=False, stage=99):
        self.B, self.LC, self.LL, self.F, self.DEPTH, self.debug = B, LC, LL, F, DEPTH, debug
        self.stage = stage
        self.sub = 99
        self.NTC, self.NTL = LC // 128, LL // 128
        self.NT = self.NTC + self.NTL
        self.NTOK = LC + LL
        self.capL, self.capC = LL // 8, LC // 8
        self.NS = self.capL + self.capC
        self.NF = F // 128


class Stop(Exception):
    pass


class T:
    __slots__ = ("h", "lw", "rd", "name", "base", "trk")

    def __init__(self, h, name="", base=0, trk=None):
        self.h, self.lw, self.rd, self.name, self.base = h, None, [], name, base
        self.trk = trk if trk is not None else self

    def __getitem__(self, idx):
        return self.h[idx]

    def c(self, lo, hi, p0=0, p1=128):
        return self.h[p0:p1, self.base + lo:self.base + hi]


class Em:
    def __init__(self, nc, stack):
        self.nc, self.stack = nc, stack
        self.eng = {"pe": nc.tensor, "act": nc.scalar, "dve": nc.vector, "pool": nc.gpsimd, "sp": nc.sync}
        self.sem, self.cnt = {}, {}
        for e in self.eng:
            self.sem[e] = stack.enter_context(nc.semaphore("s_" + e))
            self.cnt[e] = 0
        self.KD = 16
        self.dnext = {}
        for q in ("sp", "pool"):
            self.dnext[q] = 0
            for i in range(self.KD):
                key = "d_%s_%d" % (q, i)
                self.sem[key] = stack.enter_context(nc.semaphore("sd_%s_%d" % (q, i)))
                self.cnt[key] = 0
        self.seen = {e: {} for e in self.eng}
        self.n = 0
        self.uid = 0
        self.muted = False

    def sb(self, shape, dt, name="t", stack=None):
        self.uid += 1
        nm = "%s_%d" % (name, self.uid)
        h = (stack or self.stack).enter_context(self.nc.sbuf_tensor(nm, list(shape), dt))
        return T(h, nm)

    def psum_banks(self):
        banks = []
        for i in range(8):
            banks.append(self.stack.enter_context(self.nc.psum_tensor("psb%d" % i, [128, 512], F32)))
        return banks

    def dram(self, shape, dt, name="d"):
        self.uid += 1
        nm = "%s_%d" % (name, self.uid)
        return T(self.nc.dram_tensor(nm, list(shape), dt), nm)

    def _deps(self, e, reads, writes):
        need = {}
        seen = self.seen[e]

        def add(dep):
            s, v = dep
            if s == e and not SAME_ENGINE_SYNC:
                return
            if seen.get(s, 0) >= v:
                return
            if need.get(s, 0) < v:
                need[s] = v
        for t in reads:
            t = t.trk
            if t.lw is not None:
                add(t.lw)
            if t.name.startswith("bk"):
                for r in t.rd:
                    if r[0] != e:
                        add(r)
        for t in writes:
            t = t.trk
            if t.lw is not None:
                add(t.lw)
            for r in t.rd:
                add(r)
        for s, v in need.items():
            self.eng[e].wait_ge(self.sem[s], v)
            seen[s] = v
            self.n += 1

    def _rec(self, key, reads, writes):
        v = self.cnt[key]
        for t in reads:
            t.trk.rd.append((key, v))
        for t in writes:
            t.trk.lw = (key, v)
            t.trk.rd = []

    def op(self, e, fn, reads=(), writes=()):
        if self.muted:
            return None
        self._deps(e, reads, writes)
        ins = fn(self.eng[e])
        self.cnt[e] += 1
        ins.then_inc(self.sem[e], 1)
        self._rec(e, reads, writes)
        self.n += 1
        return ins

    def dma(self, q, out, in_, reads=(), writes=(), **kw):
        if self.muted:
            return None
        self._deps(q, reads, writes)
        key = "d_%s_%d" % (q, self.dnext[q] % self.KD)
        self.dnext[q] += 1
        if self.cnt[key] > 0 and self.seen[q].get(key, 0) < self.cnt[key]:
            self.eng[q].wait_ge(self.sem[key], self.cnt[key])
            self.seen[q][key] = self.cnt[key]
            self.n += 1
        ins = self.eng[q].dma_start(out=out, in_=in_, **kw)
        self.cnt[key] += 16
        ins.then_inc(self.sem[key], 16)
        self._rec(key, reads, writes)
        self.n += 1
        return ins

    def barrier(self):
        for e in self.eng:
            for s, v in self.cnt.items():
                if v > 0 and self.seen[e].get(s, 0) < v and s != e:
                    self.eng[e].wait_ge(self.sem[s], v)
                    self.seen[e][s] = v
                    self.n += 1


def make_consts(cfg):
    j = np.arange(128)
    same = (j[:, None] // SUB) == (j[None, :] // SUB)
    loc = j % SUB
    le = j[:, None] <= j[None, :]
    ge = j[:, None] >= j[None, :]
    mfm = np.zeros((2, 128, 264), np.float32)
    m3 = np.zeros((2, 128, 128), np.float32)
    mask = np.zeros((2, 128, 128), np.float32)
    bm = (j[:, None] // SUB == np.arange(NSB)[None, :]).astype(np.float32)
    mfm[0, :, 0:128] = same * (le.astype(np.float32) - (loc[:, None] <= SUB // 2 - 1))
    mfm[0, :, 128:256] = same * le
    mfm[0, :, 256:260] = bm
    m3[0] = same * (j[:, None] > j[None, :])
    mask[0] = same * le
    mfm[1, :, 0:128] = same * (ge.astype(np.float32) - (loc[:, None] >= SUB // 2))
    mfm[1, :, 128:256] = same * ge
    mfm[1, :, 256:260] = bm
    m3[1] = same * (j[:, None] < j[None, :])
    mask[1] = same * ge
    rot = np.zeros((128, 128), np.float32)
    for k in range(64):
        rot[k + 64, k] = -1.0
        rot[k, k + 64] = 1.0
    rows = cfg.LL // 64
    r_idx, c_idx = np.meshgrid(np.arange(rows), np.arange(64), indexing="ij")
    nfreq = 32
    freq = (10000.0 ** (-np.arange(nfreq, dtype=np.float32) / nfreq)).astype(np.float32)
    ang = np.concatenate([r_idx.reshape(-1, 1).astype(np.float32) * freq,
                          c_idx.reshape(-1, 1).astype(np.float32) * freq], axis=-1)
    cosT = np.concatenate([np.cos(ang).T, np.cos(ang).T], axis=0).astype(np.float32)
    sinT = np.concatenate([np.sin(ang).T, np.sin(ang).T], axis=0).astype(np.float32)
    misc = np.zeros((128, 8), np.float32)
    misc[:, 0] = LN_EPS
    misc[:, 1] = RMS_EPS
    misc[:, 2] = 1.0
    for a in range(4):
        misc[:, 4 + a] = j + 128 * a
    return {
        "c_ident": np.eye(128, dtype=np.float32),
        "c_iota": np.tile(np.arange(512, dtype=np.float32)[None, :], (128, 1)),
        "c_mfm": mfm, "c_m3": m3, "c_mask": mask, "c_bm": bm,
        "c_tri": (j[:, None] < j[None, :]).astype(np.float32),
        "c_ones": np.ones((128, 128), np.float32),
        "c_rot": rot, "c_cos": np.ascontiguousarray(cosT), "c_sin": np.ascontiguousarray(sinT),
        "c_misc": misc,
    }


INPUT_SHAPES = lambda cfg: {
    "x": [cfg.B, cfg.LL, D], "c": [cfg.B, D], "ctx": [cfg.B, cfg.LC, D], "c_ctx": [1, D],
    "ada_w": [4, D, 6 * D], "ada_b": [4, 6 * D], "ln_w": [4, 2, D], "ln_b": [4, 2, D],
    "even_w_in": [2, D, EVEN_IN], "even_w_out": [2, 1024, D], "ha_lb": [2, 2, 512], "ha_norm": [2, 128],
    "rb_decay": [2, 8], "odd_w_in": [2, D, ODD_IN], "odd_w_out": [2, 1024, D],
    "gc_w2": [2, 2, 16, 512], "gc_b2": [2, 2, 512], "gc_norm": [2, 256], "router_w": [4, D, E],
    "exp_w_gate": [4, E, D, cfg.F], "exp_w_up": [4, E, D, cfg.F], "exp_w_down": [4, E, cfg.F, D],
}


def build(cfg):
    nc = bass.Bass("TRN2", target_bir_lowering=False)
    I = {}
    for k, shp in INPUT_SHAPES(cfg).items():
        I[k] = nc.dram_tensor(k, shp, F32, kind="ExternalInput")
    consts = make_consts(cfg)
    for k, v in consts.items():
        I[k] = nc.dram_tensor(k, list(v.shape), F32, kind="ExternalInput")
    out = nc.dram_tensor("out", [cfg.B, cfg.LL, D], F32, kind="ExternalOutput")
    dbg = {}
    if cfg.debug:
        dbg["mod"] = nc.dram_tensor("dbg_mod", [cfg.DEPTH, cfg.B + 1, 6 * D], F32, kind="ExternalOutput")
        dbg["y"] = nc.dram_tensor("dbg_y", [cfg.NTOK, D], F32, kind="ExternalOutput")
        dbg["xmid"] = nc.dram_tensor("dbg_xmid", [cfg.NTOK, D], F32, kind="ExternalOutput")
        dbg["ffn"] = nc.dram_tensor("dbg_ffn", [cfg.NTOK, D], F32, kind="ExternalOutput")
        dbg["o"] = nc.dram_tensor("dbg_o", [8, 128, cfg.NTOK], F32, kind="ExternalOutput")
    B, NT, NTC, NTOK, LC, LL = cfg.B, cfg.NT, cfg.NTC, cfg.NTOK, cfg.LC, cfg.LL

    with ExitStack() as st:
        em = Em(nc, st)
        banks = em.psum_banks()
        BK = [T(banks[i], "bk%d" % i) for i in range(8)]
        PF = [T(banks[i], "pf%d" % i, trk=BK[i]) for i in range(4)]
        PQ = [T(banks[4 + i % 4], "pq%d" % i, base=(i // 4) * 128, trk=BK[4 + i % 4]) for i in range(16)]
        rr = {"f": 0, "q": 0}

        def pfull():
            rr["f"] = (rr["f"] + 1) % 4
            return PF[rr["f"]]

        def pq():
            rr["q"] = (rr["q"] + 1) % 16
            return PQ[rr["q"]]

        XS = em.dram([B, NTOK, D], F32, "XS")
        MOD = em.dram([cfg.DEPTH, B + 1, 6 * D], F32, "MOD")
        YT = em.dram([8, 128, NTOK], BF16, "YT")
        XB = em.dram([NT, 128, D], BF16, "XB")
        RK = em.dram([E, NTOK], F32, "RK")
        OUT = T(out, "out")
        DBG = {k: T(v, "dbg_" + k) for k, v in dbg.items()}

        def cload(name, shape, dt=F32, src=None, q="sp"):
            t = em.sb(shape, dt, name)
            srcap = src if src is not None else I[name].ap()
            em.dma("pool" if dt == BF16 else q, t.h.ap(), srcap, writes=[t])
            return t
        ident = cload("c_ident", [128, 128])
        identb = cload("c_ident", [128, 128], BF16)
        iota = cload("c_iota", [128, 512])
        mfm = [cload("c_mfm", [128, 264], src=I["c_mfm"][d]) for d in range(2)]
        m3 = [cload("c_m3", [128, 128], src=I["c_m3"][d]) for d in range(2)]
        amask = [cload("c_mask", [128, 128], src=I["c_mask"][d]) for d in range(2)]
        bmask = cload("c_bm", [128, 4])
        tri = cload("c_tri", [128, 128])
        ones = cload("c_ones", [128, 128])
        rotb = cload("c_rot", [128, 128], BF16)
        misc = cload("c_misc", [128, 8])
        EPS_LN, EPS_RMS, ONE = misc[:, 0:1], misc[:, 1:2], misc[:, 2:3]

        with ExitStack() as ph:
            cT = em.sb([128, NCH, B + 1], F32, "cT", ph)
            for r in range(B):
                em.dma("sp", cT[:, :, r], I["c"][r].rearrange("(c p) -> p c", p=128), writes=[cT],
                       allow_slow_non_contiguous=True)
            em.dma("sp", cT[:, :, B], I["c_ctx"][0].rearrange("(c p) -> p c", p=128), writes=[cT],
                   allow_slow_non_contiguous=True)
            em.op("act", lambda e: e.activation(out=cT[:, :, :], in_=cT[:, :, :], func=AF.Silu), reads=[cT], writes=[cT])
            wbuf = [em.sb([128, NCH, 512], F32, "adaw", ph) for _ in range(2)]
            bb = [em.sb([B + 1, 512], F32, "adab", ph) for _ in range(2)]
            mo = [em.sb([B + 1, 512], F32, "modo", ph) for _ in range(2)]
            it = 0
            for l in range(cfg.DEPTH):
                for cp in range(12):
                    w = wbuf[it % 2]; bt = bb[it % 2]; m = mo[it % 2]
                    em.dma("sp", w[:, :, :], I["ada_w"][l, :, cp * 512:(cp + 1) * 512].rearrange("(c p) n -> p c n", p=128), writes=[w])
                    em.dma("sp", bt[:, :], I["ada_b"][l:l + 1, cp * 512:(cp + 1) * 512].to_broadcast([B + 1, 512]), writes=[bt])
                    p = pfull()
                    for ch in range(NCH):
                        em.op("pe", lambda e: e.matmul(p.c(0, 512, 0, B + 1), lhsT=cT[:, ch, :], rhs=w[:, ch, :], start=(ch == 0), stop=(ch == NCH - 1)),
                              reads=[cT, w], writes=[p])
                    em.op("dve", lambda e: e.tensor_tensor(out=m[:, :], in0=p.c(0, 512, 0, B + 1), in1=bt[:, :], op=ALU.add), reads=[p, bt], writes=[m])
                    em.dma("sp", MOD[l, :, cp * 512:(cp + 1) * 512], m[:, :], reads=[m], writes=[MOD])
                    it += 1
            if cfg.debug:
                em.dma("sp", DBG["mod"].h.ap(), MOD.h.ap(), reads=[MOD], writes=[DBG["mod"]])
            for s in range(B):
                em.dma("sp", XS[s, 0:LC, :], I["ctx"][s], writes=[XS])
                em.dma("sp", XS[s, LC:NTOK, :], I["x"][s], writes=[XS])
        em.barrier()

        def layernorm_tile(ph_tmp, u, lnw_bc, lnb_bc, dst):
            st2, junk = ph_tmp["st2"], ph_tmp["junk"]
            em.op("dve", lambda e: e.memset(st2[:, :], 0.0), writes=[st2])
            em.op("act", lambda e: e.activation(out=junk[:, :], in_=u[:, :], func=AF.Identity, accum_out=st2[:, 0:1]), reads=[u], writes=[junk, st2])
            em.op("act", lambda e: e.activation(out=junk[:, :], in_=u[:, :], func=AF.Square, accum_out=st2[:, 1:2]), reads=[u], writes=[junk, st2])
            em.op("dve", lambda e: e.tensor_scalar(out=st2[:, 2:3], in0=st2[:, 0:1], scalar1=1.0 / D, scalar2=None, op0=ALU.mult), reads=[st2], writes=[st2])
            em.op("dve", lambda e: e.tensor_tensor(out=st2[:, 3:4], in0=st2[:, 2:3], in1=st2[:, 2:3], op=ALU.mult), reads=[st2], writes=[st2])
            em.op("dve", lambda e: e.scalar_tensor_tensor(out=st2[:, 4:5], in0=st2[:, 1:2], scalar=1.0 / D, in1=st2[:, 3:4], op0=ALU.mult, op1=ALU.subtract), reads=[st2], writes=[st2])
            em.op("act", lambda e: e.activation(out=st2[:, 5:6], in_=st2[:, 4:5], func=AF.Sqrt, bias=EPS_LN, scale=1.0), reads=[st2, misc], writes=[st2])
            em.op("dve", lambda e: e.reciprocal(out=st2[:, 6:7], in_=st2[:, 5:6]), reads=[st2], writes=[st2])
            em.op("dve", lambda e: e.tensor_scalar(out=u[:, :], in0=u[:, :], scalar1=st2[:, 2:3], scalar2=st2[:, 6:7], op0=ALU.subtract, op1=ALU.mult), reads=[u, st2], writes=[u])
            em.op("dve", lambda e: e.tensor_tensor(out=u[:, :], in0=u[:, :], in1=lnw_bc[:, :], op=ALU.mult), reads=[u, lnw_bc], writes=[u])
            em.op("dve", lambda e: e.tensor_tensor(out=dst[:, :], in0=u[:, :], in1=lnb_bc[:, :], op=ALU.add), reads=[u, lnb_bc], writes=[dst])

        def load_modfm(ph, l, s):
            mf = em.sb([128, 2, 6, NCH], F32, "modfm", ph)
            for i, row in enumerate((s, B)):
                for six in range(6):
                    em.dma("sp", mf[:, i, six, :], MOD[l, row, six * D:(six + 1) * D].rearrange("(c p) -> p c", p=128), reads=[MOD], writes=[mf],
                           allow_slow_non_contiguous=True)
            for six in (1, 4):
                em.op("dve", lambda e: e.tensor_scalar(out=mf[:, :, six, :], in0=mf[:, :, six, :], scalar1=1.0, scalar2=None, op0=ALU.add), reads=[mf], writes=[mf])
            return mf

        def bc_load(t, src_row_ap, srcT):
            em.dma("sp", t[:, :], src_row_ap.to_broadcast([128, D]), reads=[srcT], writes=[t])

        def mixer_phase(s, l):
            even = (l % 2 == 0)
            jl = l // 2
            w_in = I["even_w_in"] if even else I["odd_w_in"]
            w_out = I["even_w_out"] if even else I["odd_w_out"]
            def ck(k_):
                if cfg.sub == k_:
                    em.muted = True
            with ExitStack() as ph:
                mixer_body(ph, s, l, even, jl, w_in, w_out, ck)
            if em.muted:
                em.muted = False
                em.barrier()
                return
            em.barrier()
            mixer_out(s, l, jl, w_out)

        def mixer_body(ph, s, l, even, jl, w_in, w_out, ck):
            if True:
                mf = load_modfm(ph, l, s)
                ck(0)
                hT = em.sb([128, NCH, NTOK], BF16, "hT", ph)
                xt = [em.sb([128, D], F32, "xt", ph) for _ in range(2)]
                for n in range(NT):
                    x_t = xt[n % 2]
                    em.dma("sp", x_t[:, :], XS[s, n * 128:(n + 1) * 128, :], reads=[XS], writes=[x_t])
                    mi = 1 if n < NTC else 0
                    import os
                    dbgm = os.environ.get("KSUB2", "c")
                    for ch in range(NCH):
                        if dbgm == "a":
                            continue
                        p = pq()
                        em.op("pe", lambda e: e.matmul(p.c(0, 128), lhsT=x_t[:, ch * 128:(ch + 1) * 128], rhs=ident[:, :], start=True, stop=True), reads=[x_t, ident], writes=[p])
                        if dbgm == "b":
                            continue
                        if dbgm == "d":
                            em.op("act", lambda e: e.activation(out=hT[:, ch, n * 128:(n + 1) * 128], in_=p.c(0, 128), func=AF.Identity), reads=[p, mf], writes=[hT])
                            continue
                        em.op("act", lambda e: e.activation(out=hT[:, ch, n * 128:(n + 1) * 128], in_=p.c(0, 128), func=AF.Identity,
                                                            scale=mf[:, mi, 1, ch:ch + 1], bias=mf[:, mi, 0, ch:ch + 1]), reads=[p, mf], writes=[hT])
                ck(1)
                wts = [em.sb([128, NCH, 256], BF16, "wcol", ph) for _ in range(3)]
                wrr = [0]

                def load_wcols(c0, ncol):
                    wrr[0] = (wrr[0] + 1) % 3
                    w = wts[wrr[0]]
                    em.dma("pool", w[:, :, 0:ncol], w_in[jl, :, c0:c0 + ncol].rearrange("(c p) n -> p c n", p=128), writes=[w])
                    return w

                def proj_fm(c0, M, evac):
                    w = load_wcols(c0, M)
                    for t0 in range(0, NTOK, 512):
                        nt = min(512, NTOK - t0)
                        p = pfull()
                        for ch in range(NCH):
                            em.op("pe", lambda e: e.matmul(p.c(0, nt, 0, M), lhsT=w[:, ch, 0:M], rhs=hT[:, ch, t0:t0 + nt], start=(ch == 0), stop=(ch == NCH - 1)),
                                  reads=[w, hT], writes=[p])
                        evac(p, t0, nt)

                def proj_tm(c0, ncol, dst):
                    w = load_wcols(c0, ncol)
                    for n in range(NT):
                        p = pfull()
                        for ch in range(NCH):
                            em.op("pe", lambda e: e.matmul(p.c(0, ncol), lhsT=hT[:, ch, n * 128:(n + 1) * 128], rhs=w[:, ch, 0:ncol], start=(ch == 0), stop=(ch == NCH - 1)),
                                  reads=[w, hT], writes=[p])
                        em.op("act", lambda e: e.activation(out=dst[:, n, 0:ncol], in_=p.c(0, ncol), func=AF.Copy), reads=[p], writes=[dst])

                prm = em.sb([128, 40], F32, "prm", ph)
                if even:
                    cosT = em.sb([128, LL], F32, "cosT", ph)
                    sinT = em.sb([128, LL], F32, "sinT", ph)
                    em.dma("sp", cosT.h.ap(), I["c_cos"].ap(), writes=[cosT])
                    em.dma("sp", sinT.h.ap(), I["c_sin"].ap(), writes=[sinT])
                    lbc, oml = prm[:, 0:8], prm[:, 8:16]
                    if jl == 0:
                        em.op("dve", lambda e: e.memset(prm[:, 0:8], 0.0), writes=[prm])
                    else:
                        em.dma("sp", prm[:, 0:8], I["ha_lb"][1].rearrange("d (h k) -> k (d h)", k=128), writes=[prm], allow_slow_non_contiguous=True)
                        em.dma("sp", prm[:, 8:16], I["ha_lb"][0].rearrange("d (h k) -> k (d h)", k=128), writes=[prm], allow_slow_non_contiguous=True)
                        em.op("dve", lambda e: e.tensor_tensor(out=prm[:, 0:8], in0=prm[:, 0:8], in1=prm[:, 8:16], op=ALU.subtract), reads=[prm], writes=[prm])
                        em.op("act", lambda e: e.activation(out=prm[:, 0:8], in_=prm[:, 0:8], func=AF.Sigmoid), reads=[prm], writes=[prm])
                    em.op("dve", lambda e: e.tensor_scalar(out=prm[:, 8:16], in0=prm[:, 0:8], scalar1=-1.0, scalar2=1.0, op0=ALU.mult, op1=ALU.add), reads=[prm], writes=[prm])
                    em.op("dve", lambda e: e.tensor_scalar(out=prm[:, 0:8], in0=prm[:, 0:8], scalar1=1e-30, scalar2=None, op0=ALU.max), reads=[prm], writes=[prm])
                    em.dma("sp", prm[:, 16:24], I["rb_decay"][jl:jl + 1, :].to_broadcast([128, 8]), writes=[prm])
                    em.op("act", lambda e: e.activation(out=prm[:, 16:24], in_=prm[:, 16:24], func=AF.Exp, scale=-1.0), reads=[prm], writes=[prm])
                    em.op("act", lambda e: e.activation(out=prm[:, 16:24], in_=prm[:, 16:24], func=AF.Ln, bias=ONE, scale=1.0), reads=[prm, misc], writes=[prm])
                    em.op("dve", lambda e: e.tensor_scalar(out=prm[:, 16:24], in0=prm[:, 16:24], scalar1=-1.0, scalar2=None, op0=ALU.mult), reads=[prm], writes=[prm])
                    em.dma("sp", prm[:, 24:25], I["ha_norm"][jl].rearrange("(k o) -> k o", o=1), writes=[prm], allow_slow_non_contiguous=True)
                    em.op("dve", lambda e: e.memset(prm[:, 25:26], 1.0), writes=[prm])
                else:
                    em.dma("sp", prm[:, 0:8], I["gc_b2"][jl].rearrange("d (h k) -> k (d h)", k=128), writes=[prm], allow_slow_non_contiguous=True)
                    em.op("dve", lambda e: e.tensor_scalar(out=prm[:, 0:8], in0=prm[:, 0:8], scalar1=-1.0, scalar2=None, op0=ALU.mult), reads=[prm], writes=[prm])
                    em.dma("sp", prm[:, 24:26], I["gc_norm"][jl].rearrange("(c k) -> k c", k=128), writes=[prm], allow_slow_non_contiguous=True)
                    w2 = em.sb([16, 2, 512], F32, "w2", ph)
                    em.dma("sp", w2[:, :, :], I["gc_w2"][jl].rearrange("d r k -> r d k"), writes=[w2])

                ck(2)
                nvc = 1 if even else 2
                dv = 128 * nvc
                qT = em.sb([128, NTOK], BF16, "qT", ph)
                kT = em.sb([128, NTOK], BF16, "kT", ph)
                gT = em.sb([128, NTOK], F32, "gT", ph)
                vtm = em.sb([128, NT, dv], BF16, "vtm", ph)
                sgT = em.sb([128, nvc, NTOK], BF16, "sgT", ph)
                obuf = em.sb([128, nvc, NTOK], F32, "obuf", ph)
                S = em.sb([128, dv], F32, "S", ph)
                Sb = em.sb([128, dv], BF16, "Sb", ph)
                tmpE = [em.sb([128, 512], F32, "tmpE", ph) for _ in range(3)]
                lrT = None if even else em.sb([16, NTOK], F32, "lrT", ph)
                def dbl(shape, dt, name):
                    return [em.sb(shape, dt, name, ph) for _ in range(2)]
                gtm = dbl([128, 128], F32, "gtm")
                X1 = dbl([128, 128], F32, "X1"); X1i = dbl([128, 128], F32, "X1i"); X2 = dbl([128, 128], F32, "X2")
                X3 = dbl([128, 128], F32, "X3"); X4 = dbl([128, 4], F32, "X4"); Ecl = dbl([128, 128], F32, "Ecl")
                Qt = dbl([128, 128], BF16, "Qt"); Kt = dbl([128, 128], BF16, "Kt"); Qs = dbl([128, 128], BF16, "Qs")
                Kom = dbl([128, NSB, 128], BF16, "Kom"); At = dbl([128, 128], BF16, "At")
                ofin = dbl([128, nvc, 128], F32, "ofin"); sq = dbl([128, nvc, 128], F32, "sq")
                rstd = dbl([128, 128], F32, "rstd"); ytile = dbl([128, nvc, 128], BF16, "ytile")
                XR = {}

                def exp_tiles(gsrc_tm, d, bi):
                    pf = pfull()
                    em.op("pe", lambda e: e.matmul(pf.c(0, 264), lhsT=gsrc_tm[:, :], rhs=mfm[d][:, :], start=True, stop=True), reads=[gsrc_tm, mfm[d]], writes=[pf])
                    p3 = pq()
                    em.op("pe", lambda e: e.matmul(p3.c(0, 128), lhsT=m3[d][:, :], rhs=gsrc_tm[:, :], start=True, stop=True), reads=[gsrc_tm, m3[d]], writes=[p3])
                    em.op("dve", lambda e: e.tensor_scalar(out=Ecl[bi][:, :], in0=pf.c(0, 128), scalar1=-CLAMP, scalar2=CLAMP, op0=ALU.max, op1=ALU.min), reads=[pf], writes=[Ecl[bi]])
                    em.op("act", lambda e: e.activation(out=X1[bi][:, :], in_=Ecl[bi][:, :], func=AF.Exp), reads=[Ecl[bi]], writes=[X1[bi]])
                    em.op("act", lambda e: e.activation(out=X1i[bi][:, :], in_=Ecl[bi][:, :], func=AF.Exp, scale=-1.0), reads=[Ecl[bi]], writes=[X1i[bi]])
                    em.op("act", lambda e: e.activation(out=X2[bi][:, :], in_=pf.c(128, 256), func=AF.Exp), reads=[pf], writes=[X2[bi]])
                    em.op("act", lambda e: e.activation(out=X4[bi][:, :], in_=pf.c(256, 260), func=AF.Exp), reads=[pf], writes=[X4[bi]])
                    em.op("act", lambda e: e.activation(out=X3[bi][:, :], in_=p3.c(0, 128), func=AF.Exp), reads=[p3], writes=[X3[bi]])

                def scan_dir(d, const_x, post):
                    em.op("dve", lambda e: e.memset(S[:, :], 0.0), writes=[S])
                    em.op("pool", lambda e: e.memset(Sb[:, :], 0.0), writes=[Sb])
                    if d == 0:
                        order = list(range(NT))
                    else:
                        order = list(range(NTC - 1, -1, -1)) + list(range(NT - 1, NTC - 1, -1))
                    for it_, n in enumerate(order):
                        bi = it_ % 2
                        cs = slice(n * 128, (n + 1) * 128)
                        if const_x is None:
                            pg = pq()
                            em.op("pe", lambda e: e.matmul(pg.c(0, 128), lhsT=gT[:, cs], rhs=ident[:, :], start=True, stop=True), reads=[gT, ident], writes=[pg])
                            em.op("act", lambda e: e.activation(out=gtm[bi][:, :], in_=pg.c(0, 128), func=AF.Copy), reads=[pg], writes=[gtm[bi]])
                            exp_tiles(gtm[bi], d, bi)
                            x1, x1i, x2, x3, x4 = X1[bi], X1i[bi], X2[bi], X3[bi], X4[bi]
                        else:
                            x1, x1i, x2, x3, x4 = const_x
                        em.op("dve", lambda e: e.tensor_tensor(out=Qt[bi][:, :], in0=qT[:, cs], in1=x1[:, :], op=ALU.mult), reads=[qT, x1], writes=[Qt[bi]])
                        em.op("dve", lambda e: e.tensor_tensor(out=Kt[bi][:, :], in0=kT[:, cs], in1=x1i[:, :], op=ALU.mult), reads=[kT, x1i], writes=[Kt[bi]])
                        em.op("dve", lambda e: e.tensor_tensor(out=Qs[bi][:, :], in0=qT[:, cs], in1=x2[:, :], op=ALU.mult), reads=[qT, x2], writes=[Qs[bi]])
                        pk = pq()
                        em.op("pe", lambda e: e.matmul(pk.c(0, 128), lhsT=kT[:, cs], rhs=identb[:, :], start=True, stop=True), reads=[kT, identb], writes=[pk])
                        for a in range(NSB):
                            em.op("dve", lambda e: e.scalar_tensor_tensor(out=Kom[bi][:, a, :], in0=pk.c(0, 128), scalar=bmask[:, a:a + 1], in1=x3[:, :], op0=ALU.mult, op1=ALU.mult),
                                  reads=[pk, bmask, x3], writes=[Kom[bi]])
                        pa = pq()
                        em.op("pe", lambda e: e.matmul(pa.c(0, 128), lhsT=Kt[bi][:, :], rhs=Qt[bi][:, :], start=True, stop=True), reads=[Kt[bi], Qt[bi]], writes=[pa])
                        em.op("dve", lambda e: e.tensor_tensor(out=At[bi][:, :], in0=pa.c(0, 128), in1=amask[d][:, :], op=ALU.mult), reads=[pa, amask[d]], writes=[At[bi]])
                        po = [pq() for _ in range(nvc)]
                        blocks = range(NSB) if d == 0 else range(NSB - 1, -1, -1)
                        for a in blocks:
                            c0, c1 = a * SUB, (a + 1) * SUB
                            for c in range(nvc):
                                em.op("pe", lambda e: e.matmul(po[c].c(c0, c1), lhsT=vtm[:, n, c * 128:(c + 1) * 128], rhs=At[bi][:, c0:c1], start=True, stop=False),
                                      reads=[vtm, At[bi]], writes=[po[c]])
                                em.op("pe", lambda e: e.matmul(po[c].c(c0, c1), lhsT=Sb[:, c * 128:(c + 1) * 128], rhs=Qs[bi][:, c0:c1], start=False, stop=True),
                                      reads=[Sb, Qs[bi]], writes=[po[c]])
                            pst = pfull()
                            em.op("pe", lambda e: e.matmul(pst.c(0, dv), lhsT=Kom[bi][:, a, :], rhs=vtm[:, n, :], start=True, stop=True), reads=[Kom[bi], vtm], writes=[pst])
                            em.op("dve", lambda e: e.scalar_tensor_tensor(out=S[:, :], in0=S[:, :], scalar=x4[:, a:a + 1], in1=pst.c(0, dv), op0=ALU.mult, op1=ALU.add),
                                  reads=[S, x4, pst], writes=[S])
                            em.op("act", lambda e: e.activation(out=Sb[:, :], in_=S[:, :], func=AF.Copy), reads=[S], writes=[Sb])
                        if d == 1:
                            for c in range(nvc):
                                em.op("act", lambda e: e.activation(out=obuf[:, c, cs], in_=po[c].c(0, 128), func=AF.Copy), reads=[po[c]], writes=[obuf])
                        else:
                            for c in range(nvc):
                                em.op("dve", lambda e: e.tensor_tensor(out=ofin[bi][:, c, :], in0=po[c].c(0, 128), in1=obuf[:, c, cs], op=ALU.add), reads=[po[c], obuf], writes=[ofin[bi]])
                            post(n, bi)

                def run_group(gi, normcol, ech0, const_x_by_dir=None, prep_dir=None):
                    def post(n, bi):
                        cs = slice(n * 128, (n + 1) * 128)
                        if cfg.debug and l == 0 and s == 0:
                            for c in range(nvc):
                                em.dma("sp", DBG["o"][ech0 + c, :, cs], ofin[bi][:, c, :], reads=[ofin[bi]], writes=[DBG["o"]])
                        em.op("act", lambda e: e.activation(out=sq[bi][:, :, :], in_=ofin[bi][:, :, :], func=AF.Square), reads=[ofin[bi]], writes=[sq[bi]])
                        pss = pq()
                        for c in range(nvc):
                            em.op("pe", lambda e: e.matmul(pss.c(0, 128), lhsT=ones[:, :], rhs=sq[bi][:, c, :], start=(c == 0), stop=(c == nvc - 1)), reads=[ones, sq[bi]], writes=[pss])
                        em.op("act", lambda e: e.activation(out=rstd[bi][:, :], in_=pss.c(0, 128), func=AF.Ln, bias=EPS_RMS, scale=1.0 / dv), reads=[pss, misc], writes=[rstd[bi]])
                        em.op("act", lambda e: e.activation(out=rstd[bi][:, :], in_=rstd[bi][:, :], func=AF.Exp, scale=-0.5), reads=[rstd[bi]], writes=[rstd[bi]])
                        for c in range(nvc):
                            em.op("dve", lambda e: e.tensor_tensor(out=ofin[bi][:, c, :], in0=ofin[bi][:, c, :], in1=rstd[bi][:, :], op=ALU.mult), reads=[ofin[bi], rstd[bi]], writes=[ofin[bi]])
                            em.op("dve", lambda e: e.scalar_tensor_tensor(out=ytile[bi][:, c, :], in0=ofin[bi][:, c, :], scalar=normcol[:, c:c + 1], in1=sgT[:, c, cs], op0=ALU.mult, op1=ALU.mult),
                                  reads=[ofin[bi], prm, sgT], writes=[ytile[bi]])
                            em.dma("sp", YT[ech0 + c, :, cs], ytile[bi][:, c, :], reads=[ytile[bi]], writes=[YT])
                    for d in (1, 0):
                        if prep_dir is not None:
                            prep_dir(d)
                        ck(4)
                        scan_dir(d, None if const_x_by_dir is None else const_x_by_dir[d], post)

                def evac_copy(dst, scale=1.0):
                    def f(p, t0, nt):
                        em.op("act", lambda e: e.activation(out=dst[:, t0:t0 + nt], in_=p.c(0, nt), func=AF.Identity, scale=scale), reads=[p], writes=[dst])
                    return f

                def evac_silu(c):
                    def f(p, t0, nt):
                        em.op("act", lambda e: e.activation(out=sgT[:, c, t0:t0 + nt], in_=p.c(0, nt), func=AF.Silu), reads=[p], writes=[sgT])
                    return f

                if even:
                    for h in range(4):
                        proj_fm(0 + h * 128, 128, evac_copy(qT, 128 ** -0.5))
                        proj_tm(1536 + h * 128, 128, vtm)
                        proj_fm(2048 + h * 128, 128, evac_silu(0))
                        ck(3)

                        def prep_dir(d, h=h):
                            idx = d * 4 + h
                            def ev(p, t0, nt):
                                Et, L1, L2 = tmpE
                                em.op("act", lambda e: e.activation(out=Et[:, 0:nt], in_=p.c(0, nt), func=AF.Exp), reads=[p], writes=[Et])
                                em.op("act", lambda e: e.activation(out=L1[:, 0:nt], in_=Et[:, 0:nt], func=AF.Ln, bias=prm[:, idx:idx + 1], scale=1.0), reads=[Et, prm], writes=[L1])
                                em.op("act", lambda e: e.activation(out=L2[:, 0:nt], in_=Et[:, 0:nt], func=AF.Ln, bias=ONE, scale=1.0), reads=[Et, misc], writes=[L2])
                                em.op("dve", lambda e: e.tensor_tensor(out=gT[:, t0:t0 + nt], in0=L1[:, 0:nt], in1=L2[:, 0:nt], op=ALU.subtract), reads=[L1, L2], writes=[gT])
                                em.op("dve", lambda e: e.tensor_scalar(out=Et[:, 0:nt], in0=Et[:, 0:nt], scalar1=1.0, scalar2=None, op0=ALU.add), reads=[Et], writes=[Et])
                                em.op("dve", lambda e: e.reciprocal(out=Et[:, 0:nt], in_=Et[:, 0:nt]), reads=[Et], writes=[Et])
                                em.op("dve", lambda e: e.tensor_scalar(out=kT[:, t0:t0 + nt], in0=Et[:, 0:nt], scalar1=prm[:, 8 + idx:9 + idx], scalar2=None, op0=ALU.mult), reads=[Et, prm], writes=[kT])
                            proj_fm((512 if d == 0 else 1024) + h * 128, 128, ev)
                        run_group(h, prm[:, 24:25], h, None, prep_dir)
                        ck(5)
                    ck(6)
                    q0 = tmpE
                    for h in range(4):
                        def rope_evac(dst, scale):
                            def f(p, t0, nt):
                                em.op("act", lambda e: e.activation(out=dst[:, t0:t0 + nt], in_=p.c(0, nt), func=AF.Identity, scale=scale), reads=[p], writes=[dst])
                                a0 = max(t0, LC); a1 = t0 + nt
                                if a1 <= a0:
                                    return
                                w_ = a1 - a0
                                pr = pfull()
                                em.op("pe", lambda e: e.matmul(pr.c(0, w_), lhsT=rotb[:, :], rhs=dst[:, a0:a1], start=True, stop=True), reads=[rotb, dst], writes=[pr])
                                t1, t2 = tmpE[0], tmpE[1]
                                em.op("dve", lambda e: e.tensor_tensor(out=t1[:, 0:w_], in0=dst[:, a0:a1], in1=cosT[:, a0 - LC:a1 - LC], op=ALU.mult), reads=[dst, cosT], writes=[t1])
                                em.op("dve", lambda e: e.tensor_tensor(out=t2[:, 0:w_], in0=pr.c(0, w_), in1=sinT[:, a0 - LC:a1 - LC], op=ALU.mult), reads=[pr, sinT], writes=[t2])
                                em.op("dve", lambda e: e.tensor_tensor(out=dst[:, a0:a1], in0=t1[:, 0:w_], in1=t2[:, 0:w_], op=ALU.add), reads=[t1, t2], writes=[dst])
                            return f
                        proj_fm(2560 + h * 128, 128, rope_evac(qT, 128 ** -0.5))
                        proj_fm(3072 + h * 128, 128, rope_evac(kT, 1.0))
                        proj_tm(3584 + h * 128, 128, vtm)
                        proj_fm(4096 + h * 128, 128, evac_silu(0))
                        cx = {}
                        for d in (0, 1):
                            idx = 16 + d * 4 + h
                            em.op("dve", lambda e: e.tensor_scalar(out=gtm[d][:, :], in0=ones[:, :], scalar1=prm[:, idx:idx + 1], scalar2=None, op0=ALU.mult), reads=[ones, prm], writes=[gtm[d]])
                            if (h, d) not in XR:
                                XR[(h, d)] = None
                            exp_tiles(gtm[d], d, d)
                            tl = [em.sb([128, 128], F32, "xr", ph) for _ in range(4)] + [em.sb([128, 4], F32, "xr4", ph)] if XR.get("bufs%d" % d) is None else XR["bufs%d" % d]
                            XR["bufs%d" % d] = tl
                            for src, dst_ in zip((X1[d], X1i[d], X2[d], X3[d], X4[d]), tl):
                                em.op("pool", lambda e: e.tensor_copy(out=dst_.h.ap(), in_=src.h.ap()), reads=[src], writes=[dst_])
                            cx[d] = tl
                        run_group(4 + h, prm[:, 25:26], 4 + h, cx, None)
                        ck(7)
                    ck(98)
                else:
                    for h in range(4):
                        proj_fm(0 + h * 128, 128, evac_copy(qT, 128 ** -0.5))
                        proj_fm(512 + h * 128, 128, evac_copy(kT, 1.0))
                        proj_tm(1024 + h * 256, 256, vtm)
                        for c in range(2):
                            proj_fm(2048 + h * 256 + c * 128, 128, evac_silu(c))

                        def prep_dir(d, h=h):
                            idx = d * 4 + h
                            def evl(p, t0, nt):
                                em.op("act", lambda e: e.activation(out=lrT[:, t0:t0 + nt], in_=p.c(0, nt, 0, 16), func=AF.Copy), reads=[p], writes=[lrT])
                            proj_fm(3072 + d * 16, 16, evl)
                            for t0 in range(0, NTOK, 512):
                                nt = min(512, NTOK - t0)
                                p = pfull()
                                em.op("pe", lambda e: e.matmul(p.c(0, nt), lhsT=w2[:, d, h * 128:(h + 1) * 128], rhs=lrT[:, t0:t0 + nt], start=True, stop=True), reads=[w2, lrT], writes=[p])
                                Et, L1 = tmpE[0], tmpE[1]
                                em.op("act", lambda e: e.activation(out=Et[:, 0:nt], in_=p.c(0, nt), func=AF.Exp, scale=-1.0, bias=prm[:, idx:idx + 1]), reads=[p, prm], writes=[Et])
                                em.op("act", lambda e: e.activation(out=L1[:, 0:nt], in_=Et[:, 0:nt], func=AF.Ln, bias=ONE, scale=1.0), reads=[Et, misc], writes=[L1])
                                em.op("dve", lambda e: e.tensor_scalar(out=gT[:, t0:t0 + nt], in0=L1[:, 0:nt], scalar1=-1.0 / GC_TAU, scalar2=None, op0=ALU.mult), reads=[L1], writes=[gT])
                        run_group(h, prm[:, 24:26], 2 * h, None, prep_dir)

        def mixer_out(s, l, jl, w_out):
            def ck(k_):
                if cfg.sub == k_:
                    em.muted = True
            with ExitStack() as ph:
                wo = em.sb([128, NCH, D], BF16, "wo", ph)
                em.dma("pool", wo[:, :, :], w_out[jl].rearrange("(c p) n -> p c n", p=128), writes=[wo])
                g1bc = [em.sb([128, D], F32, "g1bc", ph) for _ in range(2)]
                lnw = em.sb([128, D], F32, "lnw", ph); lnb = em.sb([128, D], F32, "lnb", ph)
                bc_load(g1bc[0], MOD[l, s:s + 1, 2 * D:3 * D], MOD)
                bc_load(g1bc[1], MOD[l, B:B + 1, 2 * D:3 * D], MOD)
                em.dma("sp", lnw[:, :], I["ln_w"][l, 0:1, :].to_broadcast([128, D]), writes=[lnw])
                em.dma("sp", lnb[:, :], I["ln_b"][l, 0:1, :].to_broadcast([128, D]), writes=[lnb])
                tmp = {"st2": em.sb([128, 8], F32, "st2", ph), "junk": em.sb([128, D], F32, "junk", ph)}
                yts = [em.sb([128, NCH, 128], BF16, "yts", ph) for _ in range(2)]
                xts = [em.sb([128, D], F32, "xts", ph) for _ in range(2)]
                us = [em.sb([128, D], F32, "us", ph) for _ in range(2)]
                ck(101)
                for n in range(NT):
                    bi = n % 2
                    cs = slice(n * 128, (n + 1) * 128)
                    em.dma("sp", yts[bi][:, :, :], YT[:, :, cs].rearrange("c p t -> p c t"), reads=[YT], writes=[yts[bi]])
                    em.dma("sp", xts[bi][:, :], XS[s, cs, :], reads=[XS], writes=[xts[bi]])
                    ck(102)
                    gb = g1bc[1 if n < NTC else 0]
                    for dh in range(2):
                        p = pfull()
                        for ch in range(NCH):
                            em.op("pe", lambda e: e.matmul(p.c(0, 512), lhsT=yts[bi][:, ch, :], rhs=wo[:, ch, dh * 512:(dh + 1) * 512], start=(ch == 0), stop=(ch == NCH - 1)),
                                  reads=[yts[bi], wo], writes=[p])
                        if cfg.debug and l == 0 and s == 0:
                            em.op("act", lambda e: e.activation(out=tmp["junk"][:, dh * 512:(dh + 1) * 512], in_=p.c(0, 512), func=AF.Copy), reads=[p], writes=[tmp["junk"]])
                        em.op("dve", lambda e: e.tensor_tensor(out=us[bi][:, dh * 512:(dh + 1) * 512], in0=p.c(0, 512), in1=gb[:, dh * 512:(dh + 1) * 512], op=ALU.mult), reads=[p, gb], writes=[us[bi]])
                    if cfg.debug and l == 0 and s == 0:
                        em.dma("sp", DBG["y"][cs, :], tmp["junk"][:, :], reads=[tmp["junk"]], writes=[DBG["y"]])
                    ck(103)
                    em.op("dve", lambda e: e.scalar_tensor_tensor(out=us[bi][:, :], in0=xts[bi][:, :], scalar=DN_ALPHA, in1=us[bi][:, :], op0=ALU.mult, op1=ALU.add), reads=[xts[bi], us[bi]], writes=[us[bi]])
                    ck(104)
                    layernorm_tile(tmp, us[bi], lnw, lnb, xts[bi])
                    ck(105)
                    em.dma("sp", XS[s, cs, :], xts[bi][:, :], reads=[xts[bi]], writes=[XS])
                    if cfg.debug and l == 0 and s == 0:
                        em.dma("sp", DBG["xmid"][cs, :], xts[bi][:, :], reads=[xts[bi]], writes=[DBG["xmid"]])
            em.muted = False
            em.barrier()

        def ffn_phase(s, l):
            last = (l == cfg.DEPTH - 1)
            NS, capL, capC, NF, Fd = cfg.NS, cfg.capL, cfg.capC, cfg.NF, cfg.F
            ctiles = [(c0, min(128, NS - c0)) for c0 in range(0, NS, 128)]
            with ExitStack() as ph0:
                yacc = em.sb([128, NT, D], F32, "yacc", ph0)
                aff = em.sb([128, NT, E], F32, "aff", ph0)
                rkg = em.sb([128, NT, E], F32, "rkg", ph0)
                ph = ExitStack()
                bcs = [em.sb([128, D], F32, "bc", ph) for _ in range(2)]
                msk = em.sb([128, NT, E], F32, "msk", ph)
                affT = em.sb([E, NTOK], F32, "affT", ph)
                wrk = em.sb([E, NTOK], F32, "wrk", ph)
                mx8 = em.sb([E, 8], F32, "mx8", ph)
                wr = em.sb([128, NCH, E], F32, "wr", ph)
                xts = [em.sb([128, D], F32, "xts", ph) for _ in range(2)]
                x2f = [em.sb([128, D], F32, "x2f", ph) for _ in range(2)]
                x2b = [em.sb([128, D], BF16, "x2b", ph) for _ in range(2)]
                x2T = [em.sb([128, NCH, 128], F32, "x2T", ph) for _ in range(2)]
                sm = [em.sb([128, 4], F32, "sm", ph) for _ in range(2)]
                cum = em.sb([128, E], F32, "cum", ph)
                em.dma("sp", wr[:, :, :], I["router_w"][l].rearrange("(c p) n -> p c n", p=128), writes=[wr])
                em.op("pool", lambda e: e.memset(yacc[:, :, :], 0.0), writes=[yacc])
                for n in range(NT):
                    bi = n % 2
                    cs = slice(n * 128, (n + 1) * 128)
                    if n == 0 or n == NTC:
                        row = B if n < NTC else s
                        bc_load(bcs[0], MOD[l, row:row + 1, 4 * D:5 * D], MOD)
                        em.op("dve", lambda e: e.tensor_scalar(out=bcs[0][:, :], in0=bcs[0][:, :], scalar1=1.0, scalar2=None, op0=ALU.add), reads=[bcs[0]], writes=[bcs[0]])
                        bc_load(bcs[1], MOD[l, row:row + 1, 3 * D:4 * D], MOD)
                    em.dma("sp", xts[bi][:, :], XS[s, cs, :], reads=[XS], writes=[xts[bi]])
                    em.op("dve", lambda e: e.tensor_tensor(out=x2f[bi][:, :], in0=xts[bi][:, :], in1=bcs[0][:, :], op=ALU.mult), reads=[xts[bi], bcs[0]], writes=[x2f[bi]])
                    em.op("dve", lambda e: e.tensor_tensor(out=x2f[bi][:, :], in0=x2f[bi][:, :], in1=bcs[1][:, :], op=ALU.add), reads=[x2f[bi], bcs[1]], writes=[x2f[bi]])
                    em.op("act", lambda e: e.activation(out=x2b[bi][:, :], in_=x2f[bi][:, :], func=AF.Copy), reads=[x2f[bi]], writes=[x2b[bi]])
                    em.dma("sp", XB[n, :, :], x2b[bi][:, :], reads=[x2b[bi]], writes=[XB])
                    for ch in range(NCH):
                        p = pq()
                        em.op("pe", lambda e: e.matmul(p.c(0, 128), lhsT=x2f[bi][:, ch * 128:(ch + 1) * 128], rhs=ident[:, :], start=True, stop=True), reads=[x2f[bi], ident], writes=[p])
                        em.op("act", lambda e: e.activation(out=x2T[bi][:, ch, :], in_=p.c(0, 128), func=AF.Copy), reads=[p], writes=[x2T[bi]])
                    pl = pq()
                    for ch in range(NCH):
                        em.op("pe", lambda e: e.matmul(pl.c(0, E), lhsT=x2T[bi][:, ch, :], rhs=wr[:, ch, :], start=(ch == 0), stop=(ch == NCH - 1)), reads=[x2T[bi], wr], writes=[pl])
                    smt = sm[bi]
                    em.op("dve", lambda e: e.tensor_reduce(out=smt[:, 0:1], in_=pl.c(0, E), axis=mybir.AxisListType.X, op=ALU.max), reads=[pl], writes=[smt])
                    em.op("dve", lambda e: e.tensor_scalar(out=smt[:, 1:2], in0=smt[:, 0:1], scalar1=-1.0, scalar2=None, op0=ALU.mult), reads=[smt], writes=[smt])
                    em.op("dve", lambda e: e.memset(smt[:, 2:3], 0.0), writes=[smt])
                    em.op("act", lambda e: e.activation(out=aff[:, n, :], in_=pl.c(0, E), func=AF.Exp, bias=smt[:, 1:2], scale=1.0, accum_out=smt[:, 2:3]), reads=[pl, smt], writes=[aff, smt])
                    em.op("dve", lambda e: e.reciprocal(out=smt[:, 3:4], in_=smt[:, 2:3]), reads=[smt], writes=[smt])
                    em.op("dve", lambda e: e.tensor_scalar(out=aff[:, n, :], in0=aff[:, n, :], scalar1=smt[:, 3:4], scalar2=None, op0=ALU.mult), reads=[aff, smt], writes=[aff])
                    pt_ = pq()
                    em.op("pe", lambda e: e.matmul(pt_.c(0, 128, 0, E), lhsT=aff[:, n, :], rhs=ident[:, :], start=True, stop=True), reads=[aff, ident], writes=[pt_])
                    em.op("act", lambda e: e.activation(out=affT[:, cs], in_=pt_.c(0, 128, 0, E), func=AF.Copy), reads=[pt_], writes=[affT])
                for (t0, n_, cap, off, nt0, ntn) in ((0, LC, capC, capL, 0, NTC), (LC, LL, capL, 0, NTC, NT)):
                    em.op("dve", lambda e: e.tensor_copy(out=wrk[:, t0:t0 + n_], in_=affT[:, t0:t0 + n_]), reads=[affT], writes=[wrk])
                    for r in range(cap // 8):
                        em.op("dve", lambda e: e.max(out=mx8[:, :], in_=wrk[:, t0:t0 + n_]), reads=[wrk], writes=[mx8])
                        if r < cap // 8 - 1:
                            em.op("dve", lambda e: e.match_replace(out=wrk[:, t0:t0 + n_], in_to_replace=mx8[:, :], in_values=wrk[:, t0:t0 + n_], imm_value=-1e30), reads=[wrk, mx8], writes=[wrk])
                    em.op("dve", lambda e: e.tensor_scalar(out=wrk[:, t0:t0 + n_], in0=affT[:, t0:t0 + n_], scalar1=mx8[:, 7:8], scalar2=None, op0=ALU.is_ge), reads=[affT, mx8], writes=[wrk])
                    em.op("dve", lambda e: e.memset(cum[:, :], 0.0), writes=[cum])
                    for n in range(nt0, ntn):
                        cs = slice(n * 128, (n + 1) * 128)
                        pm = pq()
                        em.op("pe", lambda e: e.matmul(pm.c(0, E), lhsT=wrk[:, cs], rhs=ident[0:E, 0:E], start=True, stop=True), reads=[wrk, ident], writes=[pm])
                        em.op("act", lambda e: e.activation(out=msk[:, n, :], in_=pm.c(0, E), func=AF.Copy), reads=[pm], writes=[msk])
                        pr = pq()
                        em.op("pe", lambda e: e.matmul(pr.c(0, E), lhsT=tri[:, :], rhs=msk[:, n, :], start=True, stop=False), reads=[tri, msk], writes=[pr])
                        em.op("pe", lambda e: e.matmul(pr.c(0, E), lhsT=ones[:, :], rhs=cum[:, :], start=False, stop=True), reads=[ones, cum], writes=[pr])
                        em.op("dve", lambda e: e.scalar_tensor_tensor(out=rkg[:, n, :], in0=pr.c(0, E), scalar=float(off + 1), in1=msk[:, n, :], op0=ALU.add, op1=ALU.mult), reads=[pr, msk], writes=[rkg])
                        em.op("dve", lambda e: e.tensor_scalar(out=rkg[:, n, :], in0=rkg[:, n, :], scalar1=-1.0, scalar2=None, op0=ALU.add), reads=[rkg], writes=[rkg])
                        em.op("dve", lambda e: e.tensor_tensor(out=cum[:, :], in0=cum[:, :], in1=msk[:, n, :], op=ALU.add), reads=[cum, msk], writes=[cum])
                        pt_ = pq()
                        em.op("pe", lambda e: e.matmul(pt_.c(0, 128, 0, E), lhsT=rkg[:, n, :], rhs=ident[:, :], start=True, stop=True), reads=[rkg, ident], writes=[pt_])
                        em.op("act", lambda e: e.activation(out=affT[:, cs], in_=pt_.c(0, 128, 0, E), func=AF.Copy), reads=[pt_], writes=[affT])
                em.dma("sp", RK.h.ap(), affT.h.ap(), reads=[affT], writes=[RK])
                em.barrier()
                ph.close()

                ph = ExitStack()
                rbc = em.sb([128, NTOK], F32, "rbc", ph)
                PTs = [em.sb([128, NTOK], BF16, "PT", ph) for _ in ctiles]
                Pn = em.sb([128, NT, NS], BF16, "Pn", ph)
                xsT = em.sb([128, NCH, NS], BF16, "xsT", ph)
                hidT = em.sb([128, NF, NS], BF16, "hidT", ph)
                ys = [em.sb([128, D], BF16, "ys", ph) for _ in ctiles]
                FG = min(512, Fd)
                wg = [em.sb([128, NCH, FG], BF16, "wg", ph) for _ in range(2)]
                wuf = [em.sb([128, NCH, 128], F32, "wuf", ph) for _ in range(3)]
                wub = [em.sb([128, NCH, 128], BF16, "wub", ph) for _ in range(3)]
                wdf = [em.sb([128, D], F32, "wdf", ph) for _ in range(3)]
                wdb = [em.sb([128, D], BF16, "wdb", ph) for _ in range(3)]
                hs = [em.sb([128, NS], F32, "hs", ph) for _ in range(2)]
                xbt = [em.sb([128, D], BF16, "xbt", ph) for _ in range(2)]
                wi = [0]
                for ex in range(E):
                    em.dma("sp", rbc[:, :], RK[ex:ex + 1, :].to_broadcast([128, NTOK]), reads=[RK], writes=[rbc])
                    for ci, (c0, cw) in enumerate(ctiles):
                        em.op("dve", lambda e: e.tensor_scalar(out=PTs[ci][:, :], in0=rbc[:, :], scalar1=misc[:, 4 + ci:5 + ci], scalar2=None, op0=ALU.is_equal), reads=[rbc, misc], writes=[PTs[ci]])
                    for n in range(NT):
                        em.op("dve", lambda e: e.tensor_scalar(out=Pn[:, n, :], in0=iota[:, 0:NS], scalar1=rkg[:, n, ex:ex + 1], scalar2=None, op0=ALU.is_equal), reads=[iota, rkg], writes=[Pn])
                    for half in range(2):
                        pg = [PF[i] for i in range(4)]
                        for n in range(NT):
                            xb_ = xbt[n % 2]
                            em.dma("sp", xb_[:, :], XB[n, :, :], reads=[XB], writes=[xb_])
                            for k in range(4):
                                ch = half * 4 + k
                                em.op("pe", lambda e: e.matmul(pg[k].c(0, NS), lhsT=xb_[:, ch * 128:(ch + 1) * 128], rhs=Pn[:, n, :], start=(n == 0), stop=(n == NT - 1)), reads=[xb_, Pn], writes=[pg[k]])
                        for k in range(4):
                            ch = half * 4 + k
                            em.op("act", lambda e: e.activation(out=xsT[:, ch, :], in_=pg[k].c(0, NS), func=AF.Copy), reads=[pg[k]], writes=[xsT])
                    nacc = len(ctiles) * 2
                    assert nacc <= 6
                    pacc = [T(banks[i], "pacc%d" % i, trk=BK[i]) for i in range(nacc)]
                    pb = [T(banks[6], "pgate", trk=BK[6]), T(banks[7], "pup", trk=BK[7])]

                    def down_chunk(fi):
                        wb_ = wdb[fi % 3]
                        for ci, (c0, cw) in enumerate(ctiles):
                            for dh in range(2):
                                pa_ = pacc[ci * 2 + dh]
                                em.op("pe", lambda e: e.matmul(pa_.c(0, 512, 0, cw), lhsT=hidT[:, fi, c0:c0 + cw], rhs=wb_[:, dh * 512:(dh + 1) * 512], start=(fi == 0), stop=(fi == NF - 1)),
                                      reads=[hidT, wb_], writes=[pa_])
                    for f0 in range(0, Fd, FG):
                        fw = min(FG, Fd - f0)
                        wi[0] += 1
                        g_ = wg[wi[0] % 2]
                        em.dma("pool", g_[:, :, 0:fw], I["exp_w_gate"][l, ex, :, f0:f0 + fw].rearrange("(c p) f -> p c f", p=128), writes=[g_])
                        for fc in range(fw // 128):
                            fi = f0 // 128 + fc
                            wf_, wb_ = wdf[fi % 3], wdb[fi % 3]
                            em.dma("sp", wf_[:, :], I["exp_w_down"][l, ex, fi * 128:(fi + 1) * 128, :], writes=[wf_])
                            em.op("act", lambda e: e.activation(out=wb_[:, :], in_=wf_[:, :], func=AF.Copy), reads=[wf_], writes=[wb_])
                            uf_, u_ = wuf[fi % 3], wub[fi % 3]
                            em.dma("sp", uf_[:, :, :], I["exp_w_up"][l, ex, :, fi * 128:(fi + 1) * 128].rearrange("(c p) f -> p c f", p=128), writes=[uf_])
                            em.op("dve", lambda e: e.tensor_copy(out=u_[:, :, :], in_=uf_[:, :, :]), reads=[uf_], writes=[u_])
                            p1, p2 = pb
                            for ch in range(NCH):
                                em.op("pe", lambda e: e.matmul(p1.c(0, NS), lhsT=g_[:, ch, fc * 128:(fc + 1) * 128], rhs=xsT[:, ch, :], start=(ch == 0), stop=(ch == NCH - 1)), reads=[g_, xsT], writes=[p1])
                            for ch in range(NCH):
                                em.op("pe", lambda e: e.matmul(p2.c(0, NS), lhsT=u_[:, ch, :], rhs=xsT[:, ch, :], start=(ch == 0), stop=(ch == NCH - 1)), reads=[u_, xsT], writes=[p2])
                            h_ = hs[fi % 2]
                            em.op("act", lambda e: e.activation(out=h_[:, :], in_=p1.c(0, NS), func=AF.Silu), reads=[p1], writes=[h_])
                            em.op("dve", lambda e: e.tensor_tensor(out=hidT[:, fi, :], in0=h_[:, :], in1=p2.c(0, NS), op=ALU.mult), reads=[h_, p2], writes=[hidT])
                            if fi >= 1:
                                down_chunk(fi - 1)
                    down_chunk(NF - 1)
                    for ci, (c0, cw) in enumerate(ctiles):
                        for dh in range(2):
                            pa_ = pacc[ci * 2 + dh]
                            em.op("act", lambda e: e.activation(out=ys[ci][0:cw, dh * 512:(dh + 1) * 512], in_=pa_.c(0, 512, 0, cw), func=AF.Copy), reads=[pa_], writes=[ys[ci]])
                    for n in range(NT):
                        cs = slice(n * 128, (n + 1) * 128)
                        if n < NTC:
                            use = [ci for ci, (c0, cw) in enumerate(ctiles) if c0 + cw > capL]
                        else:
                            use = [ci for ci, (c0, cw) in enumerate(ctiles) if c0 < capL]
                        for dh in range(2):
                            p = pfull()
                            for k, ci in enumerate(use):
                                c0, cw = ctiles[ci]
                                em.op("pe", lambda e: e.matmul(p.c(0, 512), lhsT=PTs[ci][0:cw, cs], rhs=ys[ci][0:cw, dh * 512:(dh + 1) * 512], start=(k == 0), stop=(k == len(use) - 1)),
                                      reads=[PTs[ci], ys[ci]], writes=[p])
                            em.op("dve", lambda e: e.scalar_tensor_tensor(out=yacc[:, n, dh * 512:(dh + 1) * 512], in0=p.c(0, 512), scalar=aff[:, n, ex:ex + 1], in1=yacc[:, n, dh * 512:(dh + 1) * 512], op0=ALU.mult, op1=ALU.add),
                                  reads=[p, aff, yacc], writes=[yacc])
                em.barrier()
                ph.close()
                ph = ExitStack()
                bcs = [em.sb([128, D], F32, "bc", ph) for _ in range(3)]
                tmp = {"st2": em.sb([128, 8], F32, "st2", ph), "junk": em.sb([128, D], F32, "junk", ph)}
                xts = [em.sb([128, D], F32, "xts", ph) for _ in range(2)]
                x2f = [em.sb([128, D], F32, "x2f", ph) for _ in range(2)]
                em.dma("sp", bcs[1][:, :], I["ln_w"][l, 1:2, :].to_broadcast([128, D]), writes=[bcs[1]])
                em.dma("sp", bcs[2][:, :], I["ln_b"][l, 1:2, :].to_broadcast([128, D]), writes=[bcs[2]])
                for n in range(NT):
                    if last and n < NTC:
                        continue
                    bi = n % 2
                    cs = slice(n * 128, (n + 1) * 128)
                    if n == 0 or n == NTC or (last and n == NTC):
                        row = B if n < NTC else s
                        bc_load(bcs[0], MOD[l, row:row + 1, 5 * D:6 * D], MOD)
                    if cfg.debug and l == 0 and s == 0:
                        em.dma("sp", DBG["ffn"][cs, :], yacc[:, n, :], reads=[yacc], writes=[DBG["ffn"]])
                    em.dma("sp", xts[bi][:, :], XS[s, cs, :], reads=[XS], writes=[xts[bi]])
                    em.op("dve", lambda e: e.tensor_tensor(out=x2f[bi][:, :], in0=yacc[:, n, :], in1=bcs[0][:, :], op=ALU.mult), reads=[yacc, bcs[0]], writes=[x2f[bi]])
                    em.op("dve", lambda e: e.scalar_tensor_tensor(out=x2f[bi][:, :], in0=xts[bi][:, :], scalar=DN_ALPHA, in1=x2f[bi][:, :], op0=ALU.mult, op1=ALU.add), reads=[xts[bi], x2f[bi]], writes=[x2f[bi]])
                    layernorm_tile(tmp, x2f[bi], bcs[1], bcs[2], xts[bi])
                    if last:
                        em.dma("sp", OUT[s, (n - NTC) * 128:(n - NTC + 1) * 128, :], xts[bi][:, :], reads=[xts[bi]], writes=[OUT])
                    else:
                        em.dma("sp", XS[s, cs, :], xts[bi][:, :], reads=[xts[bi]], writes=[XS])
                em.barrier()
                ph.close()
            em.barrier()

        try:
            for s in range(B):
                for l in range(cfg.DEPTH):
                    if cfg.stage >= 2 + 2 * l:
                        mixer_phase(s, l)
                    if cfg.stage >= 3 + 2 * l:
                        ffn_phase(s, l)
        except Exception:
            import traceback
            traceback.print_exc()
            raise
        em.barrier()
        build.ninstr = em.n
    return nc, consts


_CACHE = {}


def run(cfg, inputs, ncores, trace=False):
    key = (cfg.B, cfg.LC, cfg.LL, cfg.F, cfg.DEPTH, cfg.debug, cfg.stage)
    if key not in _CACHE:
        _CACHE[key] = build(cfg)
    nc, consts = _CACHE[key]
    B = cfg.B
    in_maps = []
    shp = INPUT_SHAPES(cfg)
    for c in range(ncores):
        m = {}
        for k in shp:
            a = np.asarray(inputs[k], dtype=np.float32)
            if k in ("x", "c", "ctx"):
                a = a[c * B:(c + 1) * B]
            a = np.ascontiguousarray(a).reshape(shp[k])
            m[k] = a
        m.update(consts)
        in_maps.append(m)
    res = run_bass_kernel_spmd(nc, in_maps, core_ids=list(range(ncores)), trace=trace)
    return res


def kernel(**inputs):
    ncores = 8
    cfg = Cfg(B=32 // ncores)
    res = run(cfg, inputs, ncores)
    out = np.concatenate([r["out"] for r in res.results], axis=0)
    return out.astype(np.float32)
```

```python
import numpy as np
from contextlib import ExitStack
import concourse.bass as bass
import concourse.mybir as mybir
from concourse.bass_utils import run_bass_kernel_spmd

F32 = mybir.dt.float32
BF16 = mybir.dt.bfloat16
AF = mybir.ActivationFunctionType
ALU = mybir.AluOpType

D = 1024
NCH = 8
E = 16
SUB = 32
NSB = 4
LN_EPS = 1e-5
RMS_EPS = 1e-6
DN_ALPHA = 8.0 ** 0.25
GC_TAU = 16.0
CLAMP = 35.0
EVEN_IN = 4608
ODD_IN = 3104
SAME_ENGINE_SYNC = True


class Cfg:
    def __init__(self, B=4, LC=256, LL=2048, F=2816, DEPTH=4, debug=False, stage=99):
        self.B, self.LC, self.LL, self.F, self.DEPTH, self.debug = B, LC, LL, F, DEPTH, debug
        self.stage = stage
        self.sub = 99
        self.NTC, self.NTL = LC // 128, LL // 128
        self.NT = self.NTC + self.NTL
        self.NTOK = LC + LL
        self.capL, self.capC = LL // 8, LC // 8
        self.NS = self.capL + self.capC
        self.NF = F // 128


class Stop(Exception):
    pass


class T:
    __slots__ = ("h", "lw", "rd", "name", "base", "trk")

    def __init__(self, h, name="", base=0, trk=None):
        self.h, self.lw, self.rd, self.name, self.base = h, None, [], name, base
        self.trk = trk if trk is not None else self

    def __getitem__(self, idx):
        return self.h[idx]

    def c(self, lo, hi, p0=0, p1=128):
        return self.h[p0:p1, self.base + lo:self.base + hi]


class Em:
    def __init__(self, nc, stack):
        self.nc, self.stack = nc, stack
        self.eng = {"pe": nc.tensor, "act": nc.scalar, "dve": nc.vector, "pool": nc.gpsimd, "sp": nc.sync}
        self.sem, self.cnt = {}, {}
        for e in self.eng:
            self.sem[e] = stack.enter_context(nc.semaphore("s_" + e))
            self.cnt[e] = 0
        self.KD = 16
        self.dnext = {}
        for q in ("sp", "pool"):
            self.dnext[q] = 0
            for i in range(self.KD):
                key = "d_%s_%d" % (q, i)
                self.sem[key] = stack.enter_context(nc.semaphore("sd_%s_%d" % (q, i)))
                self.cnt[key] = 0
        self.seen = {e: {} for e in self.eng}
        self.n = 0
        self.uid = 0
        self.muted = False

    def sb(self, shape, dt, name="t", stack=None):
        self.uid += 1
        nm = "%s_%d" % (name, self.uid)
        h = (stack or self.stack).enter_context(self.nc.sbuf_tensor(nm, list(shape), dt))
        return T(h, nm)

    def psum_banks(self):
        banks = []
        for i in range(8):
            banks.append(self.stack.enter_context(self.nc.psum_tensor("psb%d" % i, [128, 512], F32)))
        return banks

    def dram(self, shape, dt, name="d"):
        self.uid += 1
        nm = "%s_%d" % (name, self.uid)
        return T(self.nc.dram_tensor(nm, list(shape), dt), nm)

    def _deps(self, e, reads, writes):
        need = {}
        seen = self.seen[e]

        def add(dep):
            s, v = dep
            if s == e and not SAME_ENGINE_SYNC:
                return
            if seen.get(s, 0) >= v:
                return
            if need.get(s, 0) < v:
                need[s] = v
        for t in reads:
            t = t.trk
            if t.lw is not None:
                add(t.lw)
            if t.name.startswith("bk"):
                for r in t.rd:
                    if r[0] != e:
                        add(r)
        for t in writes:
            t = t.trk
            if t.lw is not None:
                add(t.lw)
            for r in t.rd:
                add(r)
        for s, v in need.items():
            self.eng[e].wait_ge(self.sem[s], v)
            seen[s] = v
            self.n += 1

    def _rec(self, key, reads, writes):
        v = self.cnt[key]
        for t in reads:
            t.trk.rd.append((key, v))
        for t in writes:
            t.trk.lw = (key, v)
            t.trk.rd = []

    def op(self, e, fn, reads=(), writes=()):
        if self.muted:
            return None
        self._deps(e, reads, writes)
        ins = fn(self.eng[e])
        self.cnt[e] += 1
        ins.then_inc(self.sem[e], 1)
        self._rec(e, reads, writes)
        self.n += 1
        return ins

    def dma(self, q, out, in_, reads=(), writes=(), **kw):
        if self.muted:
            return None
        self._deps(q, reads, writes)
        key = "d_%s_%d" % (q, self.dnext[q] % self.KD)
        self.dnext[q] += 1
        if self.cnt[key] > 0 and self.seen[q].get(key, 0) < self.cnt[key]:
            self.eng[q].wait_ge(self.sem[key], self.cnt[key])
            self.seen[q][key] = self.cnt[key]
            self.n += 1
        ins = self.eng[q].dma_start(out=out, in_=in_, **kw)
        self.cnt[key] += 16
        ins.then_inc(self.sem[key], 16)
        self._rec(key, reads, writes)
        self.n += 1
        return ins

    def barrier(self):
        for e in self.eng:
            for s, v in self.cnt.items():
                if v > 0 and self.seen[e].get(s, 0) < v and s != e:
                    self.eng[e].wait_ge(self.sem[s], v)
                    self.seen[e][s] = v
                    self.n += 1


def make_consts(cfg):
    j = np.arange(128)
    same = (j[:, None] // SUB) == (j[None, :] // SUB)
    loc = j % SUB
    le = j[:, None] <= j[None, :]
    ge = j[:, None] >= j[None, :]
    mfm = np.zeros((2, 128, 264), np.float32)
    m3 = np.zeros((2, 128, 128), np.float32)
    mask = np.zeros((2, 128, 128), np.float32)
    bm = (j[:, None] // SUB == np.arange(NSB)[None, :]).astype(np.float32)
    mfm[0, :, 0:128] = same * (le.astype(np.float32) - (loc[:, None] <= SUB // 2 - 1))
    mfm[0, :, 128:256] = same * le
    mfm[0, :, 256:260] = bm
    m3[0] = same * (j[:, None] > j[None, :])
    mask[0] = same * le
    mfm[1, :, 0:128] = same * (ge.astype(np.float32) - (loc[:, None] >= SUB // 2))
    mfm[1, :, 128:256] = same * ge
    mfm[1, :, 256:260] = bm
    m3[1] = same * (j[:, None] < j[None, :])
    mask[1] = same * ge
    rot = np.zeros((128, 128), np.float32)
    for k in range(64):
        rot[k + 64, k] = -1.0
        rot[k, k + 64] = 1.0
    rows = cfg.LL // 64
    r_idx, c_idx = np.meshgrid(np.arange(rows), np.arange(64), indexing="ij")
    nfreq = 32
    freq = (10000.0 ** (-np.arange(nfreq, dtype=np.float32) / nfreq)).astype(np.float32)
    ang = np.concatenate([r_idx.reshape(-1, 1).astype(np.float32) * freq,
                          c_idx.reshape(-1, 1).astype(np.float32) * freq], axis=-1)
    cosT = np.concatenate([np.cos(ang).T, np.cos(ang).T], axis=0).astype(np.float32)
    sinT = np.concatenate([np.sin(ang).T, np.sin(ang).T], axis=0).astype(np.float32)
    misc = np.zeros((128, 8), np.float32)
    misc[:, 0] = LN_EPS
    misc[:, 1] = RMS_EPS
    misc[:, 2] = 1.0
    for a in range(4):
        misc[:, 4 + a] = j + 128 * a
    return {
        "c_ident": np.eye(128, dtype=np.float32),
        "c_iota": np.tile(np.arange(512, dtype=np.float32)[None, :], (128, 1)),
        "c_mfm": mfm, "c_m3": m3, "c_mask": mask, "c_bm": bm,
        "c_tri": (j[:, None] < j[None, :]).astype(np.float32),
        "c_ones": np.ones((128, 128), np.float32),
        "c_rot": rot, "c_cos": np.ascontiguousarray(cosT), "c_sin": np.ascontiguousarray(sinT),
        "c_misc": misc,
    }


INPUT_SHAPES = lambda cfg: {
    "x": [cfg.B, cfg.LL, D], "c": [cfg.B, D], "ctx": [cfg.B, cfg.LC, D], "c_ctx": [1, D],
    "ada_w": [4, D, 6 * D], "ada_b": [4, 6 * D], "ln_w": [4, 2, D], "ln_b": [4, 2, D],
    "even_w_in": [2, D, EVEN_IN], "even_w_out": [2, 1024, D], "ha_lb": [2, 2, 512], "ha_norm": [2, 128],
    "rb_decay": [2, 8], "odd_w_in": [2, D, ODD_IN], "odd_w_out": [2, 1024, D],
    "gc_w2": [2, 2, 16, 512], "gc_b2": [2, 2, 512], "gc_norm": [2, 256], "router_w": [4, D, E],
    "exp_w_gate": [4, E, D, cfg.F], "exp_w_up": [4, E, D, cfg.F], "exp_w_down": [4, E, cfg.F, D],
}


def build(cfg):
    nc = bass.Bass("TRN2", target_bir_lowering=False)
    I = {}
    for k, shp in INPUT_SHAPES(cfg).items():
        I[k] = nc.dram_tensor(k, shp, F32, kind="ExternalInput")
    consts = make_consts(cfg)
    for k, v in consts.items():
        I[k] = nc.dram_tensor(k, list(v.shape), F32, kind="ExternalInput")
    out = nc.dram_tensor("out", [cfg.B, cfg.LL, D], F32, kind="ExternalOutput")
    dbg = {}
    if cfg.debug:
        dbg["mod"] = nc.dram_tensor("dbg_mod", [cfg.DEPTH, cfg.B + 1, 6 * D], F32, kind="ExternalOutput")
        dbg["y"] = nc.dram_tensor("dbg_y", [cfg.NTOK, D], F32, kind="ExternalOutput")
        dbg["xmid"] = nc.dram_tensor("dbg_xmid", [cfg.NTOK, D], F32, kind="ExternalOutput")
        dbg["ffn"] = nc.dram_tensor("dbg_ffn", [cfg.NTOK, D], F32, kind="ExternalOutput")
        dbg["o"] = nc.dram_tensor("dbg_o", [8, 128, cfg.NTOK], F32, kind="ExternalOutput")
    B, NT, NTC, NTOK, LC, LL = cfg.B, cfg.NT, cfg.NTC, cfg.NTOK, cfg.LC, cfg.LL

    with ExitStack() as st:
        em = Em(nc, st)
        banks = em.psum_banks()
        BK = [T(banks[i], "bk%d" % i) for i in range(8)]
        PF = [T(banks[i], "pf%d" % i, trk=BK[i]) for i in range(4)]
        PQ = [T(banks[4 + i % 4], "pq%d" % i, base=(i // 4) * 128, trk=BK[4 + i % 4]) for i in range(16)]
        rr = {"f": 0, "q": 0}

        def pfull():
            rr["f"] = (rr["f"] + 1) % 4
            return PF[rr["f"]]

        def pq():
            rr["q"] = (rr["q"] + 1) % 16
            return PQ[rr["q"]]

        XS = em.dram([B, NTOK, D], F32, "XS")
        MOD = em.dram([cfg.DEPTH, B + 1, 6 * D], F32, "MOD")
        YT = em.dram([8, 128, NTOK], BF16, "YT")
        XB = em.dram([NT, 128, D], BF16, "XB")
        RK = em.dram([E, NTOK], F32, "RK")
        OUT = T(out, "out")
        DBG = {k: T(v, "dbg_" + k) for k, v in dbg.items()}

        def cload(name, shape, dt=F32, src=None, q="sp"):
            t = em.sb(shape, dt, name)
            srcap = src if src is not None else I[name].ap()
            em.dma("pool" if dt == BF16 else q, t.h.ap(), srcap, writes=[t])
            return t
        ident = cload("c_ident", [128, 128])
        identb = cload("c_ident", [128, 128], BF16)
        iota = cload("c_iota", [128, 512])
        mfm = [cload("c_mfm", [128, 264], src=I["c_mfm"][d]) for d in range(2)]
        m3 = [cload("c_m3", [128, 128], src=I["c_m3"][d]) for d in range(2)]
        amask = [cload("c_mask", [128, 128], src=I["c_mask"][d]) for d in range(2)]
        bmask = cload("c_bm", [128, 4])
        tri = cload("c_tri", [128, 128])
        ones = cload("c_ones", [128, 128])
        rotb = cload("c_rot", [128, 128], BF16)
        misc = cload("c_misc", [128, 8])
        EPS_LN, EPS_RMS, ONE = misc[:, 0:1], misc[:, 1:2], misc[:, 2:3]

        with ExitStack() as ph:
            cT = em.sb([128, NCH, B + 1], F32, "cT", ph)
            for r in range(B):
                em.dma("sp", cT[:, :, r], I["c"][r].rearrange("(c p) -> p c", p=128), writes=[cT],
                       allow_slow_non_contiguous=True)
            em.dma("sp", cT[:, :, B], I["c_ctx"][0].rearrange("(c p) -> p c", p=128), writes=[cT],
                   allow_slow_non_contiguous=True)
            em.op("act", lambda e: e.activation(out=cT[:, :, :], in_=cT[:, :, :], func=AF.Silu), reads=[cT], writes=[cT])
            wbuf = [em.sb([128, NCH, 512], F32, "adaw", ph) for _ in range(2)]
            bb = [em.sb([B + 1, 512], F32, "adab", ph) for _ in range(2)]
            mo = [em.sb([B + 1, 512], F32, "modo", ph) for _ in range(2)]
            it = 0
            for l in range(cfg.DEPTH):
                for cp in range(12):
                    w = wbuf[it % 2]; bt = bb[it % 2]; m = mo[it % 2]
                    em.dma("sp", w[:, :, :], I["ada_w"][l, :, cp * 512:(cp + 1) * 512].rearrange("(c p) n -> p c n", p=128), writes=[w])
                    em.dma("sp", bt[:, :], I["ada_b"][l:l + 1, cp * 512:(cp + 1) * 512].to_broadcast([B + 1, 512]), writes=[bt])
                    p = pfull()
                    for ch in range(NCH):
                        em.op("pe", lambda e: e.matmul(p.c(0, 512, 0, B + 1), lhsT=cT[:, ch, :], rhs=w[:, ch, :], start=(ch == 0), stop=(ch == NCH - 1)),
                              reads=[cT, w], writes=[p])
                    em.op("dve", lambda e: e.tensor_tensor(out=m[:, :], in0=p.c(0, 512, 0, B + 1), in1=bt[:, :], op=ALU.add), reads=[p, bt], writes=[m])
                    em.dma("sp", MOD[l, :, cp * 512:(cp + 1) * 512], m[:, :], reads=[m], writes=[MOD])
                    it += 1
            if cfg.debug:
                em.dma("sp", DBG["mod"].h.ap(), MOD.h.ap(), reads=[MOD], writes=[DBG["mod"]])
            for s in range(B):
                em.dma("sp", XS[s, 0:LC, :], I["ctx"][s], writes=[XS])
                em.dma("sp", XS[s, LC:NTOK, :], I["x"][s], writes=[XS])
        em.barrier()

        def layernorm_tile(ph_tmp, u, lnw_bc, lnb_bc, dst):
            st2, junk = ph_tmp["st2"], ph_tmp["junk"]
            em.op("dve", lambda e: e.memset(st2[:, :], 0.0), writes=[st2])
            em.op("act", lambda e: e.activation(out=junk[:, :], in_=u[:, :], func=AF.Identity, accum_out=st2[:, 0:1]), reads=[u], writes=[junk, st2])
            em.op("act", lambda e: e.activation(out=junk[:, :], in_=u[:, :], func=AF.Square, accum_out=st2[:, 1:2]), reads=[u], writes=[junk, st2])
            em.op("dve", lambda e: e.tensor_scalar(out=st2[:, 2:3], in0=st2[:, 0:1], scalar1=1.0 / D, scalar2=None, op0=ALU.mult), reads=[st2], writes=[st2])
            em.op("dve", lambda e: e.tensor_tensor(out=st2[:, 3:4], in0=st2[:, 2:3], in1=st2[:, 2:3], op=ALU.mult), reads=[st2], writes=[st2])
            em.op("dve", lambda e: e.scalar_tensor_tensor(out=st2[:, 4:5], in0=st2[:, 1:2], scalar=1.0 / D, in1=st2[:, 3:4], op0=ALU.mult, op1=ALU.subtract), reads=[st2], writes=[st2])
            em.op("act", lambda e: e.activation(out=st2[:, 5:6], in_=st2[:, 4:5], func=AF.Sqrt, bias=EPS_LN, scale=1.0), reads=[st2, misc], writes=[st2])
            em.op("dve", lambda e: e.reciprocal(out=st2[:, 6:7], in_=st2[:, 5:6]), reads=[st2], writes=[st2])
            em.op("dve", lambda e: e.tensor_scalar(out=u[:, :], in0=u[:, :], scalar1=st2[:, 2:3], scalar2=st2[:, 6:7], op0=ALU.subtract, op1=ALU.mult), reads=[u, st2], writes=[u])
            em.op("dve", lambda e: e.tensor_tensor(out=u[:, :], in0=u[:, :], in1=lnw_bc[:, :], op=ALU.mult), reads=[u, lnw_bc], writes=[u])
            em.op("dve", lambda e: e.tensor_tensor(out=dst[:, :], in0=u[:, :], in1=lnb_bc[:, :], op=ALU.add), reads=[u, lnb_bc], writes=[dst])

        def load_modfm(ph, l, s):
            mf = em.sb([128, 2, 6, NCH], F32, "modfm", ph)
            for i, row in enumerate((s, B)):
                for six in range(6):
                    em.dma("sp", mf[:, i, six, :], MOD[l, row, six * D:(six + 1) * D].rearrange("(c p) -> p c", p=128), reads=[MOD], writes=[mf],
                           allow_slow_non_contiguous=True)
            for six in (1, 4):
                em.op("dve", lambda e: e.tensor_scalar(out=mf[:, :, six, :], in0=mf[:, :, six, :], scalar1=1.0, scalar2=None, op0=ALU.add), reads=[mf], writes=[mf])
            return mf

        def bc_load(t, src_row_ap, srcT):
            em.dma("sp", t[:, :], src_row_ap.to_broadcast([128, D]), reads=[srcT], writes=[t])

        def mixer_phase(s, l):
            even = (l % 2 == 0)
            jl = l // 2
            w_in = I["even_w_in"] if even else I["odd_w_in"]
            w_out = I["even_w_out"] if even else I["odd_w_out"]
            def ck(k_):
                if cfg.sub == k_:
                    em.muted = True
            with ExitStack() as ph:
                mixer_body(ph, s, l, even, jl, w_in, w_out, ck)
            if em.muted:
                em.muted = False
                em.barrier()
                return
            em.barrier()
            mixer_out(s, l, jl, w_out)

        def mixer_body(ph, s, l, even, jl, w_in, w_out, ck):
            if True:
                mf = load_modfm(ph, l, s)
                ck(0)
                hT = em.sb([128, NCH, NTOK], BF16, "hT", ph)
                xt = [em.sb([128, D], F32, "xt", ph) for _ in range(2)]
                for n in range(NT):
                    x_t = xt[n % 2]
                    em.dma("sp", x_t[:, :], XS[s, n * 128:(n + 1) * 128, :], reads=[XS], writes=[x_t])
                    mi = 1 if n < NTC else 0
                    import os
                    dbgm = os.environ.get("KSUB2", "c")
                    for ch in range(NCH):
                        if dbgm == "a":
                            continue
                        p = pq()
                        em.op("pe", lambda e: e.matmul(p.c(0, 128), lhsT=x_t[:, ch * 128:(ch + 1) * 128], rhs=ident[:, :], start=True, stop=True), reads=[x_t, ident], writes=[p])
                        if dbgm == "b":
                            continue
                        if dbgm == "d":
                            em.op("act", lambda e: e.activation(out=hT[:, ch, n * 128:(n + 1) * 128], in_=p.c(0, 128), func=AF.Identity), reads=[p, mf], writes=[hT])
                            continue
                        em.op("act", lambda e: e.activation(out=hT[:, ch, n * 128:(n + 1) * 128], in_=p.c(0, 128), func=AF.Identity,
                                                            scale=mf[:, mi, 1, ch:ch + 1], bias=mf[:, mi, 0, ch:ch + 1]), reads=[p, mf], writes=[hT])
                ck(1)
                wts = [em.sb([128, NCH, 256], BF16, "wcol", ph) for _ in range(3)]
                wrr = [0]

                def load_wcols(c0, ncol):
                    wrr[0] = (wrr[0] + 1) % 3
                    w = wts[wrr[0]]
                    em.dma("pool", w[:, :, 0:ncol], w_in[jl, :, c0:c0 + ncol].rearrange("(c p) n -> p c n", p=128), writes=[w])
                    return w

                def proj_fm(c0, M, evac):
                    w = load_wcols(c0, M)
                    for t0 in range(0, NTOK, 512):
                        nt = min(512, NTOK - t0)
                        p = pfull()
                        for ch in range(NCH):
                            em.op("pe", lambda e: e.matmul(p.c(0, nt, 0, M), lhsT=w[:, ch, 0:M], rhs=hT[:, ch, t0:t0 + nt], start=(ch == 0), stop=(ch == NCH - 1)),
                                  reads=[w, hT], writes=[p])
                        evac(p, t0, nt)

                def proj_tm(c0, ncol, dst):
                    w = load_wcols(c0, ncol)
                    for n in range(NT):
                        p = pfull()
                        for ch in range(NCH):
                            em.op("pe", lambda e: e.matmul(p.c(0, ncol), lhsT=hT[:, ch, n * 128:(n + 1) * 128], rhs=w[:, ch, 0:ncol], start=(ch == 0), stop=(ch == NCH - 1)),
                                  reads=[w, hT], writes=[p])
                        em.op("act", lambda e: e.activation(out=dst[:, n, 0:ncol], in_=p.c(0, ncol), func=AF.Copy), reads=[p], writes=[dst])

                prm = em.sb([128, 40], F32, "prm", ph)
                if even:
                    cosT = em.sb([128, LL], F32, "cosT", ph)
                    sinT = em.sb([128, LL], F32, "sinT", ph)
                    em.dma("sp", cosT.h.ap(), I["c_cos"].ap(), writes=[cosT])
                    em.dma("sp", sinT.h.ap(), I["c_sin"].ap(), writes=[sinT])
                    lbc, oml = prm[:, 0:8], prm[:, 8:16]
                    if jl == 0:
                        em.op("dve", lambda e: e.memset(prm[:, 0:8], 0.0), writes=[prm])
                    else:
                        em.dma("sp", prm[:, 0:8], I["ha_lb"][1].rearrange("d (h k) -> k (d h)", k=128), writes=[prm], allow_slow_non_contiguous=True)
                        em.dma("sp", prm[:, 8:16], I["ha_lb"][0].rearrange("d (h k) -> k (d h)", k=128), writes=[prm], allow_slow_non_contiguous=True)
                        em.op("dve", lambda e: e.tensor_tensor(out=prm[:, 0:8], in0=prm[:, 0:8], in1=prm[:, 8:16], op=ALU.subtract), reads=[prm], writes=[prm])
                        em.op("act", lambda e: e.activation(out=prm[:, 0:8], in_=prm[:, 0:8], func=AF.Sigmoid), reads=[prm], writes=[prm])
                    em.op("dve", lambda e: e.tensor_scalar(out=prm[:, 8:16], in0=prm[:, 0:8], scalar1=-1.0, scalar2=1.0, op0=ALU.mult, op1=ALU.add), reads=[prm], writes=[prm])
                    em.op("dve", lambda e: e.tensor_scalar(out=prm[:, 0:8], in0=prm[:, 0:8], scalar1=1e-30, scalar2=None, op0=ALU.max), reads=[prm], writes=[prm])
                    em.dma("sp", prm[:, 16:24], I["rb_decay"][jl:jl + 1, :].to_broadcast([128, 8]), writes=[prm])
                    em.op("act", lambda e: e.activation(out=prm[:, 16:24], in_=prm[:, 16:24], func=AF.Exp, scale=-1.0), reads=[prm], writes=[prm])
                    em.op("act", lambda e: e.activation(out=prm[:, 16:24], in_=prm[:, 16:24], func=AF.Ln, bias=ONE, scale=1.0), reads=[prm, misc], writes=[prm])
                    em.op("dve", lambda e: e.tensor_scalar(out=prm[:, 16:24], in0=prm[:, 16:24], scalar1=-1.0, scalar2=None, op0=ALU.mult), reads=[prm], writes=[prm])
                    em.dma("sp", prm[:, 24:25], I["ha_norm"][jl].rearrange("(k o) -> k o", o=1), writes=[prm], allow_slow_non_contiguous=True)
                    em.op("dve", lambda e: e.memset(prm[:, 25:26], 1.0), writes=[prm])
                else:
                    em.dma("sp", prm[:, 0:8], I["gc_b2"][jl].rearrange("d (h k) -> k (d h)", k=128), writes=[prm], allow_slow_non_contiguous=True)
                    em.op("dve", lambda e: e.tensor_scalar(out=prm[:, 0:8], in0=prm[:, 0:8], scalar1=-1.0, scalar2=None, op0=ALU.mult), reads=[prm], writes=[prm])
                    em.dma("sp", prm[:, 24:26], I["gc_norm"][jl].rearrange("(c k) -> k c", k=128), writes=[prm], allow_slow_non_contiguous=True)
                    w2 = em.sb([16, 2, 512], F32, "w2", ph)
                    em.dma("sp", w2[:, :, :], I["gc_w2"][jl].rearrange("d r k -> r d k"), writes=[w2])

                ck(2)
                nvc = 1 if even else 2
                dv = 128 * nvc
                qT = em.sb([128, NTOK], BF16, "qT", ph)
                kT = em.sb([128, NTOK], BF16, "kT", ph)
                gT = em.sb([128, NTOK], F32, "gT", ph)
                vtm = em.sb([128, NT, dv], BF16, "vtm", ph)
                sgT = em.sb([128, nvc, NTOK], BF16, "sgT", ph)
                obuf = em.sb([128, nvc, NTOK], F32, "obuf", ph)
                S = em.sb([128, dv], F32, "S", ph)
                Sb = em.sb([128, dv], BF16, "Sb", ph)
                tmpE = [em.sb([128, 512], F32, "tmpE", ph) for _ in range(3)]
                lrT = None if even else em.sb([16, NTOK], F32, "lrT", ph)
                def dbl(shape, dt, name):
                    return [em.sb(shape, dt, name, ph) for _ in range(2)]
                gtm = dbl([128, 128], F32, "gtm")
                X1 = dbl([128, 128], F32, "X1"); X1i = dbl([128, 128], F32, "X1i"); X2 = dbl([128, 128], F32, "X2")
                X3 = dbl([128, 128], F32, "X3"); X4 = dbl([128, 4], F32, "X4"); Ecl = dbl([128, 128], F32, "Ecl")
                Qt = dbl([128, 128], BF16, "Qt"); Kt = dbl([128, 128], BF16, "Kt"); Qs = dbl([128, 128], BF16, "Qs")
                Kom = dbl([128, NSB, 128], BF16, "Kom"); At = dbl([128, 128], BF16, "At")
                ofin = dbl([128, nvc, 128], F32, "ofin"); sq = dbl([128, nvc, 128], F32, "sq")
                rstd = dbl([128, 128], F32, "rstd"); ytile = dbl([128, nvc, 128], BF16, "ytile")
                XR = {}

                def exp_tiles(gsrc_tm, d, bi):
                    pf = pfull()
                    em.op("pe", lambda e: e.matmul(pf.c(0, 264), lhsT=gsrc_tm[:, :], rhs=mfm[d][:, :], start=True, stop=True), reads=[gsrc_tm, mfm[d]], writes=[pf])
                    p3 = pq()
                    em.op("pe", lambda e: e.matmul(p3.c(0, 128), lhsT=m3[d][:, :], rhs=gsrc_tm[:, :], start=True, stop=True), reads=[gsrc_tm, m3[d]], writes=[p3])
                    em.op("dve", lambda e: e.tensor_scalar(out=Ecl[bi][:, :], in0=pf.c(0, 128), scalar1=-CLAMP, scalar2=CLAMP, op0=ALU.max, op1=ALU.min), reads=[pf], writes=[Ecl[bi]])
                    em.op("act", lambda e: e.activation(out=X1[bi][:, :], in_=Ecl[bi][:, :], func=AF.Exp), reads=[Ecl[bi]], writes=[X1[bi]])
                    em.op("act", lambda e: e.activation(out=X1i[bi][:, :], in_=Ecl[bi][:, :], func=AF.Exp, scale=-1.0), reads=[Ecl[bi]], writes=[X1i[bi]])
                    em.op("act", lambda e: e.activation(out=X2[bi][:, :], in_=pf.c(128, 256), func=AF.Exp), reads=[pf], writes=[X2[bi]])
                    em.op("act", lambda e: e.activation(out=X4[bi][:, :], in_=pf.c(256, 260), func=AF.Exp), reads=[pf], writes=[X4[bi]])
                    em.op("act", lambda e: e.activation(out=X3[bi][:, :], in_=p3.c(0, 128), func=AF.Exp), reads=[p3], writes=[X3[bi]])

                def scan_dir(d, const_x, post):
                    em.op("dve", lambda e: e.memset(S[:, :], 0.0), writes=[S])
                    em.op("pool", lambda e: e.memset(Sb[:, :], 0.0), writes=[Sb])
                    if d == 0:
                        order = list(range(NT))
                    else:
                        order = list(range(NTC - 1, -1, -1)) + list(range(NT - 1, NTC - 1, -1))
                    x4s = {}

                    def prep(it_, n):
                        bi = it_ % 2
                        cs = slice(n * 128, (n + 1) * 128)
                        if const_x is None:
                            pg = pq()
                            em.op("pe", lambda e: e.matmul(pg.c(0, 128), lhsT=gT[:, cs], rhs=ident[:, :], start=True, stop=True), reads=[gT, ident], writes=[pg])
                            em.op("act", lambda e: e.activation(out=gtm[bi][:, :], in_=pg.c(0, 128), func=AF.Copy), reads=[pg], writes=[gtm[bi]])
                            exp_tiles(gtm[bi], d, bi)
                            x1, x1i, x2, x3, x4 = X1[bi], X1i[bi], X2[bi], X3[bi], X4[bi]
                        else:
                            x1, x1i, x2, x3, x4 = const_x
                        em.op("dve", lambda e: e.tensor_tensor(out=Qt[bi][:, :], in0=qT[:, cs], in1=x1[:, :], op=ALU.mult), reads=[qT, x1], writes=[Qt[bi]])
                        em.op("dve", lambda e: e.tensor_tensor(out=Kt[bi][:, :], in0=kT[:, cs], in1=x1i[:, :], op=ALU.mult), reads=[kT, x1i], writes=[Kt[bi]])
                        em.op("dve", lambda e: e.tensor_tensor(out=Qs[bi][:, :], in0=qT[:, cs], in1=x2[:, :], op=ALU.mult), reads=[qT, x2], writes=[Qs[bi]])
                        pk = pq()
                        em.op("pe", lambda e: e.matmul(pk.c(0, 128), lhsT=kT[:, cs], rhs=identb[:, :], start=True, stop=True), reads=[kT, identb], writes=[pk])
                        for a in range(NSB):
                            em.op("dve", lambda e: e.scalar_tensor_tensor(out=Kom[bi][:, a, :], in0=pk.c(0, 128), scalar=bmask[:, a:a + 1], in1=x3[:, :], op0=ALU.mult, op1=ALU.mult),
                                  reads=[pk, bmask, x3], writes=[Kom[bi]])
                        pa = pq()
                        em.op("pe", lambda e: e.matmul(pa.c(0, 128), lhsT=Kt[bi][:, :], rhs=Qt[bi][:, :], start=True, stop=True), reads=[Kt[bi], Qt[bi]], writes=[pa])
                        em.op("dve", lambda e: e.tensor_tensor(out=At[bi][:, :], in0=pa.c(0, 128), in1=amask[d][:, :], op=ALU.mult), reads=[pa, amask[d]], writes=[At[bi]])
                        x4s[it_] = x4

                    def chain(it_, n):
                        bi = it_ % 2
                        cs = slice(n * 128, (n + 1) * 128)
                        x4 = x4s.pop(it_)
                        po = [pq() for _ in range(nvc)]
                        blocks = range(NSB) if d == 0 else range(NSB - 1, -1, -1)
                        for a in blocks:
                            c0, c1 = a * SUB, (a + 1) * SUB
                            for c in range(nvc):
                                em.op("pe", lambda e: e.matmul(po[c].c(c0, c1), lhsT=vtm[:, n, c * 128:(c + 1) * 128], rhs=At[bi][:, c0:c1], start=True, stop=False),
                                      reads=[vtm, At[bi]], writes=[po[c]])
                                em.op("pe", lambda e: e.matmul(po[c].c(c0, c1), lhsT=Sb[:, c * 128:(c + 1) * 128], rhs=Qs[bi][:, c0:c1], start=False, stop=True),
                                      reads=[Sb, Qs[bi]], writes=[po[c]])
                            pst = pfull()
                            em.op("pe", lambda e: e.matmul(pst.c(0, dv), lhsT=Kom[bi][:, a, :], rhs=vtm[:, n, :], start=True, stop=True), reads=[Kom[bi], vtm], writes=[pst])
                            em.op("dve", lambda e: e.scalar_tensor_tensor(out=S[:, :], in0=S[:, :], scalar=x4[:, a:a + 1], in1=pst.c(0, dv), op0=ALU.mult, op1=ALU.add),
                                  reads=[S, x4, pst], writes=[S])
                            em.op("act", lambda e: e.activation(out=Sb[:, :], in_=S[:, :], func=AF.Copy), reads=[S], writes=[Sb])
                        if d == 1:
                            for c in range(nvc):
                                em.op("act", lambda e: e.activation(out=obuf[:, c, cs], in_=po[c].c(0, 128), func=AF.Copy), reads=[po[c]], writes=[obuf])
                        else:
                            for c in range(nvc):
                                em.op("dve", lambda e: e.tensor_tensor(out=ofin[bi][:, c, :], in0=po[c].c(0, 128), in1=obuf[:, c, cs], op=ALU.add), reads=[po[c], obuf], writes=[ofin[bi]])
                            post(n, bi)

                    prep(0, order[0])
                    for it_, n in enumerate(order):
                        if it_ + 1 < len(order):
                            prep(it_ + 1, order[it_ + 1])
                        chain(it_, n)

                def run_group(gi, normcol, ech0, const_x_by_dir=None, prep_dir=None):
                    def post(n, bi):
                        cs = slice(n * 128, (n + 1) * 128)
                        if cfg.debug and l == 0 and s == 0:
                            for c in range(nvc):
                                em.dma("sp", DBG["o"][ech0 + c, :, cs], ofin[bi][:, c, :], reads=[ofin[bi]], writes=[DBG["o"]])
                        em.op("act", lambda e: e.activation(out=sq[bi][:, :, :], in_=ofin[bi][:, :, :], func=AF.Square), reads=[ofin[bi]], writes=[sq[bi]])
                        pss = pq()
                        for c in range(nvc):
                            em.op("pe", lambda e: e.matmul(pss.c(0, 128), lhsT=ones[:, :], rhs=sq[bi][:, c, :], start=(c == 0), stop=(c == nvc - 1)), reads=[ones, sq[bi]], writes=[pss])
                        em.op("act", lambda e: e.activation(out=rstd[bi][:, :], in_=pss.c(0, 128), func=AF.Ln, bias=EPS_RMS, scale=1.0 / dv), reads=[pss, misc], writes=[rstd[bi]])
                        em.op("act", lambda e: e.activation(out=rstd[bi][:, :], in_=rstd[bi][:, :], func=AF.Exp, scale=-0.5), reads=[rstd[bi]], writes=[rstd[bi]])
                        for c in range(nvc):
                            em.op("dve", lambda e: e.tensor_tensor(out=ofin[bi][:, c, :], in0=ofin[bi][:, c, :], in1=rstd[bi][:, :], op=ALU.mult), reads=[ofin[bi], rstd[bi]], writes=[ofin[bi]])
                            em.op("dve", lambda e: e.scalar_tensor_tensor(out=ytile[bi][:, c, :], in0=ofin[bi][:, c, :], scalar=normcol[:, c:c + 1], in1=sgT[:, c, cs], op0=ALU.mult, op1=ALU.mult),
                                  reads=[ofin[bi], prm, sgT], writes=[ytile[bi]])
                            em.dma("sp", YT[ech0 + c, :, cs], ytile[bi][:, c, :], reads=[ytile[bi]], writes=[YT])
                    for d in (1, 0):
                        if prep_dir is not None:
                            prep_dir(d)
                        ck(4)
                        scan_dir(d, None if const_x_by_dir is None else const_x_by_dir[d], post)

                def evac_copy(dst, scale=1.0):
                    def f(p, t0, nt):
                        em.op("act", lambda e: e.activation(out=dst[:, t0:t0 + nt], in_=p.c(0, nt), func=AF.Identity, scale=scale), reads=[p], writes=[dst])
                    return f

                def evac_silu(c):
                    def f(p, t0, nt):
                        em.op("act", lambda e: e.activation(out=sgT[:, c, t0:t0 + nt], in_=p.c(0, nt), func=AF.Silu), reads=[p], writes=[sgT])
                    return f

                if even:
                    for h in range(4):
                        proj_fm(0 + h * 128, 128, evac_copy(qT, 128 ** -0.5))
                        proj_tm(1536 + h * 128, 128, vtm)
                        proj_fm(2048 + h * 128, 128, evac_silu(0))
                        ck(3)

                        def prep_dir(d, h=h):
                            idx = d * 4 + h
                            def ev(p, t0, nt):
                                Et, L1, L2 = tmpE
                                em.op("act", lambda e: e.activation(out=Et[:, 0:nt], in_=p.c(0, nt), func=AF.Exp), reads=[p], writes=[Et])
                                em.op("act", lambda e: e.activation(out=L1[:, 0:nt], in_=Et[:, 0:nt], func=AF.Ln, bias=prm[:, idx:idx + 1], scale=1.0), reads=[Et, prm], writes=[L1])
                                em.op("act", lambda e: e.activation(out=L2[:, 0:nt], in_=Et[:, 0:nt], func=AF.Ln, bias=ONE, scale=1.0), reads=[Et, misc], writes=[L2])
                                em.op("dve", lambda e: e.tensor_tensor(out=gT[:, t0:t0 + nt], in0=L1[:, 0:nt], in1=L2[:, 0:nt], op=ALU.subtract), reads=[L1, L2], writes=[gT])
                                em.op("dve", lambda e: e.tensor_scalar(out=Et[:, 0:nt], in0=Et[:, 0:nt], scalar1=1.0, scalar2=None, op0=ALU.add), reads=[Et], writes=[Et])
                                em.op("dve", lambda e: e.reciprocal(out=Et[:, 0:nt], in_=Et[:, 0:nt]), reads=[Et], writes=[Et])
                                em.op("dve", lambda e: e.tensor_scalar(out=kT[:, t0:t0 + nt], in0=Et[:, 0:nt], scalar1=prm[:, 8 + idx:9 + idx], scalar2=None, op0=ALU.mult), reads=[Et, prm], writes=[kT])
                            proj_fm((512 if d == 0 else 1024) + h * 128, 128, ev)
                        run_group(h, prm[:, 24:25], h, None, prep_dir)
                        ck(5)
                    ck(6)
                    q0 = tmpE
                    for h in range(4):
                        def rope_evac(dst, scale):
                            def f(p, t0, nt):
                                em.op("act", lambda e: e.activation(out=dst[:, t0:t0 + nt], in_=p.c(0, nt), func=AF.Identity, scale=scale), reads=[p], writes=[dst])
                                a0 = max(t0, LC); a1 = t0 + nt
                                if a1 <= a0:
                                    return
                                w_ = a1 - a0
                                pr = pfull()
                                em.op("pe", lambda e: e.matmul(pr.c(0, w_), lhsT=rotb[:, :], rhs=dst[:, a0:a1], start=True, stop=True), reads=[rotb, dst], writes=[pr])
                                t1, t2 = tmpE[0], tmpE[1]
                                em.op("dve", lambda e: e.tensor_tensor(out=t1[:, 0:w_], in0=dst[:, a0:a1], in1=cosT[:, a0 - LC:a1 - LC], op=ALU.mult), reads=[dst, cosT], writes=[t1])
                                em.op("dve", lambda e: e.tensor_tensor(out=t2[:, 0:w_], in0=pr.c(0, w_), in1=sinT[:, a0 - LC:a1 - LC], op=ALU.mult), reads=[pr, sinT], writes=[t2])
                                em.op("dve", lambda e: e.tensor_tensor(out=dst[:, a0:a1], in0=t1[:, 0:w_], in1=t2[:, 0:w_], op=ALU.add), reads=[t1, t2], writes=[dst])
                            return f
                        proj_fm(2560 + h * 128, 128, rope_evac(qT, 128 ** -0.5))
                        proj_fm(3072 + h * 128, 128, rope_evac(kT, 1.0))
                        proj_tm(3584 + h * 128, 128, vtm)
                        proj_fm(4096 + h * 128, 128, evac_silu(0))
                        cx = {}
                        for d in (0, 1):
                            idx = 16 + d * 4 + h
                            em.op("dve", lambda e: e.tensor_scalar(out=gtm[d][:, :], in0=ones[:, :], scalar1=prm[:, idx:idx + 1], scalar2=None, op0=ALU.mult), reads=[ones, prm], writes=[gtm[d]])
                            if (h, d) not in XR:
                                XR[(h, d)] = None
                            exp_tiles(gtm[d], d, d)
                            tl = [em.sb([128, 128], F32, "xr", ph) for _ in range(4)] + [em.sb([128, 4], F32, "xr4", ph)] if XR.get("bufs%d" % d) is None else XR["bufs%d" % d]
                            XR["bufs%d" % d] = tl
                            for src, dst_ in zip((X1[d], X1i[d], X2[d], X3[d], X4[d]), tl):
                                em.op("pool", lambda e: e.tensor_copy(out=dst_.h.ap(), in_=src.h.ap()), reads=[src], writes=[dst_])
                            cx[d] = tl
                        run_group(4 + h, prm[:, 25:26], 4 + h, cx, None)
                        ck(7)
                    ck(98)
                else:
                    for h in range(4):
                        proj_fm(0 + h * 128, 128, evac_copy(qT, 128 ** -0.5))
                        proj_fm(512 + h * 128, 128, evac_copy(kT, 1.0))
                        proj_tm(1024 + h * 256, 256, vtm)
                        for c in range(2):
                            proj_fm(2048 + h * 256 + c * 128, 128, evac_silu(c))

                        def prep_dir(d, h=h):
                            idx = d * 4 + h
                            def evl(p, t0, nt):
                                em.op("act", lambda e: e.activation(out=lrT[:, t0:t0 + nt], in_=p.c(0, nt, 0, 16), func=AF.Copy), reads=[p], writes=[lrT])
                            proj_fm(3072 + d * 16, 16, evl)
                            for t0 in range(0, NTOK, 512):
                                nt = min(512, NTOK - t0)
                                p = pfull()
                                em.op("pe", lambda e: e.matmul(p.c(0, nt), lhsT=w2[:, d, h * 128:(h + 1) * 128], rhs=lrT[:, t0:t0 + nt], start=True, stop=True), reads=[w2, lrT], writes=[p])
                                Et, L1 = tmpE[0], tmpE[1]
                                em.op("act", lambda e: e.activation(out=Et[:, 0:nt], in_=p.c(0, nt), func=AF.Exp, scale=-1.0, bias=prm[:, idx:idx + 1]), reads=[p, prm], writes=[Et])
                                em.op("act", lambda e: e.activation(out=L1[:, 0:nt], in_=Et[:, 0:nt], func=AF.Ln, bias=ONE, scale=1.0), reads=[Et, misc], writes=[L1])
                                em.op("dve", lambda e: e.tensor_scalar(out=gT[:, t0:t0 + nt], in0=L1[:, 0:nt], scalar1=-1.0 / GC_TAU, scalar2=None, op0=ALU.mult), reads=[L1], writes=[gT])
                        run_group(h, prm[:, 24:26], 2 * h, None, prep_dir)

        def mixer_out(s, l, jl, w_out):
            def ck(k_):
                if cfg.sub == k_:
                    em.muted = True
            with ExitStack() as ph:
                wo = em.sb([128, NCH, D], BF16, "wo", ph)
                em.dma("pool", wo[:, :, :], w_out[jl].rearrange("(c p) n -> p c n", p=128), writes=[wo])
                g1bc = [em.sb([128, D], F32, "g1bc", ph) for _ in range(2)]
                lnw = em.sb([128, D], F32, "lnw", ph); lnb = em.sb([128, D], F32, "lnb", ph)
                bc_load(g1bc[0], MOD[l, s:s + 1, 2 * D:3 * D], MOD)
                bc_load(g1bc[1], MOD[l, B:B + 1, 2 * D:3 * D], MOD)
                em.dma("sp", lnw[:, :], I["ln_w"][l, 0:1, :].to_broadcast([128, D]), writes=[lnw])
                em.dma("sp", lnb[:, :], I["ln_b"][l, 0:1, :].to_broadcast([128, D]), writes=[lnb])
                tmp = {"st2": em.sb([128, 8], F32, "st2", ph), "junk": em.sb([128, D], F32, "junk", ph)}
                yts = [em.sb([128, NCH, 128], BF16, "yts", ph) for _ in range(2)]
                xts = [em.sb([128, D], F32, "xts", ph) for _ in range(2)]
                us = [em.sb([128, D], F32, "us", ph) for _ in range(2)]
                ck(101)
                for n in range(NT):
                    bi = n % 2
                    cs = slice(n * 128, (n + 1) * 128)
                    em.dma("sp", yts[bi][:, :, :], YT[:, :, cs].rearrange("c p t -> p c t"), reads=[YT], writes=[yts[bi]])
                    em.dma("sp", xts[bi][:, :], XS[s, cs, :], reads=[XS], writes=[xts[bi]])
                    ck(102)
                    gb = g1bc[1 if n < NTC else 0]
                    for dh in range(2):
                        p = pfull()
                        for ch in range(NCH):
                            em.op("pe", lambda e: e.matmul(p.c(0, 512), lhsT=yts[bi][:, ch, :], rhs=wo[:, ch, dh * 512:(dh + 1) * 512], start=(ch == 0), stop=(ch == NCH - 1)),
                                  reads=[yts[bi], wo], writes=[p])
                        if cfg.debug and l == 0 and s == 0:
                            em.op("act", lambda e: e.activation(out=tmp["junk"][:, dh * 512:(dh + 1) * 512], in_=p.c(0, 512), func=AF.Copy), reads=[p], writes=[tmp["junk"]])
                        em.op("dve", lambda e: e.tensor_tensor(out=us[bi][:, dh * 512:(dh + 1) * 512], in0=p.c(0, 512), in1=gb[:, dh * 512:(dh + 1) * 512], op=ALU.mult), reads=[p, gb], writes=[us[bi]])
                    if cfg.debug and l == 0 and s == 0:
                        em.dma("sp", DBG["y"][cs, :], tmp["junk"][:, :], reads=[tmp["junk"]], writes=[DBG["y"]])
                    ck(103)
                    em.op("dve", lambda e: e.scalar_tensor_tensor(out=us[bi][:, :], in0=xts[bi][:, :], scalar=DN_ALPHA, in1=us[bi][:, :], op0=ALU.mult, op1=ALU.add), reads=[xts[bi], us[bi]], writes=[us[bi]])
                    ck(104)
                    layernorm_tile(tmp, us[bi], lnw, lnb, xts[bi])
                    ck(105)
                    em.dma("sp", XS[s, cs, :], xts[bi][:, :], reads=[xts[bi]], writes=[XS])
                    if cfg.debug and l == 0 and s == 0:
                        em.dma("sp", DBG["xmid"][cs, :], xts[bi][:, :], reads=[xts[bi]], writes=[DBG["xmid"]])
            em.muted = False
            em.barrier()

        def ffn_phase(s, l):
            last = (l == cfg.DEPTH - 1)
            NS, capL, capC, NF, Fd = cfg.NS, cfg.capL, cfg.capC, cfg.NF, cfg.F
            ctiles = [(c0, min(128, NS - c0)) for c0 in range(0, NS, 128)]
            with ExitStack() as ph0:
                yacc = em.sb([128, NT, D], F32, "yacc", ph0)
                aff = em.sb([128, NT, E], F32, "aff", ph0)
                rkg = em.sb([128, NT, E], F32, "rkg", ph0)
                ph = ExitStack()
                bcs = [em.sb([128, D], F32, "bc", ph) for _ in range(2)]
                msk = em.sb([128, NT, E], F32, "msk", ph)
                affT = em.sb([E, NTOK], F32, "affT", ph)
                wrk = em.sb([E, NTOK], F32, "wrk", ph)
                mx8 = em.sb([E, 8], F32, "mx8", ph)
                wr = em.sb([128, NCH, E], F32, "wr", ph)
                xts = [em.sb([128, D], F32, "xts", ph) for _ in range(2)]
                x2f = [em.sb([128, D], F32, "x2f", ph) for _ in range(2)]
                x2b = [em.sb([128, D], BF16, "x2b", ph) for _ in range(2)]
                x2T = [em.sb([128, NCH, 128], F32, "x2T", ph) for _ in range(2)]
                sm = [em.sb([128, 4], F32, "sm", ph) for _ in range(2)]
                cum = em.sb([128, E], F32, "cum", ph)
                em.dma("sp", wr[:, :, :], I["router_w"][l].rearrange("(c p) n -> p c n", p=128), writes=[wr])
                em.op("pool", lambda e: e.memset(yacc[:, :, :], 0.0), writes=[yacc])
                for n in range(NT):
                    bi = n % 2
                    cs = slice(n * 128, (n + 1) * 128)
                    if n == 0 or n == NTC:
                        row = B if n < NTC else s
                        bc_load(bcs[0], MOD[l, row:row + 1, 4 * D:5 * D], MOD)
                        em.op("dve", lambda e: e.tensor_scalar(out=bcs[0][:, :], in0=bcs[0][:, :], scalar1=1.0, scalar2=None, op0=ALU.add), reads=[bcs[0]], writes=[bcs[0]])
                        bc_load(bcs[1], MOD[l, row:row + 1, 3 * D:4 * D], MOD)
                    em.dma("sp", xts[bi][:, :], XS[s, cs, :], reads=[XS], writes=[xts[bi]])
                    em.op("dve", lambda e: e.tensor_tensor(out=x2f[bi][:, :], in0=xts[bi][:, :], in1=bcs[0][:, :], op=ALU.mult), reads=[xts[bi], bcs[0]], writes=[x2f[bi]])
                    em.op("dve", lambda e: e.tensor_tensor(out=x2f[bi][:, :], in0=x2f[bi][:, :], in1=bcs[1][:, :], op=ALU.add), reads=[x2f[bi], bcs[1]], writes=[x2f[bi]])
                    em.op("act", lambda e: e.activation(out=x2b[bi][:, :], in_=x2f[bi][:, :], func=AF.Copy), reads=[x2f[bi]], writes=[x2b[bi]])
                    em.dma("sp", XB[n, :, :], x2b[bi][:, :], reads=[x2b[bi]], writes=[XB])
                    for ch in range(NCH):
                        p = pq()
                        em.op("pe", lambda e: e.matmul(p.c(0, 128), lhsT=x2f[bi][:, ch * 128:(ch + 1) * 128], rhs=ident[:, :], start=True, stop=True), reads=[x2f[bi], ident], writes=[p])
                        em.op("act", lambda e: e.activation(out=x2T[bi][:, ch, :], in_=p.c(0, 128), func=AF.Copy), reads=[p], writes=[x2T[bi]])
                    pl = pq()
                    for ch in range(NCH):
                        em.op("pe", lambda e: e.matmul(pl.c(0, E), lhsT=x2T[bi][:, ch, :], rhs=wr[:, ch, :], start=(ch == 0), stop=(ch == NCH - 1)), reads=[x2T[bi], wr], writes=[pl])
                    smt = sm[bi]
                    em.op("dve", lambda e: e.tensor_reduce(out=smt[:, 0:1], in_=pl.c(0, E), axis=mybir.AxisListType.X, op=ALU.max), reads=[pl], writes=[smt])
                    em.op("dve", lambda e: e.tensor_scalar(out=smt[:, 1:2], in0=smt[:, 0:1], scalar1=-1.0, scalar2=None, op0=ALU.mult), reads=[smt], writes=[smt])
                    em.op("dve", lambda e: e.memset(smt[:, 2:3], 0.0), writes=[smt])
                    em.op("act", lambda e: e.activation(out=aff[:, n, :], in_=pl.c(0, E), func=AF.Exp, bias=smt[:, 1:2], scale=1.0, accum_out=smt[:, 2:3]), reads=[pl, smt], writes=[aff, smt])
                    em.op("dve", lambda e: e.reciprocal(out=smt[:, 3:4], in_=smt[:, 2:3]), reads=[smt], writes=[smt])
                    em.op("dve", lambda e: e.tensor_scalar(out=aff[:, n, :], in0=aff[:, n, :], scalar1=smt[:, 3:4], scalar2=None, op0=ALU.mult), reads=[aff, smt], writes=[aff])
                    pt_ = pq()
                    em.op("pe", lambda e: e.matmul(pt_.c(0, 128, 0, E), lhsT=aff[:, n, :], rhs=ident[:, :], start=True, stop=True), reads=[aff, ident], writes=[pt_])
                    em.op("act", lambda e: e.activation(out=affT[:, cs], in_=pt_.c(0, 128, 0, E), func=AF.Copy), reads=[pt_], writes=[affT])
                for (t0, n_, cap, off, nt0, ntn) in ((0, LC, capC, capL, 0, NTC), (LC, LL, capL, 0, NTC, NT)):
                    em.op("dve", lambda e: e.tensor_copy(out=wrk[:, t0:t0 + n_], in_=affT[:, t0:t0 + n_]), reads=[affT], writes=[wrk])
                    for r in range(cap // 8):
                        em.op("dve", lambda e: e.max(out=mx8[:, :], in_=wrk[:, t0:t0 + n_]), reads=[wrk], writes=[mx8])
                        if r < cap // 8 - 1:
                            em.op("dve", lambda e: e.match_replace(out=wrk[:, t0:t0 + n_], in_to_replace=mx8[:, :], in_values=wrk[:, t0:t0 + n_], imm_value=-1e30), reads=[wrk, mx8], writes=[wrk])
                    em.op("dve", lambda e: e.tensor_scalar(out=wrk[:, t0:t0 + n_], in0=affT[:, t0:t0 + n_], scalar1=mx8[:, 7:8], scalar2=None, op0=ALU.is_ge), reads=[affT, mx8], writes=[wrk])
                    em.op("dve", lambda e: e.memset(cum[:, :], 0.0), writes=[cum])
                    for n in range(nt0, ntn):
                        cs = slice(n * 128, (n + 1) * 128)
                        pm = pq()
                        em.op("pe", lambda e: e.matmul(pm.c(0, E), lhsT=wrk[:, cs], rhs=ident[0:E, 0:E], start=True, stop=True), reads=[wrk, ident], writes=[pm])
                        em.op("act", lambda e: e.activation(out=msk[:, n, :], in_=pm.c(0, E), func=AF.Copy), reads=[pm], writes=[msk])
                        pr = pq()
                        em.op("pe", lambda e: e.matmul(pr.c(0, E), lhsT=tri[:, :], rhs=msk[:, n, :], start=True, stop=False), reads=[tri, msk], writes=[pr])
                        em.op("pe", lambda e: e.matmul(pr.c(0, E), lhsT=ones[:, :], rhs=cum[:, :], start=False, stop=True), reads=[ones, cum], writes=[pr])
                        em.op("dve", lambda e: e.scalar_tensor_tensor(out=rkg[:, n, :], in0=pr.c(0, E), scalar=float(off + 1), in1=msk[:, n, :], op0=ALU.add, op1=ALU.mult), reads=[pr, msk], writes=[rkg])
                        em.op("dve", lambda e: e.tensor_scalar(out=rkg[:, n, :], in0=rkg[:, n, :], scalar1=-1.0, scalar2=None, op0=ALU.add), reads=[rkg], writes=[rkg])
                        em.op("dve", lambda e: e.tensor_tensor(out=cum[:, :], in0=cum[:, :], in1=msk[:, n, :], op=ALU.add), reads=[cum, msk], writes=[cum])
                        pt_ = pq()
                        em.op("pe", lambda e: e.matmul(pt_.c(0, 128, 0, E), lhsT=rkg[:, n, :], rhs=ident[:, :], start=True, stop=True), reads=[rkg, ident], writes=[pt_])
                        em.op("act", lambda e: e.activation(out=affT[:, cs], in_=pt_.c(0, 128, 0, E), func=AF.Copy), reads=[pt_], writes=[affT])
                em.dma("sp", RK.h.ap(), affT.h.ap(), reads=[affT], writes=[RK])
                em.barrier()
                ph.close()

                ph = ExitStack()
                rbc = em.sb([128, NTOK], F32, "rbc", ph)
                PTs = [em.sb([128, NTOK], BF16, "PT", ph) for _ in ctiles]
                Pn = em.sb([128, NT, NS], BF16, "Pn", ph)
                xsT = em.sb([128, NCH, NS], BF16, "xsT", ph)
                hidT = em.sb([128, NF, NS], BF16, "hidT", ph)
                ys = [em.sb([128, D], BF16, "ys", ph) for _ in ctiles]
                FG = min(512, Fd)
                wg = [em.sb([128, NCH, FG], BF16, "wg", ph) for _ in range(2)]
                wuf = [em.sb([128, NCH, 128], F32, "wuf", ph) for _ in range(3)]
                wub = [em.sb([128, NCH, 128], BF16, "wub", ph) for _ in range(3)]
                wdf = [em.sb([128, D], F32, "wdf", ph) for _ in range(3)]
                wdb = [em.sb([128, D], BF16, "wdb", ph) for _ in range(3)]
                hs = [em.sb([128, NS], F32, "hs", ph) for _ in range(2)]
                xbt = [em.sb([128, D], BF16, "xbt", ph) for _ in range(2)]
                wi = [0]
                for ex in range(E):
                    em.dma("sp", rbc[:, :], RK[ex:ex + 1, :].to_broadcast([128, NTOK]), reads=[RK], writes=[rbc])
                    for ci, (c0, cw) in enumerate(ctiles):
                        em.op("dve", lambda e: e.tensor_scalar(out=PTs[ci][:, :], in0=rbc[:, :], scalar1=misc[:, 4 + ci:5 + ci], scalar2=None, op0=ALU.is_equal), reads=[rbc, misc], writes=[PTs[ci]])
                    for n in range(NT):
                        em.op("dve", lambda e: e.tensor_scalar(out=Pn[:, n, :], in0=iota[:, 0:NS], scalar1=rkg[:, n, ex:ex + 1], scalar2=None, op0=ALU.is_equal), reads=[iota, rkg], writes=[Pn])
                    for half in range(2):
                        pg = [PF[i] for i in range(4)]
                        for n in range(NT):
                            xb_ = xbt[n % 2]
                            em.dma("sp", xb_[:, :], XB[n, :, :], reads=[XB], writes=[xb_])
                            for k in range(4):
                                ch = half * 4 + k
                                em.op("pe", lambda e: e.matmul(pg[k].c(0, NS), lhsT=xb_[:, ch * 128:(ch + 1) * 128], rhs=Pn[:, n, :], start=(n == 0), stop=(n == NT - 1)), reads=[xb_, Pn], writes=[pg[k]])
                        for k in range(4):
                            ch = half * 4 + k
                            em.op("act", lambda e: e.activation(out=xsT[:, ch, :], in_=pg[k].c(0, NS), func=AF.Copy), reads=[pg[k]], writes=[xsT])
                    nacc = len(ctiles) * 2
                    assert nacc <= 6
                    pacc = [T(banks[i], "pacc%d" % i, trk=BK[i]) for i in range(nacc)]
                    pb = [T(banks[6], "pgate", trk=BK[6]), T(banks[7], "pup", trk=BK[7])]

                    def down_chunk(fi):
                        wb_ = wdb[fi % 3]
                        for ci, (c0, cw) in enumerate(ctiles):
                            for dh in range(2):
                                pa_ = pacc[ci * 2 + dh]
                                em.op("pe", lambda e: e.matmul(pa_.c(0, 512, 0, cw), lhsT=hidT[:, fi, c0:c0 + cw], rhs=wb_[:, dh * 512:(dh + 1) * 512], start=(fi == 0), stop=(fi == NF - 1)),
                                      reads=[hidT, wb_], writes=[pa_])
                    for f0 in range(0, Fd, FG):
                        fw = min(FG, Fd - f0)
                        wi[0] += 1
                        g_ = wg[wi[0] % 2]
                        em.dma("pool", g_[:, :, 0:fw], I["exp_w_gate"][l, ex, :, f0:f0 + fw].rearrange("(c p) f -> p c f", p=128), writes=[g_])
                        for fc in range(fw // 128):
                            fi = f0 // 128 + fc
                            wf_, wb_ = wdf[fi % 3], wdb[fi % 3]
                            em.dma("sp", wf_[:, :], I["exp_w_down"][l, ex, fi * 128:(fi + 1) * 128, :], writes=[wf_])
                            em.op("act", lambda e: e.activation(out=wb_[:, :], in_=wf_[:, :], func=AF.Copy), reads=[wf_], writes=[wb_])
                            uf_, u_ = wuf[fi % 3], wub[fi % 3]
                            em.dma("sp", uf_[:, :, :], I["exp_w_up"][l, ex, :, fi * 128:(fi + 1) * 128].rearrange("(c p) f -> p c f", p=128), writes=[uf_])
                            em.op("dve", lambda e: e.tensor_copy(out=u_[:, :, :], in_=uf_[:, :, :]), reads=[uf_], writes=[u_])
                            p1, p2 = pb
                            for ch in range(NCH):
                                em.op("pe", lambda e: e.matmul(p1.c(0, NS), lhsT=g_[:, ch, fc * 128:(fc + 1) * 128], rhs=xsT[:, ch, :], start=(ch == 0), stop=(ch == NCH - 1)), reads=[g_, xsT], writes=[p1])
                            for ch in range(NCH):
                                em.op("pe", lambda e: e.matmul(p2.c(0, NS), lhsT=u_[:, ch, :], rhs=xsT[:, ch, :], start=(ch == 0), stop=(ch == NCH - 1)), reads=[u_, xsT], writes=[p2])
                            h_ = hs[fi % 2]
                            em.op("act", lambda e: e.activation(out=h_[:, :], in_=p1.c(0, NS), func=AF.Silu), reads=[p1], writes=[h_])
                            em.op("dve", lambda e: e.tensor_tensor(out=hidT[:, fi, :], in0=h_[:, :], in1=p2.c(0, NS), op=ALU.mult), reads=[h_, p2], writes=[hidT])
                            if fi >= 1:
                                down_chunk(fi - 1)
                    down_chunk(NF - 1)
                    for ci, (c0, cw) in enumerate(ctiles):
                        for dh in range(2):
                            pa_ = pacc[ci * 2 + dh]
                            em.op("act", lambda e: e.activation(out=ys[ci][0:cw, dh * 512:(dh + 1) * 512], in_=pa_.c(0, 512, 0, cw), func=AF.Copy), reads=[pa_], writes=[ys[ci]])
                    for n in range(NT):
                        cs = slice(n * 128, (n + 1) * 128)
                        if n < NTC:
                            use = [ci for ci, (c0, cw) in enumerate(ctiles) if c0 + cw > capL]
                        else:
                            use = [ci for ci, (c0, cw) in enumerate(ctiles) if c0 < capL]
                        for dh in range(2):
                            p = pfull()
                            for k, ci in enumerate(use):
                                c0, cw = ctiles[ci]
                                em.op("pe", lambda e: e.matmul(p.c(0, 512), lhsT=PTs[ci][0:cw, cs], rhs=ys[ci][0:cw, dh * 512:(dh + 1) * 512], start=(k == 0), stop=(k == len(use) - 1)),
                                      reads=[PTs[ci], ys[ci]], writes=[p])
                            em.op("dve", lambda e: e.scalar_tensor_tensor(out=yacc[:, n, dh * 512:(dh + 1) * 512], in0=p.c(0, 512), scalar=aff[:, n, ex:ex + 1], in1=yacc[:, n, dh * 512:(dh + 1) * 512], op0=ALU.mult, op1=ALU.add),
                                  reads=[p, aff, yacc], writes=[yacc])
                em.barrier()
                ph.close()
                ph = ExitStack()
                bcs = [em.sb([128, D], F32, "bc", ph) for _ in range(3)]
                tmp = {"st2": em.sb([128, 8], F32, "st2", ph), "junk": em.sb([128, D], F32, "junk", ph)}
                xts = [em.sb([128, D], F32, "xts", ph) for _ in range(2)]
                x2f = [em.sb([128, D], F32, "x2f", ph) for _ in range(2)]
                em.dma("sp", bcs[1][:, :], I["ln_w"][l, 1:2, :].to_broadcast([128, D]), writes=[bcs[1]])
                em.dma("sp", bcs[2][:, :], I["ln_b"][l, 1:2, :].to_broadcast([128, D]), writes=[bcs[2]])
                for n in range(NT):
                    if last and n < NTC:
                        continue
                    bi = n % 2
                    cs = slice(n * 128, (n + 1) * 128)
                    if n == 0 or n == NTC or (last and n == NTC):
                        row = B if n < NTC else s
                        bc_load(bcs[0], MOD[l, row:row + 1, 5 * D:6 * D], MOD)
                    if cfg.debug and l == 0 and s == 0:
                        em.dma("sp", DBG["ffn"][cs, :], yacc[:, n, :], reads=[yacc], writes=[DBG["ffn"]])
                    em.dma("sp", xts[bi][:, :], XS[s, cs, :], reads=[XS], writes=[xts[bi]])
                    em.op("dve", lambda e: e.tensor_tensor(out=x2f[bi][:, :], in0=yacc[:, n, :], in1=bcs[0][:, :], op=ALU.mult), reads=[yacc, bcs[0]], writes=[x2f[bi]])
                    em.op("dve", lambda e: e.scalar_tensor_tensor(out=x2f[bi][:, :], in0=xts[bi][:, :], scalar=DN_ALPHA, in1=x2f[bi][:, :], op0=ALU.mult, op1=ALU.add), reads=[xts[bi], x2f[bi]], writes=[x2f[bi]])
                    layernorm_tile(tmp, x2f[bi], bcs[1], bcs[2], xts[bi])
                    if last:
                        em.dma("sp", OUT[s, (n - NTC) * 128:(n - NTC + 1) * 128, :], xts[bi][:, :], reads=[xts[bi]], writes=[OUT])
                    else:
                        em.dma("sp", XS[s, cs, :], xts[bi][:, :], reads=[xts[bi]], writes=[XS])
                em.barrier()
                ph.close()
            em.barrier()

        try:
            for s in range(B):
                for l in range(cfg.DEPTH):
                    if cfg.stage >= 2 + 2 * l:
                        mixer_phase(s, l)
                    if cfg.stage >= 3 + 2 * l:
                        ffn_phase(s, l)
        except Exception:
            import traceback
            traceback.print_exc()
            raise
        em.barrier()
        build.ninstr = em.n
    return nc, consts


_CACHE = {}


def run(cfg, inputs, ncores, trace=False):
    key = (cfg.B, cfg.LC, cfg.LL, cfg.F, cfg.DEPTH, cfg.debug, cfg.stage)
    if key not in _CACHE:
        _CACHE[key] = build(cfg)
    nc, consts = _CACHE[key]
    B = cfg.B
    in_maps = []
    shp = INPUT_SHAPES(cfg)
    for c in range(ncores):
        m = {}
        for k in shp:
            a = np.asarray(inputs[k], dtype=np.float32)
            if k in ("x", "c", "ctx"):
                a = a[c * B:(c + 1) * B]
            a = np.ascontiguousarray(a).reshape(shp[k])
            m[k] = a
        m.update(consts)
        in_maps.append(m)
    res = run_bass_kernel_spmd(nc, in_maps, core_ids=list(range(ncores)), trace=trace)
    return res


def kernel(**inputs):
    ncores = 8
    cfg = Cfg(B=32 // ncores)
    res = run(cfg, inputs, ncores)
    out = np.concatenate([r["out"] for r in res.results], axis=0)
    return out.astype(np.float32)
```
